# Optimizing a Trainium2 kernel written in Bass

```python
import jax, jax.numpy as jnp
from jax import lax
import numpy as np

D_MODEL = 1024
BATCH = 2
SEQ = 16384
DEPTH = 2

MEM_LEN = 256
HEAD_DIM = 64
LRU_WIDTH = 256
LRU_BLOCKS = 4
LRU_BLOCK = LRU_WIDTH // LRU_BLOCKS
CONV_WIDTH = 4
LRU_C = 8.0
FOX_HEADS = 8
FOX_WIDTH = FOX_HEADS * HEAD_DIM
RET_HEADS = 4
RET_WIDTH = RET_HEADS * HEAD_DIM
MIX_WIDTH = LRU_WIDTH + FOX_WIDTH + RET_WIDTH
SPLIT_SIZES = (LRU_WIDTH, LRU_WIDTH, FOX_WIDTH, FOX_WIDTH, FOX_WIDTH, FOX_HEADS, RET_WIDTH, RET_WIDTH, RET_WIDTH, RET_WIDTH)
IN_WIDTH = sum(SPLIT_SIZES)
CROSS_HEADS = 4
CROSS_HEAD_DIM = D_MODEL // CROSS_HEADS
D_FF = ((8 * D_MODEL + 3 * 256 - 1) // (3 * 256)) * 256
Q_BLOCK = 128
RET_CHUNK = 128
ROPE_THETA = 10000.0
EPS = 1e-6

kernel_name = 'hymba_style_lru_fox_retention_hybrid'


def rms_norm(x, g):
    xf = x.astype(jnp.float32)
    y = xf * lax.rsqrt(jnp.mean(xf * xf, axis=-1, keepdims=True) + EPS)
    return (y * g.astype(jnp.float32)).astype(x.dtype)


def causal_depthwise_conv(x, w, b):
    c = x.shape[-1]
    xp = jnp.pad(x, ((0, 0), (CONV_WIDTH - 1, 0), (0, 0)))
    out = lax.conv_general_dilated(xp, w.astype(x.dtype)[:, None, :], window_strides=(1,), padding='VALID', dimension_numbers=('NWC', 'WIO', 'NWC'), feature_group_count=c)
    return out + b.astype(x.dtype)


def rg_lru(x, w_r, b_r, w_i, b_i, lam):
    bsz, s, _ = x.shape
    xb = x.reshape(bsz, s, LRU_BLOCKS, LRU_BLOCK)
    r = jax.nn.sigmoid((jnp.einsum('bsni,nij->bsnj', xb, w_r).reshape(bsz, s, LRU_WIDTH) + b_r).astype(jnp.float32))
    i = jax.nn.sigmoid((jnp.einsum('bsni,nij->bsnj', xb, w_i).reshape(bsz, s, LRU_WIDTH) + b_i).astype(jnp.float32))
    log_a = -LRU_C * r * jax.nn.softplus(-lam.astype(jnp.float32))
    a = jnp.exp(log_a)
    u = jnp.sqrt(-jnp.expm1(2.0 * log_a)) * (i * x.astype(jnp.float32))

    def combine(left, right):
        a1, b1 = left
        a2, b2 = right
        return a1 * a2, a2 * b1 + b2

    _, h = lax.associative_scan(combine, (a, u), axis=1)
    return h.astype(x.dtype)


def forgetting_attention(q, k, v, f_logit, b_f):
    bsz, s, _ = q.shape
    q = q.reshape(bsz, s, FOX_HEADS, HEAD_DIM).transpose(0, 2, 1, 3)
    k = k.reshape(bsz, s, FOX_HEADS, HEAD_DIM).transpose(0, 2, 1, 3)
    v = v.reshape(bsz, s, FOX_HEADS, HEAD_DIM).transpose(0, 2, 1, 3)
    log_f = jax.nn.log_sigmoid((f_logit + b_f).astype(jnp.float32))
    c = jnp.cumsum(log_f, axis=1).transpose(0, 2, 1)
    key_pos = jnp.arange(s)
    scale = HEAD_DIM ** -0.5

    def block(bi):
        start = bi * Q_BLOCK
        qb = lax.dynamic_slice_in_dim(q, start, Q_BLOCK, axis=2)
        cb = lax.dynamic_slice_in_dim(c, start, Q_BLOCK, axis=2)
        logits = jnp.einsum('bhqd,bhkd->bhqk', qb, k).astype(jnp.float32) * scale + cb[..., None] - c[:, :, None, :]
        q_pos = start + jnp.arange(Q_BLOCK)
        mask = key_pos[None, :] <= q_pos[:, None]
        p = jax.nn.softmax(jnp.where(mask, logits, -jnp.inf), axis=-1)
        return jnp.einsum('bhqk,bhkd->bhqd', p.astype(v.dtype), v)

    out = lax.map(block, jnp.arange(s // Q_BLOCK))
    return out.transpose(1, 0, 3, 2, 4).reshape(bsz, s, FOX_WIDTH)


def rotary(x, positions):
    half = x.shape[-1] // 2
    inv_freq = ROPE_THETA ** (-jnp.arange(half, dtype=jnp.float32) / half)
    ang = positions.astype(jnp.float32)[..., None] * inv_freq
    cos = jnp.cos(ang)[:, :, None, :]
    sin = jnp.sin(ang)[:, :, None, :]
    xf = x.astype(jnp.float32)
    x1, x2 = xf[..., :half], xf[..., half:]
    return jnp.concatenate([x1 * cos - x2 * sin, x1 * sin + x2 * cos], axis=-1)


def retention(q, k, v, g, positions):
    bsz, s, _ = q.shape
    n_chunks = s // RET_CHUNK
    shp = (bsz, n_chunks, RET_CHUNK, RET_HEADS, HEAD_DIM)
    qc = rotary(q.reshape(bsz, s, RET_HEADS, HEAD_DIM), positions).reshape(shp)
    kc = (rotary(k.reshape(bsz, s, RET_HEADS, HEAD_DIM), positions) * HEAD_DIM ** -0.5).reshape(shp)
    vc = v.astype(jnp.float32).reshape(shp)
    log_gamma = jnp.log1p(-jnp.exp2(-5.0 - jnp.arange(RET_HEADS, dtype=jnp.float32)))
    idx = jnp.arange(RET_CHUNK, dtype=jnp.float32)
    diff = idx[:, None] - idx[None, :]
    decay = jnp.where(diff >= 0, jnp.exp(log_gamma[:, None, None] * jnp.maximum(diff, 0.0)), 0.0)
    scores = jnp.einsum('bnihd,bnjhd->bnhij', qc, kc) * decay[None, None]
    y_inner = jnp.einsum('bnhij,bnjhd->bnihd', scores, vc)
    k_w = jnp.exp(log_gamma[None, :] * (RET_CHUNK - 1.0 - idx)[:, None])
    u = jnp.einsum('bnjhd,bnjhe,jh->bnhde', kc, vc, k_w)
    chunk_decay = jnp.exp(log_gamma * RET_CHUNK)[None, :, None, None]

    def step(state, u_n):
        return chunk_decay * state + u_n, state

    init = jnp.zeros((bsz, RET_HEADS, HEAD_DIM, HEAD_DIM), jnp.float32)
    _, prev_states = lax.scan(step, init, u.transpose(1, 0, 2, 3, 4))
    q_w = jnp.exp(log_gamma[None, :] * (idx + 1.0)[:, None])
    y_cross = jnp.einsum('bnihd,nbhde,ih->bnihe', qc, prev_states, q_w)
    y = (y_inner + y_cross).reshape(bsz, s, RET_HEADS, HEAD_DIM)
    mu = jnp.mean(y, axis=-1, keepdims=True)
    var = jnp.mean(jnp.square(y - mu), axis=-1, keepdims=True)
    y = ((y - mu) * lax.rsqrt(var + EPS)).reshape(bsz, s, RET_WIDTH)
    return (jax.nn.silu(g.astype(jnp.float32)) * y).astype(g.dtype)


def parallel_mixer(h, positions, w_in, conv_w, conv_b, w_rg, b_rg, w_ig, b_ig, lru_lambda, fox_b_f, w_out):
    proj = h @ w_in
    lx, ly, fq, fk, fv, ff, rq, rk, rv, rg = jnp.split(proj, np.cumsum(SPLIT_SIZES)[:-1].tolist(), axis=-1)
    lru_out = rg_lru(causal_depthwise_conv(lx, conv_w, conv_b), w_rg, b_rg, w_ig, b_ig, lru_lambda) * jax.nn.gelu(ly)
    fox_out = forgetting_attention(fq, fk, fv, ff, fox_b_f)
    ret_out = retention(rq, rk, rv, rg, positions)
    return jnp.concatenate([lru_out, fox_out.astype(h.dtype), ret_out], axis=-1) @ w_out


def memory_cross_attention(h, mem_n, w_q, w_k, w_v, w_o):
    bsz, s, _ = h.shape
    m = mem_n.shape[1]
    q = (h @ w_q).reshape(bsz, s, CROSS_HEADS, CROSS_HEAD_DIM)
    k = (mem_n @ w_k).reshape(bsz, m, CROSS_HEADS, CROSS_HEAD_DIM)
    v = (mem_n @ w_v).reshape(bsz, m, CROSS_HEADS, CROSS_HEAD_DIM)
    logits = jnp.einsum('bshd,bmhd->bhsm', q, k).astype(jnp.float32) * CROSS_HEAD_DIM ** -0.5
    p = jax.nn.softmax(logits, axis=-1)
    o = jnp.einsum('bhsm,bmhd->bshd', p.astype(v.dtype), v).reshape(bsz, s, D_MODEL)
    return o @ w_o


def swiglu(h, w_gate, w_up, w_down):
    return (jax.nn.silu(h @ w_gate) * (h @ w_up)) @ w_down


def setup_inputs(seed: int = 0) -> dict:
    key = jax.random.key(seed)
    ks = jax.random.split(key, 32)

    def nrm(k, shape, scale):
        return scale * jax.random.normal(k, shape, jnp.float32)

    def gain(k, shape):
        return 1.0 + 0.02 * jax.random.normal(k, shape, jnp.float32)

    u = jax.random.uniform(ks[12], (DEPTH, LRU_WIDTH), jnp.float32, 0.9, 0.999)
    a_base = u ** (1.0 / LRU_C)
    lru_lambda = jnp.log(a_base) - jnp.log1p(-a_base)
    return {
        'x': nrm(ks[0], (BATCH, SEQ, D_MODEL), 1.0),
        'mem': nrm(ks[1], (BATCH, MEM_LEN, D_MODEL), 1.0),
        'positions': jnp.tile(jnp.arange(SEQ, dtype=jnp.int32)[None, :], (BATCH, 1)),
        'pre_mix_g': gain(ks[2], (DEPTH, D_MODEL)),
        'post_mix_g': gain(ks[3], (DEPTH, D_MODEL)),
        'w_in': nrm(ks[4], (DEPTH, D_MODEL, IN_WIDTH), D_MODEL ** -0.5),
        'conv_w': nrm(ks[5], (DEPTH, CONV_WIDTH, LRU_WIDTH), CONV_WIDTH ** -0.5),
        'conv_b': nrm(ks[6], (DEPTH, LRU_WIDTH), 0.01),
        'w_rg': nrm(ks[7], (DEPTH, LRU_BLOCKS, LRU_BLOCK, LRU_BLOCK), LRU_BLOCK ** -0.5),
        'b_rg': nrm(ks[8], (DEPTH, LRU_WIDTH), 0.01),
        'w_ig': nrm(ks[9], (DEPTH, LRU_BLOCKS, LRU_BLOCK, LRU_BLOCK), LRU_BLOCK ** -0.5),
        'b_ig': nrm(ks[10], (DEPTH, LRU_WIDTH), 0.01),
        'lru_lambda': lru_lambda,
        'fox_b_f': 2.0 + nrm(ks[11], (DEPTH, FOX_HEADS), 0.5),
        'w_out': nrm(ks[13], (DEPTH, MIX_WIDTH, D_MODEL), MIX_WIDTH ** -0.5),
        'pre_cross_g': gain(ks[14], (DEPTH, D_MODEL)),
        'post_cross_g': gain(ks[15], (DEPTH, D_MODEL)),
        'mem_norm_g': gain(ks[16], (D_MODEL,)),
        'w_cq': nrm(ks[17], (DEPTH, D_MODEL, D_MODEL), D_MODEL ** -0.5),
        'w_ck': nrm(ks[18], (DEPTH, D_MODEL, D_MODEL), D_MODEL ** -0.5),
        'w_cv': nrm(ks[19], (DEPTH, D_MODEL, D_MODEL), D_MODEL ** -0.5),
        'w_co': nrm(ks[20], (DEPTH, D_MODEL, D_MODEL), D_MODEL ** -0.5),
        'pre_ffn_g': gain(ks[21], (DEPTH, D_MODEL)),
        'post_ffn_g': gain(ks[22], (DEPTH, D_MODEL)),
        'w_gate': nrm(ks[23], (DEPTH, D_MODEL, D_FF), D_MODEL ** -0.5),
        'w_up': nrm(ks[24], (DEPTH, D_MODEL, D_FF), D_MODEL ** -0.5),
        'w_down': nrm(ks[25], (DEPTH, D_FF, D_MODEL), D_FF ** -0.5),
    }


def reference(x, mem, positions, pre_mix_g, post_mix_g, w_in, conv_w, conv_b, w_rg, b_rg, w_ig, b_ig, lru_lambda, fox_b_f, w_out, pre_cross_g, post_cross_g, mem_norm_g, w_cq, w_ck, w_cv, w_co, pre_ffn_g, post_ffn_g, w_gate, w_up, w_down):
    mem_n = rms_norm(mem, mem_norm_g)
    for l in range(DEPTH):
        h = rms_norm(x, pre_mix_g[l])
        mix = parallel_mixer(h, positions, w_in[l], conv_w[l], conv_b[l], w_rg[l], b_rg[l], w_ig[l], b_ig[l], lru_lambda[l], fox_b_f[l], w_out[l])
        x = x + rms_norm(mix, post_mix_g[l])
        h = rms_norm(x, pre_cross_g[l])
        x = x + rms_norm(memory_cross_attention(h, mem_n, w_cq[l], w_ck[l], w_cv[l], w_co[l]), post_cross_g[l])
        h = rms_norm(x, pre_ffn_g[l])
        x = x + rms_norm(swiglu(h, w_gate[l], w_up[l], w_down[l]), post_ffn_g[l])
    return x
```

```python
import numpy as np
import ml_dtypes
from contextlib import ExitStack
import concourse.bass as bass
import concourse.mybir as mybir
from concourse.bass_utils import run_bass_kernel_spmd

F32 = mybir.dt.float32
BF16 = mybir.dt.bfloat16
I32 = mybir.dt.int32
AF = mybir.ActivationFunctionType
ALU = mybir.AluOpType
AX = mybir.AxisListType

D = 1024
SEQ = 16384
NB = 2
DEPTH = 2
TOK = 4096
DFF = 2816
NFF = DFF // 128
EPS = 1e-6
SPLIT = (256, 256, 512, 512, 512, 8, 256, 256, 256, 256)
OFF = np.concatenate([[0], np.cumsum(SPLIT)]).tolist()
PI = float(np.pi)


class Prog:
    ENGS = ("pe", "act", "dve", "pool", "sp")

    def __init__(self, nc, es, n_dma_sems=48):
        self.nc = nc
        self.eng_sem = {e: es.enter_context(nc.semaphore("s_" + e)) for e in self.ENGS}
        self.eng_cnt = {e: 0 for e in self.ENGS}
        self.dma_sems = [es.enter_context(nc.semaphore("d%d" % i)) for i in range(n_dma_sems)]
        self.dma_cnt = [0] * n_dma_sems
        self.dma_key2idx = {}
        self.waited = {e: {} for e in self.ENGS}
        self.cc_sem = es.enter_context(nc.semaphore("s_cc"))
        self.cc_scratch = es.enter_context(nc.sbuf_tensor("cc_scratch", [128, 8], F32))
        self.cc_cnt = 0
        self.ops = []
        self.state = {}
        self.last = {}
        self.pending_dma = []

    def dma_sem_for(self, key):
        if key not in self.dma_key2idx:
            idx = len(self.dma_key2idx)
            assert idx < len(self.dma_sems), "out of dma sems"
            self.dma_key2idx[key] = idx
        return self.dma_key2idx[key]

    def op(self, eng, fn, reads=(), writes=(), dma=None, extra=()):
        deps = set(extra)
        for k in reads:
            st = self.state.setdefault(k, {"w": None, "r": []})
            if st["w"] is not None:
                deps.add(st["w"])
        for k in writes:
            st = self.state.setdefault(k, {"w": None, "r": []})
            if st["w"] is not None:
                deps.add(st["w"])
            deps.update(st["r"])
        oid = len(self.ops)
        self.ops.append(dict(id=oid, eng=eng, fn=fn, deps=deps,
                             dma=None if dma is None else self.dma_sem_for(dma)))
        for k in reads:
            self.state[k]["r"].append(oid)
        for k in writes:
            self.state[k] = {"w": oid, "r": []}
        if dma is None:
            self.last[eng] = oid
        else:
            self.pending_dma.append(oid)
        return oid

    def dma(self, q, out, in_, reads, writes, semkey, **kw):
        return self.op(q, lambda e: e.dma_start(out=out, in_=in_, **kw), reads, writes, dma=semkey)

    def collective(self, kind, in_ap, out_ap, groups, reads, writes):
        def fn(e):
            return e.collective_compute(kind, ALU.bypass, replica_groups=groups, ins=[in_ap.opt()], outs=[out_ap.opt()])
        oid = self.op("pool", fn, reads, writes)
        self.ops[oid]["cc"] = True
        scr = self.cc_scratch
        self.op("pool", lambda e: e.memset(scr[:], 0.0), list(writes), list(writes))
        return oid

    def barrier(self):
        ids = list(self.last.values()) + list(self.pending_dma)
        for e in self.ENGS:
            self.op(e, lambda en: None, extra=ids)
        self.pending_dma = []
        self.state = {}

    def emit(self):
        ops = self.ops

        def pe_pe(a, b):
            return a["eng"] == "pe" and b["eng"] == "pe" and a["dma"] is None and b["dma"] is None

        needed = set()
        for o in ops:
            for d in o["deps"]:
                if not pe_pe(ops[d], o):
                    needed.add(d)
        for o in ops:
            if o.get("cc"):
                self.cc_cnt += 1
                o["sig"] = (self.cc_sem, self.cc_cnt, None)
            elif o["dma"] is not None:
                self.dma_cnt[o["dma"]] += 16
                o["sig"] = (self.dma_sems[o["dma"]], self.dma_cnt[o["dma"]], 16)
            elif o["id"] in needed:
                self.eng_cnt[o["eng"]] += 1
                o["sig"] = (self.eng_sem[o["eng"]], self.eng_cnt[o["eng"]], 1)
            else:
                o["sig"] = None
        per = {e: [] for e in self.ENGS}
        carry = {e: [] for e in self.ENGS}
        for o in ops:
            w = {}
            for d in o["deps"]:
                if pe_pe(ops[d], o):
                    continue
                sg = ops[d]["sig"]
                k = id(sg[0])
                if k not in w or w[k][1] < sg[1]:
                    w[k] = (sg[0], sg[1])
            wl = carry[o["eng"]]
            carry[o["eng"]] = []
            wd = self.waited[o["eng"]]
            for k, (s, v) in w.items():
                if wd.get(k, 0) >= v:
                    continue
                wd[k] = v
                wl.append((s, v))
            o["waits"] = wl
            per[o["eng"]].append(o)

        def replay(lst):
            def f(e):
                pend_sig = None
                for o in lst:
                    for (s, v) in o["waits"]:
                        e.wait_ge(s, v)
                    ins = o["fn"](e)
                    if o["sig"] is not None:
                        assert ins is not None, "signalling op must emit an instruction"
                        if o["sig"][2] is None:
                            ins.then_inc(o["sig"][0])
                        else:
                            ins.then_inc(o["sig"][0], o["sig"][2])
            return f

        with self.nc.Block() as block:
            if per["pe"]:
                block.tensor(replay(per["pe"]))
            if per["act"]:
                block.scalar(replay(per["act"]))
            if per["dve"]:
                block.vector(replay(per["dve"]))
            if per["pool"]:
                block.gpsimd(replay(per["pool"]))
            if per["sp"]:
                block.sync(replay(per["sp"]))
        n = {e: len(per[e]) for e in self.ENGS}
        self.ops = []
        self.state = {}
        self.last = {}
        self.pending_dma = []
        return n


class Ctx:
    def __init__(self, nc, es, P):
        self.nc, self.es, self.P = nc, es, P
        self.n = 0

    UID = [0]

    def sb(self, name, shape, dt=F32):
        Ctx.UID[0] += 1
        return self.es.enter_context(self.nc.sbuf_tensor("sb%d_%s" % (Ctx.UID[0], name), shape, dt))

    def ps(self, name, shape, dt=F32):
        Ctx.UID[0] += 1
        return self.es.enter_context(self.nc.psum_tensor("ps%d_%s" % (Ctx.UID[0], name), shape, dt))


def make_ident(P, ident, key="ident"):
    P.op("pool", lambda e: e.memset(ident[:], 1.0), [], [key])

    def sel(e):
        if getattr(P, "zero_reg", None) is None:
            P.zero_reg = e.to_reg(0.0)
        return e.affine_select(out=ident[:], in_=ident[:], pattern=[[-1, 128]], compare_op=ALU.is_equal,
                               fill=P.zero_reg, base=0, channel_multiplier=1)
    P.op("pool", sel, [key], [key])


def load_bcast_rows(P, C, name, src_row_ap, n):
    t = C.sb(name, [128, n])
    P.dma("sp", t[:], src_row_ap.partition_broadcast(128), [], [name], name)
    return t


def load_weight_bf16(P, C, w_ap, K, N, dst, dkey, stg, eng_cast="pool"):
    wv = w_ap.rearrange("(c p) n -> p c n", p=128)
    nk = K // 128
    i = 0
    for c in range(nk):
        for n0 in range(0, N, 1024):
            n1 = min(N, n0 + 1024)
            sk = ("stg", stg["i"] % 2)
            st = stg["t"][stg["i"] % 2]
            stg["i"] += 1
            P.dma("sp", st[:, 0:n1 - n0], wv[:, c, n0:n1], [], [sk], sk)
            P.op(eng_cast, lambda e, st=st, c=c, n0=n0, n1=n1: e.tensor_copy(out=dst[:, c, n0:n1], in_=st[:, 0:n1 - n0]),
                 [sk], [dkey])


def rstd_of(P, C, src, skey, tag, junk, n=D):
    ss = C.tmp["ss"]
    P.op("act", lambda e: e.activation(out=junk[:, 0:n], in_=src, func=AF.Square, accum_out=ss[:, 0:1]),
         [skey], ["junk", "ss"])
    P.op("dve", lambda e: e.tensor_scalar(out=ss[:, 1:2], in0=ss[:, 0:1], scalar1=1.0 / n, scalar2=EPS,
                                          op0=ALU.mult, op1=ALU.add), ["ss"], ["ss1"])
    P.op("act", lambda e: e.activation(out=ss[:, 2:3], in_=ss[:, 1:2], func=AF.Sqrt), ["ss1"], ["ss2"])
    P.op("dve", lambda e: e.reciprocal(out=ss[:, 3:4], in_=ss[:, 2:3]), ["ss2"], ["ss3"])
    return ss[:, 3:4], "ss3"


def norm_T(P, C, src, skey, g_t, gkey, hbf, ident, trp, trkey, dstT, dkey, col0):
    r, rk = rstd_of(P, C, src, skey, "n", C.tmp["junk"])
    P.op("dve", lambda e: e.scalar_tensor_tensor(out=hbf[:], in0=src, scalar=r, in1=g_t[:],
                                                 op0=ALU.mult, op1=ALU.mult), [skey, rk, gkey], ["hbf"])
    for c in range(8):
        P.op("pe", lambda e, c=c: e.transpose(trp[:, c * 128:(c + 1) * 128], hbf[:, c * 128:(c + 1) * 128], ident[:]),
             ["hbf", "ident"], [trkey])
    P.op("act", lambda e: e.copy(out=dstT[:, :, col0:col0 + 128],
                                 in_=trp[:].rearrange("p (c t) -> p c t", c=8)), [trkey], [dkey])


def emit_P0(nc, P, x_ap, g_ap, hT_ap, ntok):
    with ExitStack() as es:
        C = Ctx(nc, es, P)
        C.tmp = {"ss": C.sb("ss", [128, 4]), "junk": C.sb("junk", [128, D], BF16)}
        ident = C.sb("ident", [128, 128], BF16)
        make_ident(P, ident)
        g_t = load_bcast_rows(P, C, "g0", g_ap, D)
        hbf = C.sb("hbf", [128, D], BF16)
        xs = [C.sb("x%d" % i, [128, D]) for i in range(2)]
        trp = [C.ps("tr%d" % i, [128, D], BF16) for i in range(2)]
        hT = [C.sb("hT%d" % i, [128, 8, 512], BF16) for i in range(2)]
        hTv = hT_ap.rearrange("(c p) t -> p c t", p=128)
        for st in range(ntok // 512):
            hk = ("hT", st % 2)
            for sub in range(4):
                i = st * 4 + sub
                xk = ("x", i % 2)
                P.dma("sp", xs[i % 2][:], x_ap[i * 128:(i + 1) * 128, :], [], [xk], xk)
                norm_T(P, C, xs[i % 2][:], xk, g_t, "g0", hbf, ident, trp[i % 2], ("tr", i % 2),
                       hT[st % 2], hk, sub * 128)
            P.dma("sp", hTv[:, :, st * 512:(st + 1) * 512], hT[st % 2][:], [hk], ["hT_out"], hk)
        P.barrier()
        return P.emit()


def emit_B1(nc, P, mixT_ap, x_ap, mem_ap, gm_ap, w_out, w_cq, w_ck, w_cv, w_co,
            g_post_mix, g_pre_cross, g_post_cross, x2_ap, mix_fn=None):
    with ExitStack() as es:
        C = Ctx(nc, es, P)
        C.tmp = {"ss": C.sb("ss", [128, 4]), "junk": C.sb("junk", [128, D], BF16)}
        ident = C.sb("ident", [128, 128], BF16)
        make_ident(P, ident)
        ones_bf = C.sb("ones_bf", [128, 128], BF16)
        P.op("pool", lambda e: e.memset(ones_bf[:], 1.0), [], ["ones_bf"])
        stg = {"i": 0, "t": [C.sb("stg%d" % i, [128, 1024]) for i in range(2)]}
        gpm = load_bcast_rows(P, C, "gpm", g_post_mix, D)
        gpc = load_bcast_rows(P, C, "gpc", g_pre_cross, D)
        gqc = load_bcast_rows(P, C, "gqc", g_post_cross, D)
        gmm = load_bcast_rows(P, C, "gmm", gm_ap, D)
        wo = C.sb("wo", [128, 8, D], BF16)
        wq = C.sb("wq", [128, 8, D], BF16)
        wc = C.sb("wc", [128, 8, D], BF16)
        wk = C.sb("wk", [128, 8, D], BF16)
        load_weight_bf16(P, C, w_ck, D, D, wk, "wk", stg)
        hbf = C.sb("hbf", [128, D], BF16)
        acc = [C.ps("acc%d" % i, [128, D]) for i in range(2)]
        trp = [C.ps("tr%d" % i, [128, D], BF16) for i in range(2)]
        mb = [C.ps("mb%d" % i, [128, 512]) for i in range(2)]
        memT = C.sb("memT", [128, 8, 256], BF16)
        xs = [C.sb("x%d" % i, [128, D]) for i in range(4)]
        for i in range(2):
            xk = ("x", i)
            P.dma("sp", xs[i][:], mem_ap[i * 128:(i + 1) * 128, :], [], [xk], xk)
            norm_T(P, C, xs[i][:], xk, gmm, "gmm", hbf, ident, trp[i], ("tr", i), memT, "memT", i * 128)
        kT = C.sb("kT", [128, 8, 256], BF16)
        for cc in range(8):
            for f in range(8):
                P.op("pe", lambda e, cc=cc, f=f: e.matmul(mb[cc % 2][:, 0:256], lhsT=wk[:, f, cc * 128:(cc + 1) * 128],
                                                           rhs=memT[:, f, :], start=(f == 0), stop=(f == 7)),
                     ["wk", "memT"], [("mb", cc % 2)])
            P.op("act", lambda e, cc=cc: e.copy(out=kT[:, cc, :], in_=mb[cc % 2][:, 0:256]), [("mb", cc % 2)], ["kT"])
        load_weight_bf16(P, C, w_cv, D, D, wk, "wk", stg)
        vm = C.sb("vm", [128, 2, D], BF16)
        for m in range(2):
            for half in range(2):
                for f in range(8):
                    P.op("pe", lambda e, m=m, half=half, f=f: e.matmul(
                        acc[m][:, half * 512:(half + 1) * 512], lhsT=memT[:, f, m * 128:(m + 1) * 128],
                        rhs=wk[:, f, half * 512:(half + 1) * 512], start=(f == 0), stop=(f == 7)),
                        ["wk", "memT"], [("acc", m)])
            P.op("act", lambda e, m=m: e.copy(out=vm[:, m, :], in_=acc[m][:]), [("acc", m)], ["vm"])
        load_weight_bf16(P, C, w_out, D, D, wo, "wo", stg)
        load_weight_bf16(P, C, w_cq, D, D, wq, "wq", stg)
        load_weight_bf16(P, C, w_co, D, D, wc, "wc", stg)
        mixT = [C.sb("mixT%d" % i, [128, 8, 512], BF16) for i in range(2)]
        h2T = C.sb("h2T", [128, 8, 512], BF16)
        qT = C.sb("qT", [128, 8, 512], BF16)
        PT = C.sb("PT", [128, 8, 512], BF16)
        oT = C.sb("oT", [128, 8, 512], BF16)
        rec = C.sb("rec", [128, 512])
        tmp = C.sb("tmp", [128, D])
        mix_q = "pool"
        if mix_fn is None:
            mix_q = "sp"
            mixv = mixT_ap.rearrange("(c p) t -> p c t", p=128)
            mix_fn = lambda e, st, r: mixv[:, 2 * r:2 * r + 2, st * 512:(st + 1) * 512]
        nst = TOK // 512
        for st in range(nst):
            mk = ("mixT", st % 2)
            for r_ in range(4):
                mkr = ("mixT", st % 2, r_)
                P.op(mix_q, lambda e, st=st, r_=r_: e.dma_start(out=mixT[st % 2][:, 2 * r_:2 * r_ + 2, :], in_=mix_fn(e, st, r_)), [], [mkr], dma=mkr)
            for sub in range(4):
                t0 = st * 512 + sub * 128
                xk = ("x", sub)
                ak = ("acc", sub % 2)
                a = acc[sub % 2]
                P.dma("sp", xs[sub][:], x_ap[t0:t0 + 128, :], [], [xk], xk)
                for half in range(2):
                    for c in range(8):
                        P.op("pe", lambda e, a=a, half=half, c=c, sub=sub, st=st: e.matmul(
                            a[:, half * 512:(half + 1) * 512], lhsT=mixT[st % 2][:, c, sub * 128:(sub + 1) * 128],
                            rhs=wo[:, c, half * 512:(half + 1) * 512], start=(c == 0), stop=(c == 7)),
                            [("mixT", st % 2, c // 2), "wo"], [ak])
                r, rk = rstd_of(P, C, a[:], ak, "y", C.tmp["junk"])
                P.op("dve", lambda e, a=a, r=r: e.scalar_tensor_tensor(out=tmp[:], in0=a[:], scalar=r, in1=gpm[:],
                                                                       op0=ALU.mult, op1=ALU.mult), [ak, rk, "gpm"], ["tmp"])
                P.op("pool", lambda e, sub=sub: e.tensor_tensor(out=xs[sub][:], in0=xs[sub][:], in1=tmp[:], op=ALU.add),
                     [xk, "tmp"], [xk])
                norm_T(P, C, xs[sub][:], xk, gpc, "gpc", hbf, ident, trp[sub % 2], ("tr", sub % 2), h2T, "h2T", sub * 128)
            for cc in range(8):
                for f in range(8):
                    P.op("pe", lambda e, cc=cc, f=f: e.matmul(mb[cc % 2][:], lhsT=wq[:, f, cc * 128:(cc + 1) * 128],
                                                               rhs=h2T[:, f, :], start=(f == 0), stop=(f == 7)),
                         ["wq", "h2T"], [("mb", cc % 2)])
                if cc % 2 == 0:
                    P.op("act", lambda e, cc=cc: e.copy(out=qT[:, cc, :], in_=mb[cc % 2][:]), [("mb", cc % 2)], [("qT", cc)])
                else:
                    P.op("dve", lambda e, cc=cc: e.tensor_copy(out=qT[:, cc, :], in_=mb[cc % 2][:]), [("mb", cc % 2)], [("qT", cc)])
            for h in range(4):
                for m in range(2):
                    bk = ("mb", m)
                    for dc in range(2):
                        P.op("pe", lambda e, h=h, m=m, dc=dc: e.matmul(
                            mb[m][:], lhsT=kT[:, 2 * h + dc, m * 128:(m + 1) * 128], rhs=qT[:, 2 * h + dc, :],
                            start=(dc == 0), stop=(dc == 1)), ["kT", ("qT", 2 * h + dc)], [bk])
                    P.op("act", lambda e, h=h, m=m: e.activation(out=PT[:, 2 * h + m, :], in_=mb[m][:], func=AF.Exp,
                                                                 scale=1.0 / 16.0), [bk], [("PT", 2 * h + m)])
                for m in range(2):
                    P.op("pe", lambda e, h=h, m=m: e.matmul(acc[0][:, 0:512], lhsT=ones_bf[:], rhs=PT[:, 2 * h + m, :],
                                                            start=(m == 0), stop=(m == 1)),
                         ["ones_bf", ("PT", 2 * h + m)], [("acc", 0)])
                P.op("dve", lambda e: e.reciprocal(out=rec[:], in_=acc[0][:, 0:512]), [("acc", 0)], ["rec"])
                for dc in range(2):
                    for m in range(2):
                        P.op("pe", lambda e, h=h, m=m, dc=dc: e.matmul(
                            acc[1][:, dc * 512:(dc + 1) * 512], lhsT=vm[:, m, (2 * h + dc) * 128:(2 * h + dc + 1) * 128],
                            rhs=PT[:, 2 * h + m, :], start=(m == 0), stop=(m == 1)),
                            ["vm", ("PT", 2 * h + m)], [("acc", 1)])
                for dc in range(2):
                    P.op("dve", lambda e, h=h, dc=dc: e.tensor_tensor(out=oT[:, 2 * h + dc, :],
                                                                      in0=acc[1][:, dc * 512:(dc + 1) * 512], in1=rec[:],
                                                                      op=ALU.mult), [("acc", 1), "rec"], [("oT", 2 * h + dc)])
            for sub in range(4):
                t0 = st * 512 + sub * 128
                xk = ("x", sub)
                ak = ("acc", sub % 2)
                a = acc[sub % 2]
                for half in range(2):
                    for c in range(8):
                        P.op("pe", lambda e, a=a, half=half, c=c, sub=sub: e.matmul(
                            a[:, half * 512:(half + 1) * 512], lhsT=oT[:, c, sub * 128:(sub + 1) * 128],
                            rhs=wc[:, c, half * 512:(half + 1) * 512], start=(c == 0), stop=(c == 7)),
                            [("oT", c), "wc"], [ak])
                r, rk = rstd_of(P, C, a[:], ak, "y", C.tmp["junk"])
                P.op("dve", lambda e, a=a, r=r: e.scalar_tensor_tensor(out=tmp[:], in0=a[:], scalar=r, in1=gqc[:],
                                                                       op0=ALU.mult, op1=ALU.mult), [ak, rk, "gqc"], ["tmp"])
                P.op("pool", lambda e, sub=sub: e.tensor_tensor(out=xs[sub][:], in0=xs[sub][:], in1=tmp[:], op=ALU.add),
                     [xk, "tmp"], [xk])
                P.dma("sp", x2_ap[t0:t0 + 128, :], xs[sub][:], [xk], ["x2_out"], xk)
        P.barrier()
        return P.emit()


def emit_B2(nc, P, x2_ap, w_gate, w_up, w_down, g_pre_ffn, g_post_ffn, g_next, x3_ap, hT_ap):
    ST = 512
    with ExitStack() as es:
        C = Ctx(nc, es, P)
        C.tmp = {"ss": C.sb("ss", [128, 4]), "junk": C.sb("junk", [128, D], BF16)}
        ident = C.sb("ident", [128, 128], BF16)
        make_ident(P, ident)
        stg = {"i": 0, "t": [C.sb("stg%d" % i, [128, 1024]) for i in range(2)]}
        gpf = load_bcast_rows(P, C, "gpf", g_pre_ffn, D)
        gqf = load_bcast_rows(P, C, "gqf", g_post_ffn, D)
        gnx = load_bcast_rows(P, C, "gnx", g_next, D) if g_next is not None else None
        wg = C.sb("wg", [128, 8, DFF], BF16)
        wu = C.sb("wu", [128, 8, DFF], BF16)
        wd = C.sb("wd", [128, NFF, D], BF16)
        load_weight_bf16(P, C, w_gate, D, DFF, wg, "wg", stg)
        load_weight_bf16(P, C, w_up, D, DFF, wu, "wu", stg)
        load_weight_bf16(P, C, w_down, DFF, D, wd, "wd", stg)
        hbf = C.sb("hbf", [128, D], BF16)
        acc = [C.ps("acc%d" % i, [128, D]) for i in range(2)]
        trp = [C.ps("tr%d" % i, [128, D], BF16) for i in range(2)]
        mb = [C.ps("mb%d" % i, [128, 512]) for i in range(2)]
        xs = [C.sb("x%d" % i, [128, D]) for i in range(2)]
        h3T = C.sb("h3T", [128, 8, ST], BF16)
        aT = C.sb("aT", [128, NFF, ST], BF16)
        sg = C.sb("sg", [128, ST])
        tmp = C.sb("tmp", [128, D])
        nsub = ST // 128
        hTv = hT_ap.rearrange("(c p) t -> p c t", p=128) if hT_ap is not None else None
        xi = 0
        for st in range(TOK // ST):
            for sub in range(nsub):
                t0 = st * ST + sub * 128
                xk = ("x", xi % 2)
                xt = xs[xi % 2]
                P.dma("sp", xt[:], x2_ap[t0:t0 + 128, :], [], [xk], xk)
                norm_T(P, C, xt[:], xk, gpf, "gpf", hbf, ident, trp[xi % 2], ("tr", xi % 2), h3T, "h3T", sub * 128)
                xi += 1
            for fc in range(NFF):
                for f in range(8):
                    P.op("pe", lambda e, fc=fc, f=f: e.matmul(mb[0][:, 0:ST], lhsT=wg[:, f, fc * 128:(fc + 1) * 128],
                                                               rhs=h3T[:, f, :], start=(f == 0), stop=(f == 7)),
                         ["wg", "h3T"], [("mb", 0)])
                for f in range(8):
                    P.op("pe", lambda e, fc=fc, f=f: e.matmul(mb[1][:, 0:ST], lhsT=wu[:, f, fc * 128:(fc + 1) * 128],
                                                               rhs=h3T[:, f, :], start=(f == 0), stop=(f == 7)),
                         ["wu", "h3T"], [("mb", 1)])
                P.op("act", lambda e: e.activation(out=sg[:], in_=mb[0][:, 0:ST], func=AF.Silu), [("mb", 0)], ["sg"])
                P.op("dve", lambda e, fc=fc: e.tensor_tensor(out=aT[:, fc, :], in0=mb[1][:, 0:ST], in1=sg[:], op=ALU.mult),
                     [("mb", 1), "sg"], [("aT", fc)])
            for sub in range(nsub):
                t0 = st * ST + sub * 128
                xk = ("x", xi % 2)
                xt = xs[xi % 2]
                ak = ("acc", sub % 2)
                a = acc[sub % 2]
                P.dma("sp", xt[:], x2_ap[t0:t0 + 128, :], [], [xk], xk)
                for half in range(2):
                    for fc in range(NFF):
                        P.op("pe", lambda e, a=a, half=half, fc=fc, sub=sub: e.matmul(
                            a[:, half * 512:(half + 1) * 512], lhsT=aT[:, fc, sub * 128:(sub + 1) * 128],
                            rhs=wd[:, fc, half * 512:(half + 1) * 512], start=(fc == 0), stop=(fc == NFF - 1)),
                            [("aT", fc), "wd"], [ak])
                r, rk = rstd_of(P, C, a[:], ak, "y", C.tmp["junk"])
                P.op("dve", lambda e, a=a, r=r: e.scalar_tensor_tensor(out=tmp[:], in0=a[:], scalar=r, in1=gqf[:],
                                                                       op0=ALU.mult, op1=ALU.mult), [ak, rk, "gqf"], ["tmp"])
                P.op("pool", lambda e, xt=xt: e.tensor_tensor(out=xt[:], in0=xt[:], in1=tmp[:], op=ALU.add),
                     [xk, "tmp"], [xk])
                P.dma("sp", x3_ap[t0:t0 + 128, :], xt[:], [xk], ["x3_out"], xk)
                if gnx is not None:
                    norm_T(P, C, xt[:], xk, gnx, "gnx", hbf, ident, trp[xi % 2], ("tr", xi % 2), h3T, "h3T", sub * 128)
                xi += 1
            if gnx is not None:
                P.dma("sp", hTv[:, :, st * ST:(st + 1) * ST], h3T[:], ["h3T"], ["hT_out"], "h3T")
        P.barrier()
        return P.emit()


NCH = SEQ // 512


def hT_chunk(hT_ap, ci):
    return hT_ap[:, ci // 8, :, (ci % 8) * 512:(ci % 8 + 1) * 512].rearrange("c p t -> p c t")


def emit_A_lru(nc, P, hT_ap, wa, wg2, pa, mix_ap):
    with ExitStack() as es:
        C = Ctx(nc, es, P)
        stg = {"i": 0, "t": [C.sb("stg%d" % i, [128, 1024]) for i in range(2)]}
        pat = C.sb("pat", [128, 16])
        P.dma("sp", pat[:], pa, [], ["pat"], "pat")
        wl = C.sb("wl", [128, 8, 128], BF16)
        load_weight_bf16(P, C, wa[:, 0:128], D, 128, wl, "wl", stg)
        wgs = C.sb("wgs", [64, 128])
        wgb = C.sb("wgb", [64, 128], BF16)
        P.dma("sp", wgs[:], wg2, [], ["wgs"], "wgs")
        P.op("pool", lambda e: e.tensor_copy(out=wgb[:], in_=wgs[:]), ["wgs"], ["wgb"])
        kap = C.sb("kap", [64, 4])
        P.op("act", lambda e: e.activation(out=kap[:, 0:1], in_=pat[0:64, 7:8], func=AF.Exp, scale=-1.0), ["pat"], ["kap0"])
        P.op("act", lambda e: e.activation(out=kap[:, 1:2], in_=kap[:, 0:1], func=AF.Ln, bias=1.0), ["kap0"], ["kap1"])
        P.op("dve", lambda e: e.tensor_scalar(out=kap[:, 2:3], in0=kap[:, 1:2], scalar1=-8.0, scalar2=None, op0=ALU.mult), ["kap1"], ["kap2"])
        P.op("dve", lambda e: e.tensor_scalar(out=kap[:, 3:4], in0=kap[:, 1:2], scalar1=-16.0, scalar2=None, op0=ALU.mult), ["kap1"], ["kap3"])
        hc = [C.sb("hc%d" % i, [128, 8, 512], BF16) for i in range(2)]
        pm = [C.ps("pm%d" % i, [128, 512]) for i in range(2)]
        pg = [C.ps("pg%d" % i, [128, 512]) for i in range(2)]
        lxb = C.sb("lxb", [64, 515])
        P.op("pool", lambda e: e.memset(lxb[:, 0:3], 0.0), [], ["lxb"])
        f32t = {n: C.sb(n, [64, 512]) for n in ["xc", "sr", "si", "a", "a2", "sq", "ix", "u", "gl"]}
        xcb = C.sb("xcb", [64, 512], BF16)
        hb = [C.sb("hb%d" % i, [64, 512]) for i in range(2)]
        ob = [C.sb("ob%d" % i, [64, 512], BF16) for i in range(2)]
        T = f32t
        for ci in range(NCH):
            par = ci % 2
            hk = ("hc", par)
            P.dma("sp", hc[par][:], hT_chunk(hT_ap, ci), [], [hk], hk)
            for g in range(2):
                for f in range(8):
                    P.op("pe", lambda e, g=g, f=f, par=par: e.matmul(pm[g][0:64, :], lhsT=wl[:, f, g * 64:(g + 1) * 64],
                                                                      rhs=hc[par][:, f, :], start=(f == 0), stop=(f == 7)),
                         ["wl", hk], [("pm", g)])
            P.op("act", lambda e: e.copy(out=lxb[:, 3:515], in_=pm[0][0:64, :]), [("pm", 0)], ["lxb"])
            P.op("dve", lambda e: e.tensor_scalar(out=T["xc"][:], in0=lxb[:, 3:515], scalar1=pat[0:64, 3:4], scalar2=pat[0:64, 4:5],
                                                  op0=ALU.mult, op1=ALU.add), ["lxb", "pat"], ["xc"])
            for k in (2, 1, 0):
                P.op("dve", lambda e, k=k: e.scalar_tensor_tensor(out=T["xc"][:], in0=lxb[:, k:k + 512], scalar=pat[0:64, k:k + 1],
                                                                  in1=T["xc"][:], op0=ALU.mult, op1=ALU.add), ["lxb", "pat", "xc"], ["xc"])
            P.op("dve", lambda e: e.tensor_copy(out=lxb[:, 0:3], in_=lxb[:, 512:515]), ["lxb"], ["lxb"])
            P.op("pool", lambda e: e.tensor_copy(out=xcb[:], in_=T["xc"][:]), ["xc"], ["xcb"])
            for g in range(2):
                P.op("pe", lambda e, g=g: e.matmul(pg[g][0:64, :], lhsT=wgb[:, g * 64:(g + 1) * 64], rhs=xcb[:], start=True, stop=True),
                     ["wgb", "xcb"], [("pg", g)])
            P.op("act", lambda e: e.activation(out=T["sr"][:], in_=pg[0][0:64, :], func=AF.Sigmoid, bias=pat[0:64, 5:6]), [("pg", 0), "pat"], ["sr"])
            P.op("act", lambda e: e.activation(out=T["si"][:], in_=pg[1][0:64, :], func=AF.Sigmoid, bias=pat[0:64, 6:7]), [("pg", 1), "pat"], ["si"])
            P.op("act", lambda e: e.activation(out=T["a"][:], in_=T["sr"][:], func=AF.Exp, scale=kap[:, 2:3]), ["sr", "kap2"], ["a"])
            P.op("act", lambda e: e.activation(out=T["a2"][:], in_=T["sr"][:], func=AF.Exp, scale=kap[:, 3:4]), ["sr", "kap3"], ["a2"])
            P.op("dve", lambda e: e.tensor_scalar(out=T["a2"][:], in0=T["a2"][:], scalar1=-1.0, scalar2=1.0, op0=ALU.mult, op1=ALU.add), ["a2"], ["a2"])
            P.op("act", lambda e: e.activation(out=T["sq"][:], in_=T["a2"][:], func=AF.Sqrt), ["a2"], ["sq"])
            P.op("pool", lambda e: e.tensor_tensor(out=T["ix"][:], in0=T["si"][:], in1=T["xc"][:], op=ALU.mult), ["si", "xc"], ["ix"])
            P.op("dve", lambda e: e.tensor_tensor(out=T["u"][:], in0=T["sq"][:], in1=T["ix"][:], op=ALU.mult), ["sq", "ix"], ["u"])
            init = 0.0 if ci == 0 else hb[1 - par][:, 511:512]
            P.op("dve", lambda e, init=init, par=par: e.tensor_tensor_scan(out=hb[par][:], data0=T["a"][:], data1=T["u"][:], initial=init,
                                                                           op0=ALU.mult, op1=ALU.add), ["a", "u", ("hb", 1 - par)], [("hb", par)])
            P.op("act", lambda e: e.activation(out=T["gl"][:], in_=pm[1][0:64, :], func=AF.Gelu_apprx_tanh), [("pm", 1)], ["gl"])
            P.op("dve", lambda e, par=par: e.tensor_tensor(out=ob[par][:], in0=hb[par][:], in1=T["gl"][:], op=ALU.mult), [("hb", par), "gl"], [("ob", par)])
            ok = ("ob", par)
            P.dma("sp", mix_ap[ci // 8, 0:64, (ci % 8) * 512:(ci % 8 + 1) * 512], ob[par][:], [ok], ["mix_out"], ok)
        P.barrier()
        return P.emit()


C1_2PI = 6.28125
C2_2PI = float(2.0 * np.pi - 6.28125)
PI_SAFE = 3.1415925


def emit_A_ret(nc, P, hT_ap, wa, pa, cst, pos_ap, mix_ap):
    import os
    STAGE = float(os.environ.get("RET_STAGE", "9"))
    with ExitStack() as es:
        C = Ctx(nc, es, P)
        stg = {"i": 0, "t": [C.sb("stg%d" % i, [128, 1024]) for i in range(2)]}
        pat = C.sb("pat", [128, 16])
        P.dma("sp", pat[:], pa, [], ["pat"], "pat")
        cs = C.sb("cs", [128, 416])
        P.dma("sp", cs[:], cst, [], ["cs"], "cs")
        decT = cs[:, 0:128]
        qwbc = cs[0:64, 128:256]
        invf = cs[:, 256:288]
        wr = C.sb("wr", [128, 8, 256], BF16)
        load_weight_bf16(P, C, wa[:, 514:770], D, 256, wr, "wr", stg)
        identb = C.sb("identb", [128, 128], BF16)
        make_ident(P, identb, "identb")
        identf = C.sb("identf", [128, 128])
        make_ident(P, identf, "identf")
        Tps = C.ps("T", [128, 4, 256])
        trq_ = C.ps("trq", [128, 1024], BF16)
        trq = trq_[0:64, 0:256]
        scp_ = C.ps("scp", [128, 512])
        scp = scp_[:, 0:128]
        Yp_ = C.ps("Yp", [128, 512])
        Yp = Yp_[:, 0:256]
        Up_ = C.ps("Up", [128, 512])
        Up = Up_[0:64, 0:64]
        tro_ = C.ps("tro", [128, 1024], BF16)
        tro = tro_[0:64, 0:512]
        posi = C.sb("posi", [128, 128], I32)
        posf = C.sb("posf", [128, 128])
        post = C.sb("post", [128, 128])
        P.dma("sp", posi[:], pos_ap.rearrange("(n p) -> n p", p=128), [], ["posi"], "posi")
        P.op("dve", lambda e: e.tensor_copy(out=posf[:], in_=posi[:]), ["posi"], ["posf"])
        P.op("pe", lambda e: e.matmul(scp, lhsT=posf[:], rhs=identf[:], start=True, stop=True), ["posf", "identf"], ["scp"])
        P.op("act", lambda e: e.copy(out=post[:], in_=scp), ["scp"], ["post"])
        Sf = C.sb("Sf", [64, 64])
        Sb = C.sb("Sb", [64, 64], BF16)
        P.op("pool", lambda e: e.memset(Sf[:], 0.0), [], ["Sf"])
        P.op("pool", lambda e: e.memset(Sb[:], 0.0), [], ["Sb"])
        hc = [C.sb("hc%d" % i, [128, 8, 512], BF16) for i in range(2)]
        ang = C.sb("ang", [128, 4, 32])
        yy = C.sb("yy", [128, 4, 32])
        yi = C.sb("yi", [128, 4, 32], I32)
        rr = C.sb("rr", [128, 4, 32])
        ar = C.sb("ar", [128, 4, 32])
        sin2 = C.sb("sin2", [128, 4, 2, 32])
        cos2 = C.sb("cos2", [128, 4, 2, 32])
        t1 = C.sb("t1", [128, 2, 32])
        t2 = C.sb("t2", [128, 2, 32])
        t3 = C.sb("t3", [128, 2, 32])
        t4 = C.sb("t4", [128, 2, 32])
        rot = C.sb("rot", [128, 2, 2, 32])
        rot2 = C.sb("rot2", [128, 128])
        qkf = C.sb("qkf", [128, 128])
        qkb = C.sb("qkb", [128, 128], BF16)
        kwb = C.sb("kwb", [128, 64], BF16)
        vb = C.sb("vb", [128, 64], BF16)
        qkT = C.sb("qkT", [64, 256], BF16)
        qwT = C.sb("qwT", [64, 128], BF16)
        sm = C.sb("sm", [128, 128], BF16)
        scf = C.sb("scf", [128, 128])
        qf32 = C.sb("qf32", [64, 128])
        sgl = C.sb("sgl", [128, 4, 64])
        st6 = C.sb("st6", [128, 4, 6])
        mv = C.sb("mv", [128, 4, 2])
        ve = C.sb("ve", [128, 4])
        yn = C.sb("yn", [128, 4, 64])
        obf = C.sb("obf", [128, 4, 64], BF16)
        oT = [C.sb("oT%d" % i, [64, 512], BF16) for i in range(2)]
        for ci in range(NCH):
            par = ci % 2
            hk = ("hc", par)
            P.dma("sp", hc[par][:], hT_chunk(hT_ap, ci), [], [hk], hk)
            for sub in range(4):
                for f in range(8):
                    P.op("pe", lambda e, sub=sub, f=f, par=par: e.matmul(Tps[:, sub, :], lhsT=hc[par][:, f, sub * 128:(sub + 1) * 128],
                                                                          rhs=wr[:, f, :], start=(f == 0), stop=(f == 7)),
                         ["wr", hk], [("T", sub // 2)])
                n = 4 * ci + sub
                P.op("dve", lambda e, sub=sub, n=n: e.tensor_scalar(out=ang[:, sub, :], in0=invf, scalar1=post[:, n:n + 1], scalar2=None,
                                                                    op0=ALU.mult), ["cs", "post"], ["ang"])
            if STAGE < 2:
                continue
            P.op("dve", lambda e: e.tensor_scalar(out=yy[:], in0=ang[:], scalar1=float(1.0 / (2.0 * np.pi)), scalar2=None, op0=ALU.mult), ["ang"], ["yy"])
            P.op("dve", lambda e: e.tensor_copy(out=yi[:], in_=yy[:]), ["yy"], ["yi"])
            P.op("dve", lambda e: e.tensor_copy(out=yy[:], in_=yi[:]), ["yi"], ["yy"])
            P.op("dve", lambda e: e.scalar_tensor_tensor(out=rr[:], in0=yy[:], scalar=-C1_2PI, in1=ang[:], op0=ALU.mult, op1=ALU.add), ["yy", "ang"], ["rr"])
            P.op("dve", lambda e: e.scalar_tensor_tensor(out=rr[:], in0=yy[:], scalar=-C2_2PI, in1=rr[:], op0=ALU.mult, op1=ALU.add), ["yy", "rr"], ["rr"])
            P.op("dve", lambda e: e.tensor_scalar(out=rr[:], in0=rr[:], scalar1=PI_SAFE, scalar2=-PI_SAFE, op0=ALU.min, op1=ALU.max), ["rr"], ["rr"])
            P.op("dve", lambda e: e.scalar_tensor_tensor(out=ar[:], in0=rr[:], scalar=-1.0, in1=rr[:], op0=ALU.mult, op1=ALU.max), ["rr"], ["ar"])
            P.op("dve", lambda e: e.tensor_scalar(out=ar[:], in0=ar[:], scalar1=-1.0, scalar2=float(np.pi / 2), op0=ALU.mult, op1=ALU.add), ["ar"], ["ar"])
            for k in range(2):
                P.op("act", lambda e, k=k: e.activation(out=sin2[:, :, k, :], in_=rr[:], func=AF.Sin), ["rr"], ["sin2"])
                P.op("act", lambda e, k=k: e.activation(out=cos2[:, :, k, :], in_=ar[:], func=AF.Sin), ["ar"], ["cos2"])
            if STAGE < 3:
                continue
            for sub in range(4):
                tk = ("T", sub // 2)
                P.op("act", lambda e, sub=sub: e.copy(out=qkf[:], in_=Tps[:, sub, 0:128]), [tk], ["qkf"])
                cs_ = cos2[:, sub, 0, :]
                sn_ = sin2[:, sub, 0, :]
                if STAGE < 3.05:
                    continue
                for a_ in range(2):
                    x1 = qkf[:, a_ * 64:a_ * 64 + 32]
                    x2 = qkf[:, a_ * 64 + 32:a_ * 64 + 64]
                    o1 = rot2[:, a_ * 64:a_ * 64 + 32]
                    o2 = rot2[:, a_ * 64 + 32:a_ * 64 + 64]
                    P.op("pool", lambda e, x1=x1, cs_=cs_: e.tensor_tensor(out=t1[:, 0, :], in0=x1, in1=cs_, op=ALU.mult), ["qkf", "cos2"], ["t1"])
                    P.op("pool", lambda e, x2=x2, sn_=sn_: e.tensor_tensor(out=t2[:, 0, :], in0=x2, in1=sn_, op=ALU.mult), ["qkf", "sin2"], ["t2"])
                    P.op("pool", lambda e, x1=x1, sn_=sn_: e.tensor_tensor(out=t3[:, 0, :], in0=x1, in1=sn_, op=ALU.mult), ["qkf", "sin2"], ["t3"])
                    P.op("pool", lambda e, x2=x2, cs_=cs_: e.tensor_tensor(out=t4[:, 0, :], in0=x2, in1=cs_, op=ALU.mult), ["qkf", "cos2"], ["t4"])
                    P.op("pool", lambda e, o1=o1: e.tensor_tensor(out=o1, in0=t1[:, 0, :], in1=t2[:, 0, :], op=ALU.subtract), ["t1", "t2"], ["rot0"])
                    P.op("pool", lambda e, o2=o2: e.tensor_tensor(out=o2, in0=t3[:, 0, :], in1=t4[:, 0, :], op=ALU.add), ["t3", "t4"], ["rot1"])
                if STAGE < 3.25:
                    continue
                rotf = rot2[:]
                P.op("pool", lambda e, rotf=rotf: e.tensor_copy(out=qkb[:], in_=rotf), ["rot0", "rot1"], ["qkb"])
                P.op("dve", lambda e, rotf=rotf: e.tensor_scalar(out=kwb[:], in0=rotf[:, 64:128], scalar1=pat[:, 11:12], scalar2=None, op0=ALU.mult),
                     ["rot0", "rot1", "pat"], ["kwb"])
                P.op("act", lambda e, sub=sub: e.copy(out=vb[:], in_=Tps[:, sub, 128:192]), [tk], ["vb"])
                if STAGE < 4:
                    continue
                P.op("pe", lambda e: e.transpose(trq[:, 0:128], qkb[:, 0:64], identb[:]), ["qkb", "identb"], ["trq"])
                P.op("pe", lambda e: e.transpose(trq[:, 128:256], qkb[:, 64:128], identb[:]), ["qkb", "identb"], ["trq"])
                P.op("act", lambda e: e.copy(out=qkT[:], in_=trq), ["trq"], ["qkT"])
                P.op("act", lambda e: e.copy(out=qf32[:], in_=trq[:, 0:128]), ["trq"], ["qf32"])
                P.op("pool", lambda e: e.tensor_tensor(out=qwT[:], in0=qf32[:], in1=qwbc, op=ALU.mult), ["qf32", "cs"], ["qwT"])
                P.op("pe", lambda e: e.matmul(scp, lhsT=qkT[:, 128:256], rhs=qkT[:, 0:128], start=True, stop=True), ["qkT"], ["scp"])
                P.op("act", lambda e: e.copy(out=scf[:], in_=scp), ["scp"], ["scf"])
                P.op("pool", lambda e: e.tensor_tensor(out=sm[:], in0=scf[:], in1=decT, op=ALU.mult), ["scf", "cs"], ["sm"])
                yk = "Y"
                P.op("pe", lambda e, sub=sub: e.matmul(Yp[:, sub * 64:(sub + 1) * 64], lhsT=sm[:], rhs=vb[:], start=True, stop=False), ["sm", "vb"], [yk])
                P.op("pe", lambda e, sub=sub: e.matmul(Yp[:, sub * 64:(sub + 1) * 64], lhsT=qwT[:], rhs=Sb[:], start=False, stop=True), ["qwT", "Sb"], [yk])
                P.op("pe", lambda e: e.matmul(Up, lhsT=kwb[:], rhs=vb[:], start=True, stop=True), ["kwb", "vb"], ["Up"])
                P.op("dve", lambda e: e.scalar_tensor_tensor(out=Sf[:], in0=Sf[:], scalar=pat[0:64, 8:9], in1=Up, op0=ALU.mult, op1=ALU.add),
                     ["Sf", "Up", "pat"], ["Sf"])
                P.op("act", lambda e: e.copy(out=Sb[:], in_=Sf[:]), ["Sf"], ["Sb"])
            if STAGE < 5:
                continue
            for hb_ in range(2):
                P.op("act", lambda e, hb_=hb_: e.activation(out=sgl[:, 2 * hb_:2 * hb_ + 2, :], in_=Tps[:, 2 * hb_:2 * hb_ + 2, 192:256], func=AF.Silu),
                     [("T", hb_)], [("sgl", hb_)])
            for sub in range(4):
                P.op("dve", lambda e, sub=sub: e.bn_stats(out=st6[:, sub, :], in_=Yp[:, sub * 64:(sub + 1) * 64]), ["Y"], [("st6", sub)])
                P.op("dve", lambda e, sub=sub: e.bn_aggr(out=mv[:, sub, :], in_=st6[:, sub, :]), [("st6", sub)], [("mv", sub)])
            mvk = [("mv", s_) for s_ in range(4)]
            P.op("dve", lambda e: e.tensor_scalar(out=ve[:], in0=mv[:, :, 1], scalar1=EPS, scalar2=None, op0=ALU.add), mvk, ["ve"])
            P.op("act", lambda e: e.activation(out=ve[:], in_=ve[:], func=AF.Sqrt), ["ve"], ["ve"])
            P.op("dve", lambda e: e.reciprocal(out=ve[:], in_=ve[:]), ["ve"], ["ve"])
            for sub in range(4):
                P.op("dve", lambda e, sub=sub: e.tensor_scalar(out=yn[:, sub, :], in0=Yp[:, sub * 64:(sub + 1) * 64], scalar1=mv[:, sub, 0:1],
                                                               scalar2=ve[:, sub:sub + 1], op0=ALU.subtract, op1=ALU.mult),
                     ["Y", ("mv", sub), "ve"], [("yn", sub)])
            P.op("pool", lambda e: e.tensor_tensor(out=obf[:], in0=yn[:], in1=sgl[:], op=ALU.mult), [("yn", s_) for s_ in range(4)] + [("sgl", 0), ("sgl", 1)], ["obf"])
            for sub in range(4):
                P.op("pe", lambda e, sub=sub: e.transpose(tro[:, sub * 128:(sub + 1) * 128], obf[:, sub, :], identb[:]), ["obf", "identb"], ["tro"])
            ok = ("oT", par)
            P.op("act", lambda e, par=par: e.copy(out=oT[par][:], in_=tro), ["tro"], [ok])
            P.dma("sp", mix_ap[ci // 8, 192:256, (ci % 8) * 512:(ci % 8 + 1) * 512], oT[par][:], [ok], ["mix_out"], ok)
        P.barrier()
        return P.emit()


def emit_A_fox(nc, P, hT_ap, wa, pa, cst, mix_ap, nch=NCH):
    with ExitStack() as es:
        C = Ctx(nc, es, P)
        stg = {"i": 0, "t": [C.sb("stg%d" % i, [128, 1024]) for i in range(2)]}
        pat = C.sb("pat", [128, 16])
        P.dma("sp", pat[:], pa, [], ["pat"], "pat")
        cs = C.sb("cs", [128, 128])
        P.dma("sp", cs[:], cst[:, 288:416], [], ["cs"], "cs")
        maskb = C.sb("maskb", [128, 128], BF16)
        P.op("pool", lambda e: e.tensor_copy(out=maskb[:], in_=cs[:]), ["cs"], ["maskb"])
        identb = C.sb("identb", [128, 128], BF16)
        make_ident(P, identb, "identb")
        onesf = C.sb("onesf", [128, 512])
        P.op("pool", lambda e: e.memset(onesf[:], 1.0), [], ["onesf"])
        nb = C.sb("nb", [128, 2])
        P.op("dve", lambda e: e.tensor_scalar(out=nb[:], in0=pat[:, 9:11], scalar1=-1.0, scalar2=None, op0=ALU.mult), ["pat"], ["nb"])
        wq = C.sb("wq", [128, 8, 130], BF16)
        wk = C.sb("wk", [128, 8, 128], BF16)
        wv = C.sb("wv", [128, 8, 128], BF16)
        load_weight_bf16(P, C, wa[:, 128:258], D, 130, wq, "wq", stg)
        load_weight_bf16(P, C, wa[:, 258:386], D, 128, wk, "wk", stg)
        load_weight_bf16(P, C, wa[:, 386:514], D, 128, wv, "wv", stg)
        KT = [C.sb("KT%d" % h, [65, SEQ], BF16) for h in range(2)]
        for h in range(2):
            P.op("pool", lambda e, h=h: e.memset(KT[h][64:65, :], 1.0), [], [("KT", h)])
        Vs = C.sb("Vs", [128, 128, 2, 65], BF16)
        P.op("pool", lambda e: e.memset(Vs[:], 1.0), [], ["Vs"])
        negc = C.sb("negc", [128, 128, 2])
        QT = [[C.sb("QT%d%d" % (h, p), [65, 512], BF16) for p in range(2)] for h in range(2)]
        crow = [[C.sb("crow%d%d" % (h, p), [65, 512]) for p in range(2)] for h in range(2)]
        e1 = C.sb("e1", [65, 512])
        Osb = C.sb("Osb", [65, 512])
        rcp = C.sb("rcp", [65, 512])
        ofb = [C.sb("ofb%d" % i, [64, 512], BF16) for i in range(2)]
        PTt = [C.sb("PT%d" % i, [128, 512], BF16) for i in range(3)]
        hc = [C.sb("hc%d" % i, [128, 8, 512], BF16) for i in range(2)]
        sbank = [C.ps("sbk%d" % i, [128, 512]) for i in range(3)]
        Ob = [C.ps("Ob%d" % i, [128, 512]) for i in range(2)]
        pj = [C.ps("pj%d" % i, [128, 512]) for i in range(2)]
        pmz = C.ps("pmz", [128, 512])
        pji = [0]

        def nextpj():
            i = pji[0] % 2
            pji[0] += 1
            return pj[i], ("pj", i)

        oi = 0
        for ci in range(nch):
            par = ci % 2
            hk = ("hc", par)
            P.dma("sp", hc[par][:], hT_chunk(hT_ap, ci), [], [hk], hk)
            for h in range(2):
                qk_ = ("QT", h, par)
                ck_ = ("crow", h, par)
                pq, pqk = nextpj()
                for f in range(8):
                    P.op("pe", lambda e, pq=pq, h=h, f=f, par=par: e.matmul(pq[0:65, :], lhsT=wq[:, f, h * 65:(h + 1) * 65], rhs=hc[par][:, f, :],
                                                                           start=(f == 0), stop=(f == 7)), ["wq", hk], [pqk])
                P.op("act", lambda e, pq=pq, h=h, par=par: e.mul(out=QT[h][par][0:64, :], in_=pq[0:64, :], mul=0.125), [pqk], [qk_])
                P.op("act", lambda e, pq=pq, h=h: e.activation(out=e1[64:65, :], in_=pq[64:65, :], func=AF.Exp, scale=-1.0, bias=nb[64:65, h:h + 1]),
                     [pqk, "nb"], ["e1"])
                P.op("act", lambda e: e.activation(out=e1[64:65, :], in_=e1[64:65, :], func=AF.Ln, bias=1.0), ["e1"], ["e1"])
                init = 0.0 if ci == 0 else crow[h][1 - par][64:65, 511:512]
                P.op("dve", lambda e, h=h, par=par, init=init: e.tensor_tensor_scan(out=crow[h][par][64:65, :], data0=onesf[64:65, :], data1=e1[64:65, :],
                                                                                    initial=init, op0=ALU.mult, op1=ALU.subtract),
                     ["e1", "onesf", ("crow", h, 1 - par)], [ck_])
                P.op("dve", lambda e, h=h, par=par: e.tensor_copy(out=QT[h][par][64:65, :], in_=crow[h][par][64:65, :]), [ck_], [qk_])
                pc, pck = nextpj()
                for sub in range(4):
                    P.op("pe", lambda e, pc=pc, h=h, par=par, sub=sub: e.matmul(pc[:, sub:sub + 1], lhsT=crow[h][par][64:65, sub * 128:(sub + 1) * 128],
                                                                               rhs=onesf[64:65, 0:1], start=True, stop=True), [ck_, "onesf"], [pck])
                P.op("dve", lambda e, pc=pc, h=h, ci=ci: e.tensor_scalar(out=negc[:, 4 * ci:4 * ci + 4, h], in0=pc[:, 0:4], scalar1=-1.0, scalar2=None,
                                                                        op0=ALU.mult), [pck], [("negc", h, ci)])
                pk_, pkk = nextpj()
                for f in range(8):
                    P.op("pe", lambda e, pk_=pk_, h=h, f=f, par=par: e.matmul(pk_[0:64, :], lhsT=wk[:, f, h * 64:(h + 1) * 64], rhs=hc[par][:, f, :],
                                                                             start=(f == 0), stop=(f == 7)), ["wk", hk], [pkk])
                P.op("act", lambda e, pk_=pk_, h=h, ci=ci: e.copy(out=KT[h][0:64, ci * 512:(ci + 1) * 512], in_=pk_[0:64, :]), [pkk], [("KT", h, ci)])
            for sub in range(4):
                pv, pvk = nextpj()
                for f in range(8):
                    P.op("pe", lambda e, pv=pv, sub=sub, f=f, par=par: e.matmul(pv[:, 0:128], lhsT=hc[par][:, f, sub * 128:(sub + 1) * 128], rhs=wv[:, f, :],
                                                                               start=(f == 0), stop=(f == 7)), ["wv", hk], [pvk])
                P.op("dve", lambda e, pv=pv, sub=sub, ci=ci: e.tensor_copy(out=Vs[:, 4 * ci + sub, :, 0:64],
                                                                           in_=pv[:, 0:128].rearrange("p (a d) -> p a d", a=2)), [pvk, "Vs"], [("Vs", ci)])
            for h in range(2):
                qk_ = ("QT", h, par)
                nj = 4 * ci + 4
                Okey = ("Ob", h)

                def S(j, h=h, par=par, ci=ci):
                    r = j - 4 * ci
                    q0 = max(0, r) * 128
                    bk = ("sbk", j % 3)
                    bank = sbank[j % 3]
                    diag = r >= 0
                    P.op("pe", lambda e: e.matmul(bank[:, q0:512], lhsT=KT[h][0:65, j * 128:(j + 1) * 128], rhs=QT[h][par][0:65, q0:512],
                                                  start=True, stop=not diag), [("KT", h), ("KT", h, j // 4), qk_], [bk])
                    if diag:
                        P.op("pe", lambda e: e.matmul(bank[:, q0:q0 + 128], lhsT=identb[:], rhs=maskb[:], start=False, stop=True),
                             ["identb", "maskb"], [bk])
                    P.op("act", lambda e: e.activation(out=PTt[j % 3][:, q0:512], in_=bank[:, q0:512], func=AF.Exp, bias=negc[:, j, h:h + 1]),
                         [bk, ("negc", h, j // 4)], [("PT", j % 3)])

                def PV(j, h=h, ci=ci, nj=nj):
                    r = j - 4 * ci
                    q0 = max(0, r) * 128
                    P.op("pe", lambda e: e.matmul(Ob[h][0:65, q0:512], lhsT=Vs[:, j, h, :], rhs=PTt[j % 3][:, q0:512],
                                                  start=(j == 0), stop=(j == nj - 1)), ["Vs", ("Vs", j // 4), ("PT", j % 3)], [Okey])

                S(0)
                S(1)
                for j in range(nj):
                    if j + 2 < nj:
                        S(j + 2)
                    PV(j)
                P.op("act", lambda e, h=h: e.copy(out=Osb[:], in_=Ob[h][0:65, :]), [Okey], ["Osb"])
                P.op("dve", lambda e: e.reciprocal(out=rcp[64:65, :], in_=Osb[64:65, :]), ["Osb"], ["rcp"])
                P.op("pe", lambda e: e.matmul(pmz[0:64, :], lhsT=onesf[64:65, 0:64], rhs=rcp[64:65, :], start=True, stop=True), ["onesf", "rcp"], ["pmz"])
                ok = ("ofb", oi % 2)
                o_t = ofb[oi % 2]
                oi += 1
                P.op("dve", lambda e, o_t=o_t: e.tensor_tensor(out=o_t[:], in0=Osb[0:64, :], in1=pmz[0:64, :], op=ALU.mult), ["Osb", "pmz"], [ok])
                P.dma("sp", mix_ap[ci // 8, 64 + 64 * h:128 + 64 * h, (ci % 8) * 512:(ci % 8 + 1) * 512], o_t[:], [ok], ["mix_out"], ok)
        P.barrier()
        return P.emit()


def mix_perm():
    perm = []
    for s in range(4):
        perm += list(range(64 * s, 64 * s + 64)) + list(range(256 + 128 * s, 256 + 128 * s + 128)) \
            + list(range(768 + 64 * s, 768 + 64 * s + 64))
    return np.array(perm)


def a_weights(inp, l, s):
    w = inp["w_in"][l]
    A, B = 2 * s, 2 * s + 1
    cols = []
    cols += list(range(OFF[0] + 64 * s, OFF[0] + 64 * s + 64))
    cols += list(range(OFF[1] + 64 * s, OFF[1] + 64 * s + 64))
    cols += list(range(OFF[2] + 64 * A, OFF[2] + 64 * A + 64)) + [OFF[5] + A]
    cols += list(range(OFF[2] + 64 * B, OFF[2] + 64 * B + 64)) + [OFF[5] + B]
    cols += list(range(OFF[3] + 64 * A, OFF[3] + 64 * A + 64))
    cols += list(range(OFF[3] + 64 * B, OFF[3] + 64 * B + 64))
    cols += list(range(OFF[4] + 64 * A, OFF[4] + 64 * A + 64))
    cols += list(range(OFF[4] + 64 * B, OFF[4] + 64 * B + 64))
    for g in (6, 7, 8, 9):
        cols += list(range(OFF[g] + 64 * s, OFF[g] + 64 * s + 64))
    wa = np.ascontiguousarray(w[:, cols])
    wg2 = np.ascontiguousarray(np.concatenate([inp["w_rg"][l, s], inp["w_ig"][l, s]], axis=1))
    log_gamma = np.log1p(-np.exp2(-5.0 - np.arange(4, dtype=np.float32))).astype(np.float32)
    lg = log_gamma[s]
    idx = np.arange(128, dtype=np.float32)
    pa = np.zeros((128, 16), np.float32)
    sl = slice(64 * s, 64 * s + 64)
    for k in range(4):
        pa[0:64, k] = inp["conv_w"][l, k, sl]
    pa[0:64, 4] = inp["conv_b"][l, sl]
    pa[0:64, 5] = inp["b_rg"][l, sl]
    pa[0:64, 6] = inp["b_ig"][l, sl]
    pa[0:64, 7] = inp["lru_lambda"][l, sl]
    pa[:, 8] = np.exp(lg * np.float32(128.0))
    pa[:, 9] = inp["fox_b_f"][l, A]
    pa[:, 10] = inp["fox_b_f"][l, B]
    pa[:, 11] = np.exp(lg * (np.float32(127.0) - idx)) * np.float32(0.125)
    cst = np.zeros((128, 416), np.float32)
    diff = idx[:, None] - idx[None, :]
    decay = np.where(diff >= 0, np.exp(lg * np.maximum(diff, 0.0)), 0.0).astype(np.float32)
    cst[:, 0:128] = decay.T * np.float32(0.125)
    cst[:, 128:256] = np.exp(lg * (idx + 1.0))[None, :]
    half = 32
    cst[:, 256:288] = (np.float32(10000.0) ** (-np.arange(half, dtype=np.float32) / np.float32(half))).astype(np.float32)[None, :]
    kk = np.arange(128)
    cst[:, 288:416] = np.where(kk[:, None] > kk[None, :], -30000.0, 0.0)
    return wa, wg2, pa, cst


def _dt(nc, n, s, t=F32, k="ExternalInput"):
    return nc.dram_tensor(n, s, t, kind=k).ap()


GROUPS = [[0, 1, 2, 3], [4, 5, 6, 7]]


def build_fused(stop=99):
    from concourse.bass import DynSlice
    nc = bass.Bass("TRN2", target_bir_lowering=False)
    x = _dt(nc, "x", [TOK, D])
    mem = _dt(nc, "mem", [256, D])
    gm = _dt(nc, "gm", [D])
    pos = _dt(nc, "pos", [SEQ], I32)
    cst = _dt(nc, "cst", [128, 416])
    g0 = _dt(nc, "g0", [D])
    L = []
    for l in range(DEPTH):
        d = {"wa": _dt(nc, "wa%d" % l, [D, 770]), "wg2": _dt(nc, "wg2%d" % l, [64, 128]), "pa": _dt(nc, "pa%d" % l, [128, 16])}
        for n in ["w_out", "w_cq", "w_ck", "w_cv", "w_co"]:
            d[n] = _dt(nc, "%s%d" % (n, l), [D, D])
        for n in ["g_post_mix", "g_pre_cross", "g_post_cross", "g_pre_ffn", "g_post_ffn", "g_next"]:
            d[n] = _dt(nc, "%s%d" % (n, l), [D])
        d["w_gate"] = _dt(nc, "w_gate%d" % l, [D, DFF])
        d["w_up"] = _dt(nc, "w_up%d" % l, [D, DFF])
        d["w_down"] = _dt(nc, "w_down%d" % l, [DFF, D])
        L.append(d)
    out = _dt(nc, "out", [TOK, D], F32, "ExternalOutput")
    hT_own = nc.dram_tensor("hT_own", [D, TOK], BF16).ap()
    hT_all = nc.dram_tensor("hT_all", [4 * D, TOK], BF16).ap()
    mix_c = nc.dram_tensor("mix_c", [4 * 256, TOK], BF16).ap()
    mixG = nc.dram_tensor("mixG", [16 * 256, TOK], BF16).ap()
    x2s = nc.dram_tensor("x2s", [TOK, D], F32).ap()
    x3s = nc.dram_tensor("x3s", [TOK, D], F32).ap()
    hT3 = hT_all.rearrange("(c r p) t -> c r p t", c=8, r=4, p=128)
    mix3 = mix_c.rearrange("(j f) t -> j f t", j=4)
    mixGv = mixG.rearrange("(r j a p) t -> p r j a t", r=4, j=4, a=2, p=128)

    mixT_own = nc.dram_tensor("mixT_own", [D, TOK], BF16).ap()
    mixG5 = mixG.rearrange("(j a r p) t -> j a r p t", j=4, a=2, r=4, p=128)

    with ExitStack() as es:
        P = Prog(nc, es)

        def gather8(src, dst):
            for q in range(8):
                P.collective("AllGather", src[q * 128:(q + 1) * 128, :], dst[q * 512:(q + 1) * 512, :], GROUPS, ["a"], [("b", q)])

        def gather(src, dst, rk, wk):
            gather8(src, dst)
            P.barrier()
            P.emit()

        emit_P0(nc, P, x, g0, hT_own, TOK)
        if stop <= 0:
            return nc
        gather(hT_own, hT_all, "a", "b")
        if stop <= 1:
            return nc
        for l in range(DEPTH):
            d = L[l]
            last = l == DEPTH - 1
            if stop <= 2 + 10 * l:
                return nc
            emit_A_lru(nc, P, hT3, d["wa"], d["wg2"], d["pa"], mix3)
            emit_A_ret(nc, P, hT3, d["wa"], d["pa"], cst, pos, mix3)
            emit_A_fox(nc, P, hT3, d["wa"], d["pa"], cst, mix3)
            if stop <= 3 + 10 * l:
                return nc
            gather8(mix_c, mixG)
            sv = {}
            for r_ in range(4):
                def sel(e, r_=r_):
                    if "s" not in sv:
                        sv["s"] = e.snap(e.partition_id() % 4)
                    return e.dma_start(out=mixT_own[r_ * 256:(r_ + 1) * 256, :].rearrange("(a p) t -> a p t", a=2),
                                       in_=mixG5[DynSlice(sv["s"], 1), :, r_, :, :].rearrange("j a p t -> (j a) p t"))
                P.op("pool", sel, [("b", q) for q in range(8)], [("mo", r_)], dma=("mo", r_))
            P.barrier()
            P.emit()
            if stop <= 4 + 10 * l:
                return nc
            emit_B1(nc, P, mixT_own, x if l == 0 else x3s, mem, gm, d["w_out"], d["w_cq"], d["w_ck"], d["w_cv"], d["w_co"],
                    d["g_post_mix"], d["g_pre_cross"], d["g_post_cross"], x2s)
            emit_B2(nc, P, x2s, d["w_gate"], d["w_up"], d["w_down"], d["g_pre_ffn"], d["g_post_ffn"],
                    None if last else d["g_next"], out if last else x3s, None if last else hT_own)
            if not last:
                gather(hT_own, hT_all, "a", "b")
    return nc


def kernel(**inp):
    inp = {k: np.asarray(v) for k, v in inp.items()}
    cores = list(range(8))
    perm = mix_perm()
    x = np.ascontiguousarray(inp["x"], dtype=np.float32)
    maps = []
    for c in cores:
        b, s = c // 4, c % 4
        m = {"x": np.ascontiguousarray(x[b, s * TOK:(s + 1) * TOK]), "mem": np.ascontiguousarray(inp["mem"][b]),
             "gm": inp["mem_norm_g"], "pos": np.ascontiguousarray(inp["positions"][b]).astype(np.int32),
             "g0": inp["pre_mix_g"][0]}
        for l in range(DEPTH):
            wa, wg2, pa, cst = a_weights(inp, l, s)
            m["cst"] = cst
            m["wa%d" % l] = wa
            m["wg2%d" % l] = wg2
            m["pa%d" % l] = pa
            m["w_out%d" % l] = np.ascontiguousarray(inp["w_out"][l][perm])
            for n in ["w_cq", "w_ck", "w_cv", "w_co", "w_gate", "w_up", "w_down"]:
                m["%s%d" % (n, l)] = np.ascontiguousarray(inp[n][l])
            m["g_post_mix%d" % l] = inp["post_mix_g"][l]
            m["g_pre_cross%d" % l] = inp["pre_cross_g"][l]
            m["g_post_cross%d" % l] = inp["post_cross_g"][l]
            m["g_pre_ffn%d" % l] = inp["pre_ffn_g"][l]
            m["g_post_ffn%d" % l] = inp["post_ffn_g"][l]
            m["g_next%d" % l] = inp["pre_mix_g"][min(l + 1, DEPTH - 1)]
        maps.append({k: np.ascontiguousarray(v) for k, v in m.items()})
    res = run_bass_kernel_spmd(build_fused(), maps, core_ids=cores)
    out = np.zeros((NB, SEQ, D), np.float32)
    for c in cores:
        out[c // 4, (c % 4) * TOK:(c % 4 + 1) * TOK] = res.results[c]["out"]
    return out
```

```python
import numpy as np
import ml_dtypes
from contextlib import ExitStack
import concourse.bass as bass
import concourse.mybir as mybir
from concourse.bass_utils import run_bass_kernel_spmd

F32 = mybir.dt.float32
BF16 = mybir.dt.bfloat16
I32 = mybir.dt.int32
AF = mybir.ActivationFunctionType
ALU = mybir.AluOpType
AX = mybir.AxisListType

D = 1024
SEQ = 16384
NB = 2
DEPTH = 2
TOK = 4096
DFF = 2816
NFF = DFF // 128
EPS = 1e-6
SPLIT = (256, 256, 512, 512, 512, 8, 256, 256, 256, 256)
OFF = np.concatenate([[0], np.cumsum(SPLIT)]).tolist()
PI = float(np.pi)


class Prog:
    ENGS = ("pe", "act", "dve", "pool", "sp")

    def __init__(self, nc, es, n_dma_sems=48):
        self.nc = nc
        self.eng_sem = {e: es.enter_context(nc.semaphore("s_" + e)) for e in self.ENGS}
        self.eng_cnt = {e: 0 for e in self.ENGS}
        self.dma_sems = [es.enter_context(nc.semaphore("d%d" % i)) for i in range(n_dma_sems)]
        self.dma_cnt = [0] * n_dma_sems
        self.dma_key2idx = {}
        self.waited = {e: {} for e in self.ENGS}
        self.cc_sem = es.enter_context(nc.semaphore("s_cc"))
        self.cc_scratch = es.enter_context(nc.sbuf_tensor("cc_scratch", [128, 8], F32))
        self.cc_cnt = 0
        self.ops = []
        self.state = {}
        self.last = {}
        self.pending_dma = []

    def dma_sem_for(self, key):
        if key not in self.dma_key2idx:
            idx = len(self.dma_key2idx)
            assert idx < len(self.dma_sems), "out of dma sems"
            self.dma_key2idx[key] = idx
        return self.dma_key2idx[key]

    def op(self, eng, fn, reads=(), writes=(), dma=None, extra=()):
        deps = set(extra)
        for k in reads:
            st = self.state.setdefault(k, {"w": None, "r": []})
            if st["w"] is not None:
                deps.add(st["w"])
        for k in writes:
            st = self.state.setdefault(k, {"w": None, "r": []})
            if st["w"] is not None:
                deps.add(st["w"])
            deps.update(st["r"])
        oid = len(self.ops)
        self.ops.append(dict(id=oid, eng=eng, fn=fn, deps=deps,
                             dma=None if dma is None else self.dma_sem_for(dma)))
        for k in reads:
            self.state[k]["r"].append(oid)
        for k in writes:
            self.state[k] = {"w": oid, "r": []}
        if dma is None:
            self.last[eng] = oid
        else:
            self.pending_dma.append(oid)
        return oid

    def dma(self, q, out, in_, reads, writes, semkey, **kw):
        return self.op(q, lambda e: e.dma_start(out=out, in_=in_, **kw), reads, writes, dma=semkey)

    def collective(self, kind, in_ap, out_ap, groups, reads, writes):
        def fn(e):
            return e.collective_compute(kind, ALU.bypass, replica_groups=groups, ins=[in_ap.opt()], outs=[out_ap.opt()])
        oid = self.op("pool", fn, reads, writes)
        self.ops[oid]["cc"] = True
        scr = self.cc_scratch
        self.op("pool", lambda e: e.memset(scr[:], 0.0), list(writes), list(writes))
        return oid

    def barrier(self):
        ids = list(self.last.values()) + list(self.pending_dma)
        for e in self.ENGS:
            self.op(e, lambda en: None, extra=ids)
        self.pending_dma = []
        self.state = {}

    def emit(self):
        ops = self.ops

        def pe_pe(a, b):
            return a["eng"] == "pe" and b["eng"] == "pe" and a["dma"] is None and b["dma"] is None

        needed = set()
        for o in ops:
            for d in o["deps"]:
                if not pe_pe(ops[d], o):
                    needed.add(d)
        for o in ops:
            if o.get("cc"):
                self.cc_cnt += 1
                o["sig"] = (self.cc_sem, self.cc_cnt, None)
            elif o["dma"] is not None:
                self.dma_cnt[o["dma"]] += 16
                o["sig"] = (self.dma_sems[o["dma"]], self.dma_cnt[o["dma"]], 16)
            elif o["id"] in needed:
                self.eng_cnt[o["eng"]] += 1
                o["sig"] = (self.eng_sem[o["eng"]], self.eng_cnt[o["eng"]], 1)
            else:
                o["sig"] = None
        per = {e: [] for e in self.ENGS}
        carry = {e: [] for e in self.ENGS}
        for o in ops:
            w = {}
            for d in o["deps"]:
                if pe_pe(ops[d], o):
                    continue
                sg = ops[d]["sig"]
                k = id(sg[0])
                if k not in w or w[k][1] < sg[1]:
                    w[k] = (sg[0], sg[1])
            wl = carry[o["eng"]]
            carry[o["eng"]] = []
            wd = self.waited[o["eng"]]
            for k, (s, v) in w.items():
                if wd.get(k, 0) >= v:
                    continue
                wd[k] = v
                wl.append((s, v))
            o["waits"] = wl
            per[o["eng"]].append(o)

        def replay(lst):
            def f(e):
                pend_sig = None
                for o in lst:
                    for (s, v) in o["waits"]:
                        e.wait_ge(s, v)
                    ins = o["fn"](e)
                    if o["sig"] is not None:
                        assert ins is not None, "signalling op must emit an instruction"
                        if o["sig"][2] is None:
                            ins.then_inc(o["sig"][0])
                        else:
                            ins.then_inc(o["sig"][0], o["sig"][2])
            return f

        with self.nc.Block() as block:
            if per["pe"]:
                block.tensor(replay(per["pe"]))
            if per["act"]:
                block.scalar(replay(per["act"]))
            if per["dve"]:
                block.vector(replay(per["dve"]))
            if per["pool"]:
                block.gpsimd(replay(per["pool"]))
            if per["sp"]:
                block.sync(replay(per["sp"]))
        n = {e: len(per[e]) for e in self.ENGS}
        self.ops = []
        self.state = {}
        self.last = {}
        self.pending_dma = []
        return n


class Ctx:
    def __init__(self, nc, es, P):
        self.nc, self.es, self.P = nc, es, P
        self.n = 0

    UID = [0]

    def sb(self, name, shape, dt=F32):
        Ctx.UID[0] += 1
        return self.es.enter_context(self.nc.sbuf_tensor("sb%d_%s" % (Ctx.UID[0], name), shape, dt))

    def ps(self, name, shape, dt=F32):
        Ctx.UID[0] += 1
        return self.es.enter_context(self.nc.psum_tensor("ps%d_%s" % (Ctx.UID[0], name), shape, dt))


def make_ident(P, ident, key="ident"):
    P.op("pool", lambda e: e.memset(ident[:], 1.0), [], [key])

    def sel(e):
        if getattr(P, "zero_reg", None) is None:
            P.zero_reg = e.to_reg(0.0)
        return e.affine_select(out=ident[:], in_=ident[:], pattern=[[-1, 128]], compare_op=ALU.is_equal,
                               fill=P.zero_reg, base=0, channel_multiplier=1)
    P.op("pool", sel, [key], [key])


def load_bcast_rows(P, C, name, src_row_ap, n):
    t = C.sb(name, [128, n])
    P.dma("sp", t[:], src_row_ap.partition_broadcast(128), [], [name], name)
    return t


def load_weight_bf16(P, C, w_ap, K, N, dst, dkey, stg, eng_cast="pool"):
    wv = w_ap.rearrange("(c p) n -> p c n", p=128)
    nk = K // 128
    i = 0
    for c in range(nk):
        for n0 in range(0, N, 1024):
            n1 = min(N, n0 + 1024)
            sk = ("stg", stg["i"] % 2)
            st = stg["t"][stg["i"] % 2]
            stg["i"] += 1
            P.dma("sp", st[:, 0:n1 - n0], wv[:, c, n0:n1], [], [sk], sk)
            if stg["i"] % 2 == 0:
                P.op("dve", lambda e, st=st, c=c, n0=n0, n1=n1: e.tensor_copy(out=dst[:, c, n0:n1], in_=st[:, 0:n1 - n0]),
                     [sk], [dkey])
            else:
                P.op("act", lambda e, st=st, c=c, n0=n0, n1=n1: e.copy(out=dst[:, c, n0:n1], in_=st[:, 0:n1 - n0]),
                     [sk], [dkey])


def rstd_of(P, C, src, skey, tag, junk, n=D):
    ss = C.tmp["ss"]
    P.op("act", lambda e: e.activation(out=junk[:, 0:n], in_=src, func=AF.Square, accum_out=ss[:, 0:1]),
         [skey], ["junk", "ss"])
    P.op("dve", lambda e: e.tensor_scalar(out=ss[:, 1:2], in0=ss[:, 0:1], scalar1=1.0 / n, scalar2=EPS,
                                          op0=ALU.mult, op1=ALU.add), ["ss"], ["ss1"])
    P.op("act", lambda e: e.activation(out=ss[:, 2:3], in_=ss[:, 1:2], func=AF.Sqrt), ["ss1"], ["ss2"])
    P.op("dve", lambda e: e.reciprocal(out=ss[:, 3:4], in_=ss[:, 2:3]), ["ss2"], ["ss3"])
    return ss[:, 3:4], "ss3"


def norm_T(P, C, src, skey, g_t, gkey, hbf, ident, trp, trkey, dstT, dkey, col0):
    r, rk = rstd_of(P, C, src, skey, "n", C.tmp["junk"])
    P.op("dve", lambda e: e.scalar_tensor_tensor(out=hbf[:], in0=src, scalar=r, in1=g_t[:],
                                                 op0=ALU.mult, op1=ALU.mult), [skey, rk, gkey], ["hbf"])
    for c in range(8):
        P.op("pe", lambda e, c=c: e.transpose(trp[:, c * 128:(c + 1) * 128], hbf[:, c * 128:(c + 1) * 128], ident[:]),
             ["hbf", "ident"], [trkey])
    P.op("act", lambda e: e.copy(out=dstT[:, :, col0:col0 + 128],
                                 in_=trp[:].rearrange("p (c t) -> p c t", c=8)), [trkey], [dkey])


def emit_P0(nc, P, x_ap, g_ap, hT_ap, ntok):
    with ExitStack() as es:
        C = Ctx(nc, es, P)
        C.tmp = {"ss": C.sb("ss", [128, 4]), "junk": C.sb("junk", [128, D], BF16)}
        ident = C.sb("ident", [128, 128], BF16)
        make_ident(P, ident)
        g_t = load_bcast_rows(P, C, "g0", g_ap, D)
        hbf = C.sb("hbf", [128, D], BF16)
        xs = [C.sb("x%d" % i, [128, D]) for i in range(2)]
        trp = [C.ps("tr%d" % i, [128, D], BF16) for i in range(2)]
        hT = [C.sb("hT%d" % i, [128, 8, 512], BF16) for i in range(2)]
        hTv = hT_ap.rearrange("(c p) t -> p c t", p=128)
        for st in range(ntok // 512):
            hk = ("hT", st % 2)
            for sub in range(4):
                i = st * 4 + sub
                xk = ("x", i % 2)
                P.dma("sp", xs[i % 2][:], x_ap[i * 128:(i + 1) * 128, :], [], [xk], xk)
                norm_T(P, C, xs[i % 2][:], xk, g_t, "g0", hbf, ident, trp[i % 2], ("tr", i % 2),
                       hT[st % 2], hk, sub * 128)
            P.dma("sp", hTv[:, :, st * 512:(st + 1) * 512], hT[st % 2][:], [hk], ["hT_out"], hk)
        P.barrier()
        return P.emit()


def emit_B1(nc, P, mixT_ap, x_ap, mem_ap, gm_ap, w_out, w_cq, w_ck, w_cv, w_co,
            g_post_mix, g_pre_cross, g_post_cross, x2_ap, mix_fn=None):
    with ExitStack() as es:
        C = Ctx(nc, es, P)
        C.tmp = {"ss": C.sb("ss", [128, 4]), "junk": C.sb("junk", [128, D], BF16)}
        ident = C.sb("ident", [128, 128], BF16)
        make_ident(P, ident)
        ones_bf = C.sb("ones_bf", [128, 128], BF16)
        P.op("pool", lambda e: e.memset(ones_bf[:], 1.0), [], ["ones_bf"])
        stg = {"i": 0, "t": [C.sb("stg%d" % i, [128, 1024]) for i in range(2)]}
        gpm = load_bcast_rows(P, C, "gpm", g_post_mix, D)
        gpc = load_bcast_rows(P, C, "gpc", g_pre_cross, D)
        gqc = load_bcast_rows(P, C, "gqc", g_post_cross, D)
        gmm = load_bcast_rows(P, C, "gmm", gm_ap, D)
        wo = C.sb("wo", [128, 8, D], BF16)
        wq = C.sb("wq", [128, 8, D], BF16)
        wc = C.sb("wc", [128, 8, D], BF16)
        wk = C.sb("wk", [128, 8, D], BF16)
        load_weight_bf16(P, C, w_ck, D, D, wk, "wk", stg)
        hbf = C.sb("hbf", [128, D], BF16)
        acc = [C.ps("acc%d" % i, [128, D]) for i in range(2)]
        trp = [C.ps("tr%d" % i, [128, D], BF16) for i in range(2)]
        mb = [C.ps("mb%d" % i, [128, 512]) for i in range(2)]
        memT = C.sb("memT", [128, 8, 256], BF16)
        xs = [C.sb("x%d" % i, [128, D]) for i in range(4)]
        for i in range(2):
            xk = ("x", i)
            P.dma("sp", xs[i][:], mem_ap[i * 128:(i + 1) * 128, :], [], [xk], xk)
            norm_T(P, C, xs[i][:], xk, gmm, "gmm", hbf, ident, trp[i], ("tr", i), memT, "memT", i * 128)
        kT = C.sb("kT", [128, 8, 256], BF16)
        for cc in range(8):
            for f in range(8):
                P.op("pe", lambda e, cc=cc, f=f: e.matmul(mb[cc % 2][:, 0:256], lhsT=wk[:, f, cc * 128:(cc + 1) * 128],
                                                           rhs=memT[:, f, :], start=(f == 0), stop=(f == 7)),
                     ["wk", "memT"], [("mb", cc % 2)])
            P.op("act", lambda e, cc=cc: e.copy(out=kT[:, cc, :], in_=mb[cc % 2][:, 0:256]), [("mb", cc % 2)], ["kT"])
        load_weight_bf16(P, C, w_cv, D, D, wk, "wk", stg)
        vm = C.sb("vm", [128, 2, D], BF16)
        for m in range(2):
            for half in range(2):
                for f in range(8):
                    P.op("pe", lambda e, m=m, half=half, f=f: e.matmul(
                        acc[m][:, half * 512:(half + 1) * 512], lhsT=memT[:, f, m * 128:(m + 1) * 128],
                        rhs=wk[:, f, half * 512:(half + 1) * 512], start=(f == 0), stop=(f == 7)),
                        ["wk", "memT"], [("acc", m)])
            P.op("act", lambda e, m=m: e.copy(out=vm[:, m, :], in_=acc[m][:]), [("acc", m)], ["vm"])
        load_weight_bf16(P, C, w_out, D, D, wo, "wo", stg)
        load_weight_bf16(P, C, w_cq, D, D, wq, "wq", stg)
        load_weight_bf16(P, C, w_co, D, D, wc, "wc", stg)
        mixT = [C.sb("mixT%d" % i, [128, 8, 512], BF16) for i in range(2)]
        h2T = C.sb("h2T", [128, 8, 512], BF16)
        qT = C.sb("qT", [128, 8, 512], BF16)
        PT = C.sb("PT", [128, 8, 512], BF16)
        oT = C.sb("oT", [128, 8, 512], BF16)
        rec = C.sb("rec", [128, 512])
        tmp = C.sb("tmp", [128, D])
        mix_q = "pool"
        if mix_fn is None:
            mix_q = "sp"
            mixv = mixT_ap.rearrange("(c p) t -> p c t", p=128)
            mix_fn = lambda e, st, r: mixv[:, 2 * r:2 * r + 2, st * 512:(st + 1) * 512]
        nst = TOK // 512
        for st in range(nst):
            mk = ("mixT", st % 2)
            for r_ in range(4):
                mkr = ("mixT", st % 2, r_)
                P.op(mix_q, lambda e, st=st, r_=r_: e.dma_start(out=mixT[st % 2][:, 2 * r_:2 * r_ + 2, :], in_=mix_fn(e, st, r_)), [], [mkr], dma=mkr)
            for sub in range(4):
                t0 = st * 512 + sub * 128
                xk = ("x", sub)
                ak = ("acc", sub % 2)
                a = acc[sub % 2]
                P.dma("sp", xs[sub][:], x_ap[t0:t0 + 128, :], [], [xk], xk)
                for half in range(2):
                    for c in range(8):
                        P.op("pe", lambda e, a=a, half=half, c=c, sub=sub, st=st: e.matmul(
                            a[:, half * 512:(half + 1) * 512], lhsT=mixT[st % 2][:, c, sub * 128:(sub + 1) * 128],
                            rhs=wo[:, c, half * 512:(half + 1) * 512], start=(c == 0), stop=(c == 7)),
                            [("mixT", st % 2, c // 2), "wo"], [ak])
                r, rk = rstd_of(P, C, a[:], ak, "y", C.tmp["junk"])
                P.op("dve", lambda e, a=a, r=r: e.scalar_tensor_tensor(out=tmp[:], in0=a[:], scalar=r, in1=gpm[:],
                                                                       op0=ALU.mult, op1=ALU.mult), [ak, rk, "gpm"], ["tmp"])
                P.op("pool", lambda e, sub=sub: e.tensor_tensor(out=xs[sub][:], in0=xs[sub][:], in1=tmp[:], op=ALU.add),
                     [xk, "tmp"], [xk])
                norm_T(P, C, xs[sub][:], xk, gpc, "gpc", hbf, ident, trp[sub % 2], ("tr", sub % 2), h2T, "h2T", sub * 128)
            for cc in range(8):
                for f in range(8):
                    P.op("pe", lambda e, cc=cc, f=f: e.matmul(mb[cc % 2][:], lhsT=wq[:, f, cc * 128:(cc + 1) * 128],
                                                               rhs=h2T[:, f, :], start=(f == 0), stop=(f == 7)),
                         ["wq", "h2T"], [("mb", cc % 2)])
                if cc % 2 == 0:
                    P.op("act", lambda e, cc=cc: e.copy(out=qT[:, cc, :], in_=mb[cc % 2][:]), [("mb", cc % 2)], [("qT", cc)])
                else:
                    P.op("dve", lambda e, cc=cc: e.tensor_copy(out=qT[:, cc, :], in_=mb[cc % 2][:]), [("mb", cc % 2)], [("qT", cc)])
            for h in range(4):
                for m in range(2):
                    bk = ("mb", m)
                    for dc in range(2):
                        P.op("pe", lambda e, h=h, m=m, dc=dc: e.matmul(
                            mb[m][:], lhsT=kT[:, 2 * h + dc, m * 128:(m + 1) * 128], rhs=qT[:, 2 * h + dc, :],
                            start=(dc == 0), stop=(dc == 1)), ["kT", ("qT", 2 * h + dc)], [bk])
                    P.op("act", lambda e, h=h, m=m: e.activation(out=PT[:, 2 * h + m, :], in_=mb[m][:], func=AF.Exp,
                                                                 scale=1.0 / 16.0), [bk], [("PT", 2 * h + m)])
                for m in range(2):
                    P.op("pe", lambda e, h=h, m=m: e.matmul(acc[0][:, 0:512], lhsT=ones_bf[:], rhs=PT[:, 2 * h + m, :],
                                                            start=(m == 0), stop=(m == 1)),
                         ["ones_bf", ("PT", 2 * h + m)], [("acc", 0)])
                P.op("dve", lambda e: e.reciprocal(out=rec[:], in_=acc[0][:, 0:512]), [("acc", 0)], ["rec"])
                for dc in range(2):
                    for m in range(2):
                        P.op("pe", lambda e, h=h, m=m, dc=dc: e.matmul(
                            acc[1][:, dc * 512:(dc + 1) * 512], lhsT=vm[:, m, (2 * h + dc) * 128:(2 * h + dc + 1) * 128],
                            rhs=PT[:, 2 * h + m, :], start=(m == 0), stop=(m == 1)),
                            ["vm", ("PT", 2 * h + m)], [("acc", 1)])
                for dc in range(2):
                    P.op("dve", lambda e, h=h, dc=dc: e.tensor_tensor(out=oT[:, 2 * h + dc, :],
                                                                      in0=acc[1][:, dc * 512:(dc + 1) * 512], in1=rec[:],
                                                                      op=ALU.mult), [("acc", 1), "rec"], [("oT", 2 * h + dc)])
            for sub in range(4):
                t0 = st * 512 + sub * 128
                xk = ("x", sub)
                ak = ("acc", sub % 2)
                a = acc[sub % 2]
                for half in range(2):
                    for c in range(8):
                        P.op("pe", lambda e, a=a, half=half, c=c, sub=sub: e.matmul(
                            a[:, half * 512:(half + 1) * 512], lhsT=oT[:, c, sub * 128:(sub + 1) * 128],
                            rhs=wc[:, c, half * 512:(half + 1) * 512], start=(c == 0), stop=(c == 7)),
                            [("oT", c), "wc"], [ak])
                r, rk = rstd_of(P, C, a[:], ak, "y", C.tmp["junk"])
                P.op("dve", lambda e, a=a, r=r: e.scalar_tensor_tensor(out=tmp[:], in0=a[:], scalar=r, in1=gqc[:],
                                                                       op0=ALU.mult, op1=ALU.mult), [ak, rk, "gqc"], ["tmp"])
                P.op("pool", lambda e, sub=sub: e.tensor_tensor(out=xs[sub][:], in0=xs[sub][:], in1=tmp[:], op=ALU.add),
                     [xk, "tmp"], [xk])
                P.dma("sp", x2_ap[t0:t0 + 128, :], xs[sub][:], [xk], ["x2_out"], xk)
        P.barrier()
        return P.emit()


def emit_B2(nc, P, x2_ap, w_gate, w_up, w_down, g_pre_ffn, g_post_ffn, g_next, x3_ap, hT_ap):
    ST = 512
    with ExitStack() as es:
        C = Ctx(nc, es, P)
        C.tmp = {"ss": C.sb("ss", [128, 4]), "junk": C.sb("junk", [128, D], BF16)}
        ident = C.sb("ident", [128, 128], BF16)
        make_ident(P, ident)
        stg = {"i": 0, "t": [C.sb("stg%d" % i, [128, 1024]) for i in range(2)]}
        gpf = load_bcast_rows(P, C, "gpf", g_pre_ffn, D)
        gqf = load_bcast_rows(P, C, "gqf", g_post_ffn, D)
        gnx = load_bcast_rows(P, C, "gnx", g_next, D) if g_next is not None else None
        wg = C.sb("wg", [128, 8, DFF], BF16)
        wu = C.sb("wu", [128, 8, DFF], BF16)
        wd = C.sb("wd", [128, NFF, D], BF16)
        load_weight_bf16(P, C, w_gate, D, DFF, wg, "wg", stg)
        load_weight_bf16(P, C, w_up, D, DFF, wu, "wu", stg)
        load_weight_bf16(P, C, w_down, DFF, D, wd, "wd", stg)
        hbf = C.sb("hbf", [128, D], BF16)
        acc = [C.ps("acc%d" % i, [128, D]) for i in range(2)]
        trp = [C.ps("tr%d" % i, [128, D], BF16) for i in range(2)]
        mb = [C.ps("mb%d" % i, [128, 512]) for i in range(2)]
        xs = [C.sb("x%d" % i, [128, D]) for i in range(2)]
        h3T = C.sb("h3T", [128, 8, ST], BF16)
        aT = C.sb("aT", [128, NFF, ST], BF16)
        sg = C.sb("sg", [128, ST])
        tmp = C.sb("tmp", [128, D])
        nsub = ST // 128
        hTv = hT_ap.rearrange("(c p) t -> p c t", p=128) if hT_ap is not None else None
        xi = 0
        for st in range(TOK // ST):
            for sub in range(nsub):
                t0 = st * ST + sub * 128
                xk = ("x", xi % 2)
                xt = xs[xi % 2]
                P.dma("sp", xt[:], x2_ap[t0:t0 + 128, :], [], [xk], xk)
                norm_T(P, C, xt[:], xk, gpf, "gpf", hbf, ident, trp[xi % 2], ("tr", xi % 2), h3T, "h3T", sub * 128)
                xi += 1
            for fc in range(NFF):
                for f in range(8):
                    P.op("pe", lambda e, fc=fc, f=f: e.matmul(mb[0][:, 0:ST], lhsT=wg[:, f, fc * 128:(fc + 1) * 128],
                                                               rhs=h3T[:, f, :], start=(f == 0), stop=(f == 7)),
                         ["wg", "h3T"], [("mb", 0)])
                for f in range(8):
                    P.op("pe", lambda e, fc=fc, f=f: e.matmul(mb[1][:, 0:ST], lhsT=wu[:, f, fc * 128:(fc + 1) * 128],
                                                               rhs=h3T[:, f, :], start=(f == 0), stop=(f == 7)),
                         ["wu", "h3T"], [("mb", 1)])
                P.op("act", lambda e: e.activation(out=sg[:], in_=mb[0][:, 0:ST], func=AF.Silu), [("mb", 0)], ["sg"])
                P.op("dve", lambda e, fc=fc: e.tensor_tensor(out=aT[:, fc, :], in0=mb[1][:, 0:ST], in1=sg[:], op=ALU.mult),
                     [("mb", 1), "sg"], [("aT", fc)])
            for sub in range(nsub):
                t0 = st * ST + sub * 128
                xk = ("x", xi % 2)
                xt = xs[xi % 2]
                ak = ("acc", sub % 2)
                a = acc[sub % 2]
                P.dma("sp", xt[:], x2_ap[t0:t0 + 128, :], [], [xk], xk)
                for half in range(2):
                    for fc in range(NFF):
                        P.op("pe", lambda e, a=a, half=half, fc=fc, sub=sub: e.matmul(
                            a[:, half * 512:(half + 1) * 512], lhsT=aT[:, fc, sub * 128:(sub + 1) * 128],
                            rhs=wd[:, fc, half * 512:(half + 1) * 512], start=(fc == 0), stop=(fc == NFF - 1)),
                            [("aT", fc), "wd"], [ak])
                r, rk = rstd_of(P, C, a[:], ak, "y", C.tmp["junk"])
                P.op("dve", lambda e, a=a, r=r: e.scalar_tensor_tensor(out=tmp[:], in0=a[:], scalar=r, in1=gqf[:],
                                                                       op0=ALU.mult, op1=ALU.mult), [ak, rk, "gqf"], ["tmp"])
                P.op("pool", lambda e, xt=xt: e.tensor_tensor(out=xt[:], in0=xt[:], in1=tmp[:], op=ALU.add),
                     [xk, "tmp"], [xk])
                P.dma("sp", x3_ap[t0:t0 + 128, :], xt[:], [xk], ["x3_out"], xk)
                if gnx is not None:
                    norm_T(P, C, xt[:], xk, gnx, "gnx", hbf, ident, trp[xi % 2], ("tr", xi % 2), h3T, "h3T", sub * 128)
                xi += 1
            if gnx is not None:
                P.dma("sp", hTv[:, :, st * ST:(st + 1) * ST], h3T[:], ["h3T"], ["hT_out"], "h3T")
        P.barrier()
        return P.emit()


NCH = SEQ // 512


def hT_chunk(hT_ap, ci):
    return hT_ap[:, ci // 8, :, (ci % 8) * 512:(ci % 8 + 1) * 512].rearrange("c p t -> p c t")


def emit_A_lru(nc, P, hT_ap, wa, wg2, pa, mix_ap):
    with ExitStack() as es:
        C = Ctx(nc, es, P)
        stg = {"i": 0, "t": [C.sb("stg%d" % i, [128, 1024]) for i in range(2)]}
        pat = C.sb("pat", [128, 16])
        P.dma("sp", pat[:], pa, [], ["pat"], "pat")
        wl = C.sb("wl", [128, 8, 128], BF16)
        load_weight_bf16(P, C, wa[:, 0:128], D, 128, wl, "wl", stg)
        wgs = C.sb("wgs", [64, 128])
        wgb = C.sb("wgb", [64, 128], BF16)
        P.dma("sp", wgs[:], wg2, [], ["wgs"], "wgs")
        P.op("pool", lambda e: e.tensor_copy(out=wgb[:], in_=wgs[:]), ["wgs"], ["wgb"])
        kap = C.sb("kap", [64, 4])
        P.op("act", lambda e: e.activation(out=kap[:, 0:1], in_=pat[0:64, 7:8], func=AF.Exp, scale=-1.0), ["pat"], ["kap0"])
        P.op("act", lambda e: e.activation(out=kap[:, 1:2], in_=kap[:, 0:1], func=AF.Ln, bias=1.0), ["kap0"], ["kap1"])
        P.op("dve", lambda e: e.tensor_scalar(out=kap[:, 2:3], in0=kap[:, 1:2], scalar1=-8.0, scalar2=None, op0=ALU.mult), ["kap1"], ["kap2"])
        P.op("dve", lambda e: e.tensor_scalar(out=kap[:, 3:4], in0=kap[:, 1:2], scalar1=-16.0, scalar2=None, op0=ALU.mult), ["kap1"], ["kap3"])
        hc = [C.sb("hc%d" % i, [128, 8, 512], BF16) for i in range(2)]
        pm2 = [[C.ps("pm%d%d" % (p_, i), [128, 512]) for i in range(2)] for p_ in range(2)]
        pg2 = [[C.ps("pg%d%d" % (p_, i), [128, 512]) for i in range(2)] for p_ in range(2)]
        lxb = C.sb("lxb", [64, 515])
        P.op("pool", lambda e: e.memset(lxb[:, 0:3], 0.0), [], ["lxb"])
        f32t2 = [{n: C.sb(n + str(p_), [64, 512]) for n in ["xc", "sr", "si", "a", "a2", "sq", "ix", "u", "gl"]} for p_ in range(2)]
        xcb2 = [C.sb("xcb%d" % p_, [64, 512], BF16) for p_ in range(2)]
        hb = [C.sb("hb%d" % i, [64, 512]) for i in range(2)]
        ob = [C.sb("ob%d" % i, [64, 512], BF16) for i in range(2)]
        for ci in range(NCH):
            par = ci % 2
            T = f32t2[par]
            xcb = xcb2[par]
            pm = pm2[par]
            pg = pg2[par]
            K_ = lambda n, par=par: (n, par)
            hk = ("hc", par)
            P.dma("sp", hc[par][:], hT_chunk(hT_ap, ci), [], [hk], hk)
            for g in range(2):
                for f in range(8):
                    P.op("pe", lambda e, g=g, f=f, par=par, pm=pm: e.matmul(pm[g][0:64, :], lhsT=wl[:, f, g * 64:(g + 1) * 64],
                                                                      rhs=hc[par][:, f, :], start=(f == 0), stop=(f == 7)),
                         ["wl", hk], [("pm", par, g)])
            P.op("act", lambda e, T=T, pm=pm, pg=pg, xcb=xcb: e.copy(out=lxb[:, 3:515], in_=pm[0][0:64, :]), [("pm", par, 0)], ["lxb"])
            P.op("dve", lambda e, T=T, pm=pm, pg=pg, xcb=xcb: e.tensor_scalar(out=T[("xc")][:], in0=lxb[:, 3:515], scalar1=pat[0:64, 3:4], scalar2=pat[0:64, 4:5],
                                                  op0=ALU.mult, op1=ALU.add), ["lxb", "pat"], [K_("xc")])
            for k in (2, 1, 0):
                P.op("dve", lambda e, k=k, T=T: e.scalar_tensor_tensor(out=T[("xc")][:], in0=lxb[:, k:k + 512], scalar=pat[0:64, k:k + 1],
                                                                  in1=T[("xc")][:], op0=ALU.mult, op1=ALU.add), ["lxb", "pat", K_("xc")], [K_("xc")])
            P.op("dve", lambda e, T=T, pm=pm, pg=pg, xcb=xcb: e.tensor_copy(out=lxb[:, 0:3], in_=lxb[:, 512:515]), ["lxb"], ["lxb"])
            P.op("pool", lambda e, T=T, pm=pm, pg=pg, xcb=xcb: e.tensor_copy(out=xcb[:], in_=T[("xc")][:]), [K_("xc")], [K_("xcb")])
            for g in range(2):
                P.op("pe", lambda e, g=g, pg=pg, xcb=xcb: e.matmul(pg[g][0:64, :], lhsT=wgb[:, g * 64:(g + 1) * 64], rhs=xcb[:], start=True, stop=True),
                     ["wgb", K_("xcb")], [("pg", par, g)])
            P.op("act", lambda e, T=T, pm=pm, pg=pg, xcb=xcb: e.activation(out=T[("sr")][:], in_=pg[0][0:64, :], func=AF.Sigmoid, bias=pat[0:64, 5:6]), [("pg", par, 0), "pat"], [K_("sr")])
            P.op("act", lambda e, T=T, pm=pm, pg=pg, xcb=xcb: e.activation(out=T[("si")][:], in_=pg[1][0:64, :], func=AF.Sigmoid, bias=pat[0:64, 6:7]), [("pg", par, 1), "pat"], [K_("si")])
            P.op("act", lambda e, T=T, pm=pm, pg=pg, xcb=xcb: e.activation(out=T[("a")][:], in_=T[("sr")][:], func=AF.Exp, scale=kap[:, 2:3]), [K_("sr"), "kap2"], [K_("a")])
            P.op("act", lambda e, T=T, pm=pm, pg=pg, xcb=xcb: e.activation(out=T[("a2")][:], in_=T[("sr")][:], func=AF.Exp, scale=kap[:, 3:4]), [K_("sr"), "kap3"], [K_("a2")])
            P.op("dve", lambda e, T=T, pm=pm, pg=pg, xcb=xcb: e.tensor_scalar(out=T[("a2")][:], in0=T[("a2")][:], scalar1=-1.0, scalar2=1.0, op0=ALU.mult, op1=ALU.add), [K_("a2")], [K_("a2")])
            P.op("act", lambda e, T=T, pm=pm, pg=pg, xcb=xcb: e.activation(out=T[("sq")][:], in_=T[("a2")][:], func=AF.Sqrt), [K_("a2")], [K_("sq")])
            P.op("pool", lambda e, T=T, pm=pm, pg=pg, xcb=xcb: e.tensor_tensor(out=T[("ix")][:], in0=T[("si")][:], in1=T[("xc")][:], op=ALU.mult), [K_("si"), K_("xc")], [K_("ix")])
            P.op("dve", lambda e, T=T, pm=pm, pg=pg, xcb=xcb: e.tensor_tensor(out=T[("u")][:], in0=T[("sq")][:], in1=T[("ix")][:], op=ALU.mult), [K_("sq"), K_("ix")], [K_("u")])
            init = 0.0 if ci == 0 else hb[1 - par][:, 511:512]
            P.op("dve", lambda e, init=init, par=par, T=T: e.tensor_tensor_scan(out=hb[par][:], data0=T[("a")][:], data1=T[("u")][:], initial=init,
                                                                           op0=ALU.mult, op1=ALU.add), [K_("a"), K_("u"), ("hb", 1 - par)], [("hb", par)])
            P.op("act", lambda e, T=T, pm=pm, pg=pg, xcb=xcb: e.activation(out=T[("gl")][:], in_=pm[1][0:64, :], func=AF.Gelu_apprx_tanh), [("pm", par, 1)], [K_("gl")])
            P.op("dve", lambda e, par=par, T=T: e.tensor_tensor(out=ob[par][:], in0=hb[par][:], in1=T[("gl")][:], op=ALU.mult), [("hb", par), K_("gl")], [("ob", par)])
            ok = ("ob", par)
            P.dma("sp", mix_ap[ci // 8, 0:64, (ci % 8) * 512:(ci % 8 + 1) * 512], ob[par][:], [ok], ["mix_out"], ok)
        P.barrier()
        return P.emit()


C1_2PI = 6.28125
C2_2PI = float(2.0 * np.pi - 6.28125)
PI_SAFE = 3.1415925


def emit_A_ret(nc, P, hT_ap, wa, pa, cst, pos_ap, mix_ap):
    import os
    STAGE = float(os.environ.get("RET_STAGE", "9"))
    with ExitStack() as es:
        C = Ctx(nc, es, P)
        stg = {"i": 0, "t": [C.sb("stg%d" % i, [128, 1024]) for i in range(2)]}
        pat = C.sb("pat", [128, 16])
        P.dma("sp", pat[:], pa, [], ["pat"], "pat")
        cs = C.sb("cs", [128, 416])
        P.dma("sp", cs[:], cst, [], ["cs"], "cs")
        decT = cs[:, 0:128]
        qwbc = cs[0:64, 128:256]
        invf = cs[:, 256:288]
        wr = C.sb("wr", [128, 8, 256], BF16)
        load_weight_bf16(P, C, wa[:, 514:770], D, 256, wr, "wr", stg)
        identb = C.sb("identb", [128, 128], BF16)
        make_ident(P, identb, "identb")
        identf = C.sb("identf", [128, 128])
        make_ident(P, identf, "identf")
        Tps = C.ps("T", [128, 4, 256])
        trq_ = C.ps("trq", [128, 1024], BF16)
        trq = trq_[0:64, 0:256]
        scp_ = C.ps("scp", [128, 512])
        scp = scp_[:, 0:128]
        Yp_ = C.ps("Yp", [128, 512])
        Yp = Yp_[:, 0:256]
        Up_ = C.ps("Up", [128, 512])
        Up = Up_[0:64, 0:64]
        tro_ = C.ps("tro", [128, 1024], BF16)
        tro = tro_[0:64, 0:512]
        posi = C.sb("posi", [128, 128], I32)
        posf = C.sb("posf", [128, 128])
        post = C.sb("post", [128, 128])
        P.dma("sp", posi[:], pos_ap.rearrange("(n p) -> n p", p=128), [], ["posi"], "posi")
        P.op("dve", lambda e: e.tensor_copy(out=posf[:], in_=posi[:]), ["posi"], ["posf"])
        P.op("pe", lambda e: e.matmul(scp, lhsT=posf[:], rhs=identf[:], start=True, stop=True), ["posf", "identf"], ["scp"])
        P.op("act", lambda e: e.copy(out=post[:], in_=scp), ["scp"], ["post"])
        Sf = C.sb("Sf", [64, 64])
        Sb = C.sb("Sb", [64, 64], BF16)
        P.op("pool", lambda e: e.memset(Sf[:], 0.0), [], ["Sf"])
        P.op("pool", lambda e: e.memset(Sb[:], 0.0), [], ["Sb"])
        hc = [C.sb("hc%d" % i, [128, 8, 512], BF16) for i in range(2)]
        ang = C.sb("ang", [128, 4, 32])
        yy = C.sb("yy", [128, 4, 32])
        yi = C.sb("yi", [128, 4, 32], I32)
        rr = C.sb("rr", [128, 4, 32])
        ar = C.sb("ar", [128, 4, 32])
        sin2 = C.sb("sin2", [128, 4, 2, 32])
        cos2 = C.sb("cos2", [128, 4, 2, 32])
        t1 = C.sb("t1", [128, 2, 32])
        t2 = C.sb("t2", [128, 2, 32])
        t3 = C.sb("t3", [128, 2, 32])
        t4 = C.sb("t4", [128, 2, 32])
        rot = C.sb("rot", [128, 2, 2, 32])
        rot2 = C.sb("rot2", [128, 128])
        qkf = C.sb("qkf", [128, 128])
        qkb = C.sb("qkb", [128, 128], BF16)
        kwb = C.sb("kwb", [128, 64], BF16)
        vb = C.sb("vb", [128, 64], BF16)
        qkT = C.sb("qkT", [64, 256], BF16)
        qwT = C.sb("qwT", [64, 128], BF16)
        sm = C.sb("sm", [128, 128], BF16)
        scf = C.sb("scf", [128, 128])
        qf32 = C.sb("qf32", [64, 128])
        sgl = C.sb("sgl", [128, 4, 64])
        st6 = C.sb("st6", [128, 4, 6])
        mv = C.sb("mv", [128, 4, 2])
        ve = C.sb("ve", [128, 4])
        yn = C.sb("yn", [128, 4, 64])
        obf = C.sb("obf", [128, 4, 64], BF16)
        oT = [C.sb("oT%d" % i, [64, 512], BF16) for i in range(2)]
        for ci in range(NCH):
            par = ci % 2
            hk = ("hc", par)
            P.dma("sp", hc[par][:], hT_chunk(hT_ap, ci), [], [hk], hk)
            for sub in range(4):
                for f in range(8):
                    P.op("pe", lambda e, sub=sub, f=f, par=par: e.matmul(Tps[:, sub, :], lhsT=hc[par][:, f, sub * 128:(sub + 1) * 128],
                                                                          rhs=wr[:, f, :], start=(f == 0), stop=(f == 7)),
                         ["wr", hk], [("T", sub // 2)])
                n = 4 * ci + sub
                P.op("dve", lambda e, sub=sub, n=n: e.tensor_scalar(out=ang[:, sub, :], in0=invf, scalar1=post[:, n:n + 1], scalar2=None,
                                                                    op0=ALU.mult), ["cs", "post"], ["ang"])
            if STAGE < 2:
                continue
            P.op("dve", lambda e: e.tensor_scalar(out=yy[:], in0=ang[:], scalar1=float(1.0 / (2.0 * np.pi)), scalar2=None, op0=ALU.mult), ["ang"], ["yy"])
            P.op("dve", lambda e: e.tensor_copy(out=yi[:], in_=yy[:]), ["yy"], ["yi"])
            P.op("dve", lambda e: e.tensor_copy(out=yy[:], in_=yi[:]), ["yi"], ["yy"])
            P.op("dve", lambda e: e.scalar_tensor_tensor(out=rr[:], in0=yy[:], scalar=-C1_2PI, in1=ang[:], op0=ALU.mult, op1=ALU.add), ["yy", "ang"], ["rr"])
            P.op("dve", lambda e: e.scalar_tensor_tensor(out=rr[:], in0=yy[:], scalar=-C2_2PI, in1=rr[:], op0=ALU.mult, op1=ALU.add), ["yy", "rr"], ["rr"])
            P.op("dve", lambda e: e.tensor_scalar(out=rr[:], in0=rr[:], scalar1=PI_SAFE, scalar2=-PI_SAFE, op0=ALU.min, op1=ALU.max), ["rr"], ["rr"])
            P.op("dve", lambda e: e.scalar_tensor_tensor(out=ar[:], in0=rr[:], scalar=-1.0, in1=rr[:], op0=ALU.mult, op1=ALU.max), ["rr"], ["ar"])
            P.op("dve", lambda e: e.tensor_scalar(out=ar[:], in0=ar[:], scalar1=-1.0, scalar2=float(np.pi / 2), op0=ALU.mult, op1=ALU.add), ["ar"], ["ar"])
            for k in range(2):
                P.op("act", lambda e, k=k: e.activation(out=sin2[:, :, k, :], in_=rr[:], func=AF.Sin), ["rr"], ["sin2"])
                P.op("act", lambda e, k=k: e.activation(out=cos2[:, :, k, :], in_=ar[:], func=AF.Sin), ["ar"], ["cos2"])
            if STAGE < 3:
                continue
            for sub in range(4):
                tk = ("T", sub // 2)
                P.op("act", lambda e, sub=sub: e.copy(out=qkf[:], in_=Tps[:, sub, 0:128]), [tk], ["qkf"])
                cs_ = cos2[:, sub, 0, :]
                sn_ = sin2[:, sub, 0, :]
                if STAGE < 3.05:
                    continue
                for a_ in range(2):
                    x1 = qkf[:, a_ * 64:a_ * 64 + 32]
                    x2 = qkf[:, a_ * 64 + 32:a_ * 64 + 64]
                    o1 = rot2[:, a_ * 64:a_ * 64 + 32]
                    o2 = rot2[:, a_ * 64 + 32:a_ * 64 + 64]
                    P.op("pool", lambda e, x1=x1, cs_=cs_: e.tensor_tensor(out=t1[:, 0, :], in0=x1, in1=cs_, op=ALU.mult), ["qkf", "cos2"], ["t1"])
                    P.op("pool", lambda e, x2=x2, sn_=sn_: e.tensor_tensor(out=t2[:, 0, :], in0=x2, in1=sn_, op=ALU.mult), ["qkf", "sin2"], ["t2"])
                    P.op("pool", lambda e, x1=x1, sn_=sn_: e.tensor_tensor(out=t3[:, 0, :], in0=x1, in1=sn_, op=ALU.mult), ["qkf", "sin2"], ["t3"])
                    P.op("pool", lambda e, x2=x2, cs_=cs_: e.tensor_tensor(out=t4[:, 0, :], in0=x2, in1=cs_, op=ALU.mult), ["qkf", "cos2"], ["t4"])
                    P.op("pool", lambda e, o1=o1: e.tensor_tensor(out=o1, in0=t1[:, 0, :], in1=t2[:, 0, :], op=ALU.subtract), ["t1", "t2"], ["rot0"])
                    P.op("pool", lambda e, o2=o2: e.tensor_tensor(out=o2, in0=t3[:, 0, :], in1=t4[:, 0, :], op=ALU.add), ["t3", "t4"], ["rot1"])
                if STAGE < 3.25:
                    continue
                rotf = rot2[:]
                P.op("pool", lambda e, rotf=rotf: e.tensor_copy(out=qkb[:], in_=rotf), ["rot0", "rot1"], ["qkb"])
                P.op("dve", lambda e, rotf=rotf: e.tensor_scalar(out=kwb[:], in0=rotf[:, 64:128], scalar1=pat[:, 11:12], scalar2=None, op0=ALU.mult),
                     ["rot0", "rot1", "pat"], ["kwb"])
                P.op("act", lambda e, sub=sub: e.copy(out=vb[:], in_=Tps[:, sub, 128:192]), [tk], ["vb"])
                if STAGE < 4:
                    continue
                P.op("pe", lambda e: e.transpose(trq[:, 0:128], qkb[:, 0:64], identb[:]), ["qkb", "identb"], ["trq"])
                P.op("pe", lambda e: e.transpose(trq[:, 128:256], qkb[:, 64:128], identb[:]), ["qkb", "identb"], ["trq"])
                P.op("act", lambda e: e.copy(out=qkT[:], in_=trq), ["trq"], ["qkT"])
                P.op("act", lambda e: e.copy(out=qf32[:], in_=trq[:, 0:128]), ["trq"], ["qf32"])
                P.op("pool", lambda e: e.tensor_tensor(out=qwT[:], in0=qf32[:], in1=qwbc, op=ALU.mult), ["qf32", "cs"], ["qwT"])
                P.op("pe", lambda e: e.matmul(scp, lhsT=qkT[:, 128:256], rhs=qkT[:, 0:128], start=True, stop=True), ["qkT"], ["scp"])
                P.op("act", lambda e: e.copy(out=scf[:], in_=scp), ["scp"], ["scf"])
                P.op("pool", lambda e: e.tensor_tensor(out=sm[:], in0=scf[:], in1=decT, op=ALU.mult), ["scf", "cs"], ["sm"])
                yk = "Y"
                P.op("pe", lambda e, sub=sub: e.matmul(Yp[:, sub * 64:(sub + 1) * 64], lhsT=sm[:], rhs=vb[:], start=True, stop=False), ["sm", "vb"], [yk])
                P.op("pe", lambda e, sub=sub: e.matmul(Yp[:, sub * 64:(sub + 1) * 64], lhsT=qwT[:], rhs=Sb[:], start=False, stop=True), ["qwT", "Sb"], [yk])
                P.op("pe", lambda e: e.matmul(Up, lhsT=kwb[:], rhs=vb[:], start=True, stop=True), ["kwb", "vb"], ["Up"])
                P.op("dve", lambda e: e.scalar_tensor_tensor(out=Sf[:], in0=Sf[:], scalar=pat[0:64, 8:9], in1=Up, op0=ALU.mult, op1=ALU.add),
                     ["Sf", "Up", "pat"], ["Sf"])
                P.op("act", lambda e: e.copy(out=Sb[:], in_=Sf[:]), ["Sf"], ["Sb"])
            if STAGE < 5:
                continue
            for hb_ in range(2):
                P.op("act", lambda e, hb_=hb_: e.activation(out=sgl[:, 2 * hb_:2 * hb_ + 2, :], in_=Tps[:, 2 * hb_:2 * hb_ + 2, 192:256], func=AF.Silu),
                     [("T", hb_)], [("sgl", hb_)])
            for sub in range(4):
                P.op("dve", lambda e, sub=sub: e.bn_stats(out=st6[:, sub, :], in_=Yp[:, sub * 64:(sub + 1) * 64]), ["Y"], [("st6", sub)])
                P.op("dve", lambda e, sub=sub: e.bn_aggr(out=mv[:, sub, :], in_=st6[:, sub, :]), [("st6", sub)], [("mv", sub)])
            mvk = [("mv", s_) for s_ in range(4)]
            P.op("dve", lambda e: e.tensor_scalar(out=ve[:], in0=mv[:, :, 1], scalar1=EPS, scalar2=None, op0=ALU.add), mvk, ["ve"])
            P.op("act", lambda e: e.activation(out=ve[:], in_=ve[:], func=AF.Sqrt), ["ve"], ["ve"])
            P.op("dve", lambda e: e.reciprocal(out=ve[:], in_=ve[:]), ["ve"], ["ve"])
            for sub in range(4):
                P.op("dve", lambda e, sub=sub: e.tensor_scalar(out=yn[:, sub, :], in0=Yp[:, sub * 64:(sub + 1) * 64], scalar1=mv[:, sub, 0:1],
                                                               scalar2=ve[:, sub:sub + 1], op0=ALU.subtract, op1=ALU.mult),
                     ["Y", ("mv", sub), "ve"], [("yn", sub)])
            P.op("pool", lambda e: e.tensor_tensor(out=obf[:], in0=yn[:], in1=sgl[:], op=ALU.mult), [("yn", s_) for s_ in range(4)] + [("sgl", 0), ("sgl", 1)], ["obf"])
            for sub in range(4):
                P.op("pe", lambda e, sub=sub: e.transpose(tro[:, sub * 128:(sub + 1) * 128], obf[:, sub, :], identb[:]), ["obf", "identb"], ["tro"])
            ok = ("oT", par)
            P.op("act", lambda e, par=par: e.copy(out=oT[par][:], in_=tro), ["tro"], [ok])
            P.dma("sp", mix_ap[ci // 8, 192:256, (ci % 8) * 512:(ci % 8 + 1) * 512], oT[par][:], [ok], ["mix_out"], ok)
        P.barrier()
        return P.emit()


def emit_A_fox(nc, P, hT_ap, wa, pa, cst, mix_ap, nch=NCH):
    with ExitStack() as es:
        C = Ctx(nc, es, P)
        stg = {"i": 0, "t": [C.sb("stg%d" % i, [128, 1024]) for i in range(2)]}
        pat = C.sb("pat", [128, 16])
        P.dma("sp", pat[:], pa, [], ["pat"], "pat")
        cs = C.sb("cs", [128, 128])
        P.dma("sp", cs[:], cst[:, 288:416], [], ["cs"], "cs")
        maskb = C.sb("maskb", [128, 128], BF16)
        P.op("pool", lambda e: e.tensor_copy(out=maskb[:], in_=cs[:]), ["cs"], ["maskb"])
        identb = C.sb("identb", [128, 128], BF16)
        make_ident(P, identb, "identb")
        onesf = C.sb("onesf", [128, 512])
        P.op("pool", lambda e: e.memset(onesf[:], 1.0), [], ["onesf"])
        nb = C.sb("nb", [128, 2])
        P.op("dve", lambda e: e.tensor_scalar(out=nb[:], in0=pat[:, 9:11], scalar1=-1.0, scalar2=None, op0=ALU.mult), ["pat"], ["nb"])
        wq = C.sb("wq", [128, 8, 130], BF16)
        wk = C.sb("wk", [128, 8, 128], BF16)
        wv = C.sb("wv", [128, 8, 128], BF16)
        load_weight_bf16(P, C, wa[:, 128:258], D, 130, wq, "wq", stg)
        load_weight_bf16(P, C, wa[:, 258:386], D, 128, wk, "wk", stg)
        load_weight_bf16(P, C, wa[:, 386:514], D, 128, wv, "wv", stg)
        KT = [C.sb("KT%d" % h, [65, SEQ], BF16) for h in range(2)]
        for h in range(2):
            P.op("pool", lambda e, h=h: e.memset(KT[h][64:65, :], 1.0), [], [("KT", h)])
        Vs = C.sb("Vs", [128, 128, 2, 65], BF16)
        P.op("pool", lambda e: e.memset(Vs[:], 1.0), [], ["Vs"])
        negc = C.sb("negc", [128, 128, 2])
        QT = [[C.sb("QT%d%d" % (h, p), [65, 512], BF16) for p in range(2)] for h in range(2)]
        crow = [[C.sb("crow%d%d" % (h, p), [65, 512]) for p in range(2)] for h in range(2)]
        e1 = C.sb("e1", [65, 512])
        Osb = C.sb("Osb", [65, 512])
        rcp = C.sb("rcp", [65, 512])
        ofb = [C.sb("ofb%d" % i, [64, 512], BF16) for i in range(2)]
        PTt = [C.sb("PT%d" % i, [128, 512], BF16) for i in range(3)]
        hc = [C.sb("hc%d" % i, [128, 8, 512], BF16) for i in range(2)]
        sbank = [C.ps("sbk%d" % i, [128, 512]) for i in range(3)]
        Ob = [C.ps("Ob%d" % i, [128, 512]) for i in range(2)]
        pj = [C.ps("pj%d" % i, [128, 512]) for i in range(2)]
        pmz = C.ps("pmz", [128, 512])
        pji = [0]

        def nextpj():
            i = pji[0] % 2
            pji[0] += 1
            return pj[i], ("pj", i)

        oi = 0
        pending = []
        for ci in range(nch):
            par = ci % 2
            hk = ("hc", par)
            P.dma("sp", hc[par][:], hT_chunk(hT_ap, ci), [], [hk], hk)
            for h in range(2):
                qk_ = ("QT", h, par)
                ck_ = ("crow", h, par)
                pq, pqk = nextpj()
                for f in range(8):
                    P.op("pe", lambda e, pq=pq, h=h, f=f, par=par: e.matmul(pq[0:65, :], lhsT=wq[:, f, h * 65:(h + 1) * 65], rhs=hc[par][:, f, :],
                                                                           start=(f == 0), stop=(f == 7)), ["wq", hk], [pqk])
                P.op("act", lambda e, pq=pq, h=h, par=par: e.mul(out=QT[h][par][0:64, :], in_=pq[0:64, :], mul=0.125), [pqk], [qk_])
                P.op("act", lambda e, pq=pq, h=h: e.activation(out=e1[64:65, :], in_=pq[64:65, :], func=AF.Exp, scale=-1.0, bias=nb[64:65, h:h + 1]),
                     [pqk, "nb"], ["e1"])
                P.op("act", lambda e: e.activation(out=e1[64:65, :], in_=e1[64:65, :], func=AF.Ln, bias=1.0), ["e1"], ["e1"])
                init = 0.0 if ci == 0 else crow[h][1 - par][64:65, 511:512]
                P.op("dve", lambda e, h=h, par=par, init=init: e.tensor_tensor_scan(out=crow[h][par][64:65, :], data0=onesf[64:65, :], data1=e1[64:65, :],
                                                                                    initial=init, op0=ALU.mult, op1=ALU.subtract),
                     ["e1", "onesf", ("crow", h, 1 - par)], [ck_])
                P.op("dve", lambda e, h=h, par=par: e.tensor_copy(out=QT[h][par][64:65, :], in_=crow[h][par][64:65, :]), [ck_], [qk_])
                pc, pck = nextpj()
                for sub in range(4):
                    P.op("pe", lambda e, pc=pc, h=h, par=par, sub=sub: e.matmul(pc[:, sub:sub + 1], lhsT=crow[h][par][64:65, sub * 128:(sub + 1) * 128],
                                                                               rhs=onesf[64:65, 0:1], start=True, stop=True), [ck_, "onesf"], [pck])
                P.op("dve", lambda e, pc=pc, h=h, ci=ci: e.tensor_scalar(out=negc[:, 4 * ci:4 * ci + 4, h], in0=pc[:, 0:4], scalar1=-1.0, scalar2=None,
                                                                        op0=ALU.mult), [pck], [("negc", h, ci)])
                pk_, pkk = nextpj()
                for f in range(8):
                    P.op("pe", lambda e, pk_=pk_, h=h, f=f, par=par: e.matmul(pk_[0:64, :], lhsT=wk[:, f, h * 64:(h + 1) * 64], rhs=hc[par][:, f, :],
                                                                             start=(f == 0), stop=(f == 7)), ["wk", hk], [pkk])
                P.op("act", lambda e, pk_=pk_, h=h, ci=ci: e.copy(out=KT[h][0:64, ci * 512:(ci + 1) * 512], in_=pk_[0:64, :]), [pkk], [("KT", h, ci)])
            for sub in range(4):
                pv, pvk = nextpj()
                for f in range(8):
                    P.op("pe", lambda e, pv=pv, sub=sub, f=f, par=par: e.matmul(pv[:, 0:128], lhsT=hc[par][:, f, sub * 128:(sub + 1) * 128], rhs=wv[:, f, :],
                                                                               start=(f == 0), stop=(f == 7)), ["wv", hk], [pvk])
                P.op("dve", lambda e, pv=pv, sub=sub, ci=ci: e.tensor_copy(out=Vs[:, 4 * ci + sub, :, 0:64],
                                                                           in_=pv[:, 0:128].rearrange("p (a d) -> p a d", a=2)), [pvk, "Vs"], [("Vs", ci)])
            for h in range(2):
                qk_ = ("QT", h, par)
                nj = 4 * ci + 4
                Okey = ("Ob", h)

                def S(j, h=h, par=par, ci=ci):
                    r = j - 4 * ci
                    q0 = max(0, r) * 128
                    bk = ("sbk", j % 3)
                    bank = sbank[j % 3]
                    diag = r >= 0
                    P.op("pe", lambda e: e.matmul(bank[:, q0:512], lhsT=KT[h][0:65, j * 128:(j + 1) * 128], rhs=QT[h][par][0:65, q0:512],
                                                  start=True, stop=not diag), [("KT", h), ("KT", h, j // 4), qk_], [bk])
                    if diag:
                        P.op("pe", lambda e: e.matmul(bank[:, q0:q0 + 128], lhsT=identb[:], rhs=maskb[:], start=False, stop=True),
                             ["identb", "maskb"], [bk])
                    P.op("act", lambda e: e.activation(out=PTt[j % 3][:, q0:512], in_=bank[:, q0:512], func=AF.Exp, bias=negc[:, j, h:h + 1]),
                         [bk, ("negc", h, j // 4)], [("PT", j % 3)])

                def PV(j, h=h, ci=ci, nj=nj):
                    r = j - 4 * ci
                    q0 = max(0, r) * 128
                    P.op("pe", lambda e: e.matmul(Ob[h][0:65, q0:512], lhsT=Vs[:, j, h, :], rhs=PTt[j % 3][:, q0:512],
                                                  start=(j == 0), stop=(j == nj - 1)), ["Vs", ("Vs", j // 4), ("PT", j % 3)], [Okey])

                S(0)
                S(1)
                for j in range(nj):
                    if j + 2 < nj:
                        S(j + 2)
                    PV(j)
                    if j == 1 and pending:
                        pending.pop(0)()
                def norm(h=h, ci=ci, Okey=Okey):
                    nonlocal oi
                    P.op("act", lambda e: e.copy(out=Osb[:], in_=Ob[h][0:65, :]), [Okey], ["Osb"])
                    P.op("dve", lambda e: e.reciprocal(out=rcp[64:65, :], in_=Osb[64:65, :]), ["Osb"], ["rcp"])
                    P.op("pe", lambda e: e.matmul(pmz[0:64, :], lhsT=onesf[64:65, 0:64], rhs=rcp[64:65, :], start=True, stop=True), ["onesf", "rcp"], ["pmz"])
                    ok = ("ofb", oi % 2)
                    o_t = ofb[oi % 2]
                    oi += 1
                    P.op("dve", lambda e: e.tensor_tensor(out=o_t[:], in0=Osb[0:64, :], in1=pmz[0:64, :], op=ALU.mult), ["Osb", "pmz"], [ok])
                    P.dma("sp", mix_ap[ci // 8, 64 + 64 * h:128 + 64 * h, (ci % 8) * 512:(ci % 8 + 1) * 512], o_t[:], [ok], ["mix_out"], ok)
                pending.append(norm)
        while pending:
            pending.pop(0)()
        P.barrier()
        return P.emit()


def mix_perm():
    perm = []
    for s in range(4):
        perm += list(range(64 * s, 64 * s + 64)) + list(range(256 + 128 * s, 256 + 128 * s + 128)) \
            + list(range(768 + 64 * s, 768 + 64 * s + 64))
    return np.array(perm)


def a_weights(inp, l, s):
    w = inp["w_in"][l]
    A, B = 2 * s, 2 * s + 1
    cols = []
    cols += list(range(OFF[0] + 64 * s, OFF[0] + 64 * s + 64))
    cols += list(range(OFF[1] + 64 * s, OFF[1] + 64 * s + 64))
    cols += list(range(OFF[2] + 64 * A, OFF[2] + 64 * A + 64)) + [OFF[5] + A]
    cols += list(range(OFF[2] + 64 * B, OFF[2] + 64 * B + 64)) + [OFF[5] + B]
    cols += list(range(OFF[3] + 64 * A, OFF[3] + 64 * A + 64))
    cols += list(range(OFF[3] + 64 * B, OFF[3] + 64 * B + 64))
    cols += list(range(OFF[4] + 64 * A, OFF[4] + 64 * A + 64))
    cols += list(range(OFF[4] + 64 * B, OFF[4] + 64 * B + 64))
    for g in (6, 7, 8, 9):
        cols += list(range(OFF[g] + 64 * s, OFF[g] + 64 * s + 64))
    wa = np.ascontiguousarray(w[:, cols])
    wg2 = np.ascontiguousarray(np.concatenate([inp["w_rg"][l, s], inp["w_ig"][l, s]], axis=1))
    log_gamma = np.log1p(-np.exp2(-5.0 - np.arange(4, dtype=np.float32))).astype(np.float32)
    lg = log_gamma[s]
    idx = np.arange(128, dtype=np.float32)
    pa = np.zeros((128, 16), np.float32)
    sl = slice(64 * s, 64 * s + 64)
    for k in range(4):
        pa[0:64, k] = inp["conv_w"][l, k, sl]
    pa[0:64, 4] = inp["conv_b"][l, sl]
    pa[0:64, 5] = inp["b_rg"][l, sl]
    pa[0:64, 6] = inp["b_ig"][l, sl]
    pa[0:64, 7] = inp["lru_lambda"][l, sl]
    pa[:, 8] = np.exp(lg * np.float32(128.0))
    pa[:, 9] = inp["fox_b_f"][l, A]
    pa[:, 10] = inp["fox_b_f"][l, B]
    pa[:, 11] = np.exp(lg * (np.float32(127.0) - idx)) * np.float32(0.125)
    cst = np.zeros((128, 416), np.float32)
    diff = idx[:, None] - idx[None, :]
    decay = np.where(diff >= 0, np.exp(lg * np.maximum(diff, 0.0)), 0.0).astype(np.float32)
    cst[:, 0:128] = decay.T * np.float32(0.125)
    cst[:, 128:256] = np.exp(lg * (idx + 1.0))[None, :]
    half = 32
    cst[:, 256:288] = (np.float32(10000.0) ** (-np.arange(half, dtype=np.float32) / np.float32(half))).astype(np.float32)[None, :]
    kk = np.arange(128)
    cst[:, 288:416] = np.where(kk[:, None] > kk[None, :], -30000.0, 0.0)
    return wa, wg2, pa, cst


def _dt(nc, n, s, t=F32, k="ExternalInput"):
    return nc.dram_tensor(n, s, t, kind=k).ap()


GROUPS = [[0, 1, 2, 3], [4, 5, 6, 7]]


def build_fused(stop=99):
    from concourse.bass import DynSlice
    nc = bass.Bass("TRN2", target_bir_lowering=False)
    x = _dt(nc, "x", [TOK, D])
    mem = _dt(nc, "mem", [256, D])
    gm = _dt(nc, "gm", [D])
    pos = _dt(nc, "pos", [SEQ], I32)
    cst = _dt(nc, "cst", [128, 416])
    g0 = _dt(nc, "g0", [D])
    L = []
    for l in range(DEPTH):
        d = {"wa": _dt(nc, "wa%d" % l, [D, 770]), "wg2": _dt(nc, "wg2%d" % l, [64, 128]), "pa": _dt(nc, "pa%d" % l, [128, 16])}
        for n in ["w_out", "w_cq", "w_ck", "w_cv", "w_co"]:
            d[n] = _dt(nc, "%s%d" % (n, l), [D, D])
        for n in ["g_post_mix", "g_pre_cross", "g_post_cross", "g_pre_ffn", "g_post_ffn", "g_next"]:
            d[n] = _dt(nc, "%s%d" % (n, l), [D])
        d["w_gate"] = _dt(nc, "w_gate%d" % l, [D, DFF])
        d["w_up"] = _dt(nc, "w_up%d" % l, [D, DFF])
        d["w_down"] = _dt(nc, "w_down%d" % l, [DFF, D])
        L.append(d)
    out = _dt(nc, "out", [TOK, D], F32, "ExternalOutput")
    hT_own = nc.dram_tensor("hT_own", [D, TOK], BF16).ap()
    hT_all = nc.dram_tensor("hT_all", [4 * D, TOK], BF16).ap()
    mix_c = nc.dram_tensor("mix_c", [4 * 256, TOK], BF16).ap()
    mixG = nc.dram_tensor("mixG", [16 * 256, TOK], BF16).ap()
    x2s = nc.dram_tensor("x2s", [TOK, D], F32).ap()
    x3s = nc.dram_tensor("x3s", [TOK, D], F32).ap()
    hT3 = hT_all.rearrange("(c r p) t -> c r p t", c=8, r=4, p=128)
    mix3 = mix_c.rearrange("(j f) t -> j f t", j=4)
    mixGv = mixG.rearrange("(r j a p) t -> p r j a t", r=4, j=4, a=2, p=128)

    mixT_own = nc.dram_tensor("mixT_own", [D, TOK], BF16).ap()
    mixG5 = mixG.rearrange("(j a r p) t -> j a r p t", j=4, a=2, r=4, p=128)

    with ExitStack() as es:
        P = Prog(nc, es)

        def gather8(src, dst):
            for q in range(8):
                P.collective("AllGather", src[q * 128:(q + 1) * 128, :], dst[q * 512:(q + 1) * 512, :], GROUPS, ["a"], [("b", q)])

        def gather(src, dst, rk, wk):
            gather8(src, dst)
            P.barrier()
            P.emit()

        emit_P0(nc, P, x, g0, hT_own, TOK)
        if stop <= 0:
            return nc
        gather(hT_own, hT_all, "a", "b")
        if stop <= 1:
            return nc
        for l in range(DEPTH):
            d = L[l]
            last = l == DEPTH - 1
            if stop <= 2 + 10 * l:
                return nc
            emit_A_lru(nc, P, hT3, d["wa"], d["wg2"], d["pa"], mix3)
            emit_A_ret(nc, P, hT3, d["wa"], d["pa"], cst, pos, mix3)
            emit_A_fox(nc, P, hT3, d["wa"], d["pa"], cst, mix3)
            if stop <= 3 + 10 * l:
                return nc
            gather8(mix_c, mixG)
            sv = {}
            for r_ in range(4):
                def sel(e, r_=r_):
                    if "s" not in sv:
                        sv["s"] = e.snap(e.partition_id() % 4)
                    return e.dma_start(out=mixT_own[r_ * 256:(r_ + 1) * 256, :].rearrange("(a p) t -> a p t", a=2),
                                       in_=mixG5[DynSlice(sv["s"], 1), :, r_, :, :].rearrange("j a p t -> (j a) p t"))
                P.op("pool", sel, [("b", q) for q in range(8)], [("mo", r_)], dma=("mo", r_))
            P.barrier()
            P.emit()
            if stop <= 4 + 10 * l:
                return nc
            emit_B1(nc, P, mixT_own, x if l == 0 else x3s, mem, gm, d["w_out"], d["w_cq"], d["w_ck"], d["w_cv"], d["w_co"],
                    d["g_post_mix"], d["g_pre_cross"], d["g_post_cross"], x2s)
            emit_B2(nc, P, x2s, d["w_gate"], d["w_up"], d["w_down"], d["g_pre_ffn"], d["g_post_ffn"],
                    None if last else d["g_next"], out if last else x3s, None if last else hT_own)
            if not last:
                gather(hT_own, hT_all, "a", "b")
    return nc


def kernel(**inp):
    inp = {k: np.asarray(v) for k, v in inp.items()}
    cores = list(range(8))
    perm = mix_perm()
    x = np.ascontiguousarray(inp["x"], dtype=np.float32)
    maps = []
    for c in cores:
        b, s = c // 4, c % 4
        m = {"x": np.ascontiguousarray(x[b, s * TOK:(s + 1) * TOK]), "mem": np.ascontiguousarray(inp["mem"][b]),
             "gm": inp["mem_norm_g"], "pos": np.ascontiguousarray(inp["positions"][b]).astype(np.int32),
             "g0": inp["pre_mix_g"][0]}
        for l in range(DEPTH):
            wa, wg2, pa, cst = a_weights(inp, l, s)
            m["cst"] = cst
            m["wa%d" % l] = wa
            m["wg2%d" % l] = wg2
            m["pa%d" % l] = pa
            m["w_out%d" % l] = np.ascontiguousarray(inp["w_out"][l][perm])
            for n in ["w_cq", "w_ck", "w_cv", "w_co", "w_gate", "w_up", "w_down"]:
                m["%s%d" % (n, l)] = np.ascontiguousarray(inp[n][l])
            m["g_post_mix%d" % l] = inp["post_mix_g"][l]
            m["g_pre_cross%d" % l] = inp["pre_cross_g"][l]
            m["g_post_cross%d" % l] = inp["post_cross_g"][l]
            m["g_pre_ffn%d" % l] = inp["pre_ffn_g"][l]
            m["g_post_ffn%d" % l] = inp["post_ffn_g"][l]
            m["g_next%d" % l] = inp["pre_mix_g"][min(l + 1, DEPTH - 1)]
        maps.append({k: np.ascontiguousarray(v) for k, v in m.items()})
    res = run_bass_kernel_spmd(build_fused(), maps, core_ids=cores)
    out = np.zeros((NB, SEQ, D), np.float32)
    for c in cores:
        out[c // 4, (c % 4) * TOK:(c % 4 + 1) * TOK] = res.results[c]["out"]
    return out
```

```python
import numpy as np
import ml_dtypes
from contextlib import ExitStack
import concourse.bass as bass
import concourse.mybir as mybir
from concourse.bass_utils import run_bass_kernel_spmd

F32 = mybir.dt.float32
BF16 = mybir.dt.bfloat16
I32 = mybir.dt.int32
AF = mybir.ActivationFunctionType
ALU = mybir.AluOpType
AX = mybir.AxisListType

D = 1024
SEQ = 16384
NB = 2
DEPTH = 2
TOK = 4096
DFF = 2816
NFF = DFF // 128
EPS = 1e-6
SPLIT = (256, 256, 512, 512, 512, 8, 256, 256, 256, 256)
OFF = np.concatenate([[0], np.cumsum(SPLIT)]).tolist()
PI = float(np.pi)


class Prog:
    ENGS = ("pe", "act", "dve", "pool", "sp")

    def __init__(self, nc, es, n_dma_sems=48):
        self.nc = nc
        self.eng_sem = {e: es.enter_context(nc.semaphore("s_" + e)) for e in self.ENGS}
        self.eng_cnt = {e: 0 for e in self.ENGS}
        self.dma_sems = [es.enter_context(nc.semaphore("d%d" % i)) for i in range(n_dma_sems)]
        self.dma_cnt = [0] * n_dma_sems
        self.dma_key2idx = {}
        self.waited = {e: {} for e in self.ENGS}
        self.cc_sem = es.enter_context(nc.semaphore("s_cc"))
        self.cc_scratch = es.enter_context(nc.sbuf_tensor("cc_scratch", [128, 8], F32))
        self.cc_cnt = 0
        self.ops = []
        self.state = {}
        self.last = {}
        self.pending_dma = []

    def dma_sem_for(self, key):
        if key not in self.dma_key2idx:
            idx = len(self.dma_key2idx)
            assert idx < len(self.dma_sems), "out of dma sems"
            self.dma_key2idx[key] = idx
        return self.dma_key2idx[key]

    def op(self, eng, fn, reads=(), writes=(), dma=None, extra=()):
        deps = set(extra)
        for k in reads:
            st = self.state.setdefault(k, {"w": None, "r": []})
            if st["w"] is not None:
                deps.add(st["w"])
        for k in writes:
            st = self.state.setdefault(k, {"w": None, "r": []})
            if st["w"] is not None:
                deps.add(st["w"])
            deps.update(st["r"])
        oid = len(self.ops)
        self.ops.append(dict(id=oid, eng=eng, fn=fn, deps=deps,
                             dma=None if dma is None else self.dma_sem_for(dma)))
        for k in reads:
            self.state[k]["r"].append(oid)
        for k in writes:
            self.state[k] = {"w": oid, "r": []}
        if dma is None:
            self.last[eng] = oid
        else:
            self.pending_dma.append(oid)
        return oid

    def dma(self, q, out, in_, reads, writes, semkey, **kw):
        return self.op(q, lambda e: e.dma_start(out=out, in_=in_, **kw), reads, writes, dma=semkey)

    def collective(self, kind, in_ap, out_ap, groups, reads, writes):
        def fn(e):
            return e.collective_compute(kind, ALU.bypass, replica_groups=groups, ins=[in_ap.opt()], outs=[out_ap.opt()])
        oid = self.op("pool", fn, reads, writes)
        self.ops[oid]["cc"] = True
        return oid

    def cc_follow(self, keys):
        scr = self.cc_scratch
        self.op("pool", lambda e: e.memset(scr[:], 0.0), list(keys), list(keys))

    def barrier(self):
        ids = list(self.last.values()) + list(self.pending_dma)
        for e in self.ENGS:
            self.op(e, lambda en: None, extra=ids)
        self.pending_dma = []
        self.state = {}

    def emit(self):
        ops = self.ops

        def pe_pe(a, b):
            return a["eng"] == "pe" and b["eng"] == "pe" and a["dma"] is None and b["dma"] is None

        needed = set()
        for o in ops:
            for d in o["deps"]:
                if not pe_pe(ops[d], o):
                    needed.add(d)
        for o in ops:
            if o.get("cc"):
                self.cc_cnt += 1
                o["sig"] = (self.cc_sem, self.cc_cnt, None)
            elif o["dma"] is not None:
                self.dma_cnt[o["dma"]] += 16
                o["sig"] = (self.dma_sems[o["dma"]], self.dma_cnt[o["dma"]], 16)
            elif o["id"] in needed:
                self.eng_cnt[o["eng"]] += 1
                o["sig"] = (self.eng_sem[o["eng"]], self.eng_cnt[o["eng"]], 1)
            else:
                o["sig"] = None
        per = {e: [] for e in self.ENGS}
        carry = {e: [] for e in self.ENGS}
        for o in ops:
            w = {}
            for d in o["deps"]:
                if pe_pe(ops[d], o):
                    continue
                sg = ops[d]["sig"]
                k = id(sg[0])
                if k not in w or w[k][1] < sg[1]:
                    w[k] = (sg[0], sg[1])
            wl = carry[o["eng"]]
            carry[o["eng"]] = []
            wd = self.waited[o["eng"]]
            for k, (s, v) in w.items():
                if wd.get(k, 0) >= v:
                    continue
                wd[k] = v
                wl.append((s, v))
            o["waits"] = wl
            per[o["eng"]].append(o)

        def replay(lst):
            def f(e):
                pend_sig = None
                for o in lst:
                    for (s, v) in o["waits"]:
                        e.wait_ge(s, v)
                    ins = o["fn"](e)
                    if o["sig"] is not None:
                        assert ins is not None, "signalling op must emit an instruction"
                        if o["sig"][2] is None:
                            ins.then_inc(o["sig"][0])
                        else:
                            ins.then_inc(o["sig"][0], o["sig"][2])
            return f

        with self.nc.Block() as block:
            if per["pe"]:
                block.tensor(replay(per["pe"]))
            if per["act"]:
                block.scalar(replay(per["act"]))
            if per["dve"]:
                block.vector(replay(per["dve"]))
            if per["pool"]:
                block.gpsimd(replay(per["pool"]))
            if per["sp"]:
                block.sync(replay(per["sp"]))
        n = {e: len(per[e]) for e in self.ENGS}
        self.ops = []
        self.state = {}
        self.last = {}
        self.pending_dma = []
        return n


class Ctx:
    def __init__(self, nc, es, P):
        self.nc, self.es, self.P = nc, es, P
        self.n = 0

    UID = [0]

    def sb(self, name, shape, dt=F32):
        Ctx.UID[0] += 1
        return self.es.enter_context(self.nc.sbuf_tensor("sb%d_%s" % (Ctx.UID[0], name), shape, dt))

    def ps(self, name, shape, dt=F32):
        Ctx.UID[0] += 1
        return self.es.enter_context(self.nc.psum_tensor("ps%d_%s" % (Ctx.UID[0], name), shape, dt))


def make_ident(P, ident, key="ident"):
    P.op("pool", lambda e: e.memset(ident[:], 1.0), [], [key])

    def sel(e):
        if getattr(P, "zero_reg", None) is None:
            P.zero_reg = e.to_reg(0.0)
        return e.affine_select(out=ident[:], in_=ident[:], pattern=[[-1, 128]], compare_op=ALU.is_equal,
                               fill=P.zero_reg, base=0, channel_multiplier=1)
    P.op("pool", sel, [key], [key])


def load_bcast_rows(P, C, name, src_row_ap, n):
    t = C.sb(name, [128, n])
    P.dma("sp", t[:], src_row_ap.partition_broadcast(128), [], [name], name)
    return t


def load_weight_bf16(P, C, w_ap, K, N, dst, dkey, stg, eng_cast="pool"):
    wv = w_ap.rearrange("(c p) n -> p c n", p=128)
    nk = K // 128
    i = 0
    for c in range(nk):
        for n0 in range(0, N, 1024):
            n1 = min(N, n0 + 1024)
            sk = ("stg", stg["i"] % 2)
            st = stg["t"][stg["i"] % 2]
            stg["i"] += 1
            P.dma("sp", st[:, 0:n1 - n0], wv[:, c, n0:n1], [], [sk], sk)
            if stg["i"] % 2 == 0:
                P.op("dve", lambda e, st=st, c=c, n0=n0, n1=n1: e.tensor_copy(out=dst[:, c, n0:n1], in_=st[:, 0:n1 - n0]),
                     [sk], [dkey])
            else:
                P.op("act", lambda e, st=st, c=c, n0=n0, n1=n1: e.copy(out=dst[:, c, n0:n1], in_=st[:, 0:n1 - n0]),
                     [sk], [dkey])


def rstd_of(P, C, src, skey, tag, junk, n=D):
    ss = C.tmp["ss"]
    P.op("act", lambda e: e.activation(out=junk[:, 0:n], in_=src, func=AF.Square, accum_out=ss[:, 0:1]),
         [skey], ["junk", "ss"])
    P.op("dve", lambda e: e.tensor_scalar(out=ss[:, 1:2], in0=ss[:, 0:1], scalar1=1.0 / n, scalar2=EPS,
                                          op0=ALU.mult, op1=ALU.add), ["ss"], ["ss1"])
    P.op("act", lambda e: e.activation(out=ss[:, 2:3], in_=ss[:, 1:2], func=AF.Sqrt), ["ss1"], ["ss2"])
    P.op("dve", lambda e: e.reciprocal(out=ss[:, 3:4], in_=ss[:, 2:3]), ["ss2"], ["ss3"])
    return ss[:, 3:4], "ss3"


def norm_T(P, C, src, skey, g_t, gkey, hbf, ident, trp, trkey, dstT, dkey, col0):
    r, rk = rstd_of(P, C, src, skey, "n", C.tmp["junk"])
    P.op("dve", lambda e: e.scalar_tensor_tensor(out=hbf[:], in0=src, scalar=r, in1=g_t[:],
                                                 op0=ALU.mult, op1=ALU.mult), [skey, rk, gkey], ["hbf"])
    for c in range(8):
        P.op("pe", lambda e, c=c: e.transpose(trp[:, c * 128:(c + 1) * 128], hbf[:, c * 128:(c + 1) * 128], ident[:]),
             ["hbf", "ident"], [trkey])
    P.op("act", lambda e: e.copy(out=dstT[:, :, col0:col0 + 128],
                                 in_=trp[:].rearrange("p (c t) -> p c t", c=8)), [trkey], [dkey])


def emit_P0(nc, P, x_ap, g_ap, hT_ap, ntok):
    with ExitStack() as es:
        C = Ctx(nc, es, P)
        C.tmp = {"ss": C.sb("ss", [128, 4]), "junk": C.sb("junk", [128, D], BF16)}
        ident = C.sb("ident", [128, 128], BF16)
        make_ident(P, ident)
        g_t = load_bcast_rows(P, C, "g0", g_ap, D)
        hbf = C.sb("hbf", [128, D], BF16)
        xs = [C.sb("x%d" % i, [128, D]) for i in range(2)]
        trp = [C.ps("tr%d" % i, [128, D], BF16) for i in range(2)]
        hT = [C.sb("hT%d" % i, [128, 8, 512], BF16) for i in range(2)]
        hTv = hT_ap.rearrange("(c p) t -> p c t", p=128)
        for st in range(ntok // 512):
            hk = ("hT", st % 2)
            for sub in range(4):
                i = st * 4 + sub
                xk = ("x", i % 2)
                P.dma("sp", xs[i % 2][:], x_ap[i * 128:(i + 1) * 128, :], [], [xk], xk)
                norm_T(P, C, xs[i % 2][:], xk, g_t, "g0", hbf, ident, trp[i % 2], ("tr", i % 2),
                       hT[st % 2], hk, sub * 128)
            P.dma("sp", hTv[:, :, st * 512:(st + 1) * 512], hT[st % 2][:], [hk], ["hT_out"], hk)
        P.barrier()
        return P.emit()


def emit_B1(nc, P, mixT_ap, x_ap, mem_ap, gm_ap, w_out, w_cq, w_ck, w_cv, w_co,
            g_post_mix, g_pre_cross, g_post_cross, x2_ap, mix_fn=None):
    with ExitStack() as es:
        C = Ctx(nc, es, P)
        C.tmp = {"ss": C.sb("ss", [128, 4]), "junk": C.sb("junk", [128, D], BF16)}
        ident = C.sb("ident", [128, 128], BF16)
        make_ident(P, ident)
        ones_bf = C.sb("ones_bf", [128, 128], BF16)
        P.op("pool", lambda e: e.memset(ones_bf[:], 1.0), [], ["ones_bf"])
        stg = {"i": 0, "t": [C.sb("stg%d" % i, [128, 1024]) for i in range(2)]}
        gpm = load_bcast_rows(P, C, "gpm", g_post_mix, D)
        gpc = load_bcast_rows(P, C, "gpc", g_pre_cross, D)
        gqc = load_bcast_rows(P, C, "gqc", g_post_cross, D)
        gmm = load_bcast_rows(P, C, "gmm", gm_ap, D)
        wo = C.sb("wo", [128, 8, D], BF16)
        wq = C.sb("wq", [128, 8, D], BF16)
        wc = C.sb("wc", [128, 8, D], BF16)
        wk = C.sb("wk", [128, 8, D], BF16)
        load_weight_bf16(P, C, w_ck, D, D, wk, "wk", stg)
        hbf = C.sb("hbf", [128, D], BF16)
        acc = [C.ps("acc%d" % i, [128, D]) for i in range(2)]
        trp = [C.ps("tr%d" % i, [128, D], BF16) for i in range(2)]
        mb = [C.ps("mb%d" % i, [128, 512]) for i in range(2)]
        memT = C.sb("memT", [128, 8, 256], BF16)
        xs = [C.sb("x%d" % i, [128, D]) for i in range(4)]
        for i in range(2):
            xk = ("x", i)
            P.dma("sp", xs[i][:], mem_ap[i * 128:(i + 1) * 128, :], [], [xk], xk)
            norm_T(P, C, xs[i][:], xk, gmm, "gmm", hbf, ident, trp[i], ("tr", i), memT, "memT", i * 128)
        kT = C.sb("kT", [128, 8, 256], BF16)
        for cc in range(8):
            for f in range(8):
                P.op("pe", lambda e, cc=cc, f=f: e.matmul(mb[cc % 2][:, 0:256], lhsT=wk[:, f, cc * 128:(cc + 1) * 128],
                                                           rhs=memT[:, f, :], start=(f == 0), stop=(f == 7)),
                     ["wk", "memT"], [("mb", cc % 2)])
            P.op("act", lambda e, cc=cc: e.copy(out=kT[:, cc, :], in_=mb[cc % 2][:, 0:256]), [("mb", cc % 2)], ["kT"])
        load_weight_bf16(P, C, w_cv, D, D, wk, "wk", stg)
        vm = C.sb("vm", [128, 2, D], BF16)
        for m in range(2):
            for half in range(2):
                for f in range(8):
                    P.op("pe", lambda e, m=m, half=half, f=f: e.matmul(
                        acc[m][:, half * 512:(half + 1) * 512], lhsT=memT[:, f, m * 128:(m + 1) * 128],
                        rhs=wk[:, f, half * 512:(half + 1) * 512], start=(f == 0), stop=(f == 7)),
                        ["wk", "memT"], [("acc", m)])
            P.op("act", lambda e, m=m: e.copy(out=vm[:, m, :], in_=acc[m][:]), [("acc", m)], ["vm"])
        load_weight_bf16(P, C, w_out, D, D, wo, "wo", stg)
        load_weight_bf16(P, C, w_cq, D, D, wq, "wq", stg)
        load_weight_bf16(P, C, w_co, D, D, wc, "wc", stg)
        mixT = [C.sb("mixT%d" % i, [128, 8, 512], BF16) for i in range(2)]
        h2T = C.sb("h2T", [128, 8, 512], BF16)
        qT = C.sb("qT", [128, 8, 512], BF16)
        PT = C.sb("PT", [128, 8, 512], BF16)
        oT = C.sb("oT", [128, 8, 512], BF16)
        rec = C.sb("rec", [128, 512])
        tmp = C.sb("tmp", [128, D])
        mix_q = "pool"
        if mix_fn is None:
            mix_q = "sp"
            mixv = mixT_ap.rearrange("(c p) t -> p c t", p=128)
            mix_fn = lambda e, st, r: mixv[:, 2 * r:2 * r + 2, st * 512:(st + 1) * 512]
        nst = TOK // 512
        for st in range(nst):
            mk = ("mixT", st % 2)
            for r_ in range(4):
                mkr = ("mixT", st % 2, r_)
                P.op(mix_q, lambda e, st=st, r_=r_: e.dma_start(out=mixT[st % 2][:, 2 * r_:2 * r_ + 2, :], in_=mix_fn(e, st, r_)), [], [mkr], dma=mkr)
            for sub in range(4):
                t0 = st * 512 + sub * 128
                xk = ("x", sub)
                ak = ("acc", sub % 2)
                a = acc[sub % 2]
                P.dma("sp", xs[sub][:], x_ap[t0:t0 + 128, :], [], [xk], xk)
                for half in range(2):
                    for c in range(8):
                        P.op("pe", lambda e, a=a, half=half, c=c, sub=sub, st=st: e.matmul(
                            a[:, half * 512:(half + 1) * 512], lhsT=mixT[st % 2][:, c, sub * 128:(sub + 1) * 128],
                            rhs=wo[:, c, half * 512:(half + 1) * 512], start=(c == 0), stop=(c == 7)),
                            [("mixT", st % 2, c // 2), "wo"], [ak])
                r, rk = rstd_of(P, C, a[:], ak, "y", C.tmp["junk"])
                P.op("dve", lambda e, a=a, r=r: e.scalar_tensor_tensor(out=tmp[:], in0=a[:], scalar=r, in1=gpm[:],
                                                                       op0=ALU.mult, op1=ALU.mult), [ak, rk, "gpm"], ["tmp"])
                P.op("pool", lambda e, sub=sub: e.tensor_tensor(out=xs[sub][:], in0=xs[sub][:], in1=tmp[:], op=ALU.add),
                     [xk, "tmp"], [xk])
                norm_T(P, C, xs[sub][:], xk, gpc, "gpc", hbf, ident, trp[sub % 2], ("tr", sub % 2), h2T, "h2T", sub * 128)
            for cc in range(8):
                for f in range(8):
                    P.op("pe", lambda e, cc=cc, f=f: e.matmul(mb[cc % 2][:], lhsT=wq[:, f, cc * 128:(cc + 1) * 128],
                                                               rhs=h2T[:, f, :], start=(f == 0), stop=(f == 7)),
                         ["wq", "h2T"], [("mb", cc % 2)])
                if cc % 2 == 0:
                    P.op("act", lambda e, cc=cc: e.copy(out=qT[:, cc, :], in_=mb[cc % 2][:]), [("mb", cc % 2)], [("qT", cc)])
                else:
                    P.op("dve", lambda e, cc=cc: e.tensor_copy(out=qT[:, cc, :], in_=mb[cc % 2][:]), [("mb", cc % 2)], [("qT", cc)])
            for h in range(4):
                for m in range(2):
                    bk = ("mb", m)
                    for dc in range(2):
                        P.op("pe", lambda e, h=h, m=m, dc=dc: e.matmul(
                            mb[m][:], lhsT=kT[:, 2 * h + dc, m * 128:(m + 1) * 128], rhs=qT[:, 2 * h + dc, :],
                            start=(dc == 0), stop=(dc == 1)), ["kT", ("qT", 2 * h + dc)], [bk])
                    P.op("act", lambda e, h=h, m=m: e.activation(out=PT[:, 2 * h + m, :], in_=mb[m][:], func=AF.Exp,
                                                                 scale=1.0 / 16.0), [bk], [("PT", 2 * h + m)])
                for m in range(2):
                    P.op("pe", lambda e, h=h, m=m: e.matmul(acc[0][:, 0:512], lhsT=ones_bf[:], rhs=PT[:, 2 * h + m, :],
                                                            start=(m == 0), stop=(m == 1)),
                         ["ones_bf", ("PT", 2 * h + m)], [("acc", 0)])
                P.op("dve", lambda e: e.reciprocal(out=rec[:], in_=acc[0][:, 0:512]), [("acc", 0)], ["rec"])
                for dc in range(2):
                    for m in range(2):
                        P.op("pe", lambda e, h=h, m=m, dc=dc: e.matmul(
                            acc[1][:, dc * 512:(dc + 1) * 512], lhsT=vm[:, m, (2 * h + dc) * 128:(2 * h + dc + 1) * 128],
                            rhs=PT[:, 2 * h + m, :], start=(m == 0), stop=(m == 1)),
                            ["vm", ("PT", 2 * h + m)], [("acc", 1)])
                for dc in range(2):
                    P.op("dve", lambda e, h=h, dc=dc: e.tensor_tensor(out=oT[:, 2 * h + dc, :],
                                                                      in0=acc[1][:, dc * 512:(dc + 1) * 512], in1=rec[:],
                                                                      op=ALU.mult), [("acc", 1), "rec"], [("oT", 2 * h + dc)])
            for sub in range(4):
                t0 = st * 512 + sub * 128
                xk = ("x", sub)
                ak = ("acc", sub % 2)
                a = acc[sub % 2]
                for half in range(2):
                    for c in range(8):
                        P.op("pe", lambda e, a=a, half=half, c=c, sub=sub: e.matmul(
                            a[:, half * 512:(half + 1) * 512], lhsT=oT[:, c, sub * 128:(sub + 1) * 128],
                            rhs=wc[:, c, half * 512:(half + 1) * 512], start=(c == 0), stop=(c == 7)),
                            [("oT", c), "wc"], [ak])
                r, rk = rstd_of(P, C, a[:], ak, "y", C.tmp["junk"])
                P.op("dve", lambda e, a=a, r=r: e.scalar_tensor_tensor(out=tmp[:], in0=a[:], scalar=r, in1=gqc[:],
                                                                       op0=ALU.mult, op1=ALU.mult), [ak, rk, "gqc"], ["tmp"])
                P.op("pool", lambda e, sub=sub: e.tensor_tensor(out=xs[sub][:], in0=xs[sub][:], in1=tmp[:], op=ALU.add),
                     [xk, "tmp"], [xk])
                P.dma("sp", x2_ap[t0:t0 + 128, :], xs[sub][:], [xk], ["x2_out"], xk)
        P.barrier()
        return P.emit()


def emit_B2(nc, P, x2_ap, w_gate, w_up, w_down, g_pre_ffn, g_post_ffn, g_next, x3_ap, hT_ap):
    ST = 512
    with ExitStack() as es:
        C = Ctx(nc, es, P)
        C.tmp = {"ss": C.sb("ss", [128, 4]), "junk": C.sb("junk", [128, D], BF16)}
        ident = C.sb("ident", [128, 128], BF16)
        make_ident(P, ident)
        stg = {"i": 0, "t": [C.sb("stg%d" % i, [128, 1024]) for i in range(2)]}
        gpf = load_bcast_rows(P, C, "gpf", g_pre_ffn, D)
        gqf = load_bcast_rows(P, C, "gqf", g_post_ffn, D)
        gnx = load_bcast_rows(P, C, "gnx", g_next, D) if g_next is not None else None
        wg = C.sb("wg", [128, 8, DFF], BF16)
        wu = C.sb("wu", [128, 8, DFF], BF16)
        wd = C.sb("wd", [128, NFF, D], BF16)
        load_weight_bf16(P, C, w_gate, D, DFF, wg, "wg", stg)
        load_weight_bf16(P, C, w_up, D, DFF, wu, "wu", stg)
        load_weight_bf16(P, C, w_down, DFF, D, wd, "wd", stg)
        hbf = C.sb("hbf", [128, D], BF16)
        acc = [C.ps("acc%d" % i, [128, D]) for i in range(2)]
        trp = [C.ps("tr%d" % i, [128, D], BF16) for i in range(2)]
        mb = [C.ps("mb%d" % i, [128, 512]) for i in range(2)]
        xs = [C.sb("x%d" % i, [128, D]) for i in range(2)]
        h3T = C.sb("h3T", [128, 8, ST], BF16)
        aT = C.sb("aT", [128, NFF, ST], BF16)
        sg = C.sb("sg", [128, ST])
        tmp = C.sb("tmp", [128, D])
        nsub = ST // 128
        hTv = hT_ap.rearrange("(c p) t -> p c t", p=128) if hT_ap is not None else None
        xi = 0
        for st in range(TOK // ST):
            for sub in range(nsub):
                t0 = st * ST + sub * 128
                xk = ("x", xi % 2)
                xt = xs[xi % 2]
                P.dma("sp", xt[:], x2_ap[t0:t0 + 128, :], [], [xk], xk)
                norm_T(P, C, xt[:], xk, gpf, "gpf", hbf, ident, trp[xi % 2], ("tr", xi % 2), h3T, "h3T", sub * 128)
                xi += 1
            for fc in range(NFF):
                for f in range(8):
                    P.op("pe", lambda e, fc=fc, f=f: e.matmul(mb[0][:, 0:ST], lhsT=wg[:, f, fc * 128:(fc + 1) * 128],
                                                               rhs=h3T[:, f, :], start=(f == 0), stop=(f == 7)),
                         ["wg", "h3T"], [("mb", 0)])
                for f in range(8):
                    P.op("pe", lambda e, fc=fc, f=f: e.matmul(mb[1][:, 0:ST], lhsT=wu[:, f, fc * 128:(fc + 1) * 128],
                                                               rhs=h3T[:, f, :], start=(f == 0), stop=(f == 7)),
                         ["wu", "h3T"], [("mb", 1)])
                P.op("act", lambda e: e.activation(out=sg[:], in_=mb[0][:, 0:ST], func=AF.Silu), [("mb", 0)], ["sg"])
                P.op("dve", lambda e, fc=fc: e.tensor_tensor(out=aT[:, fc, :], in0=mb[1][:, 0:ST], in1=sg[:], op=ALU.mult),
                     [("mb", 1), "sg"], [("aT", fc)])
            for sub in range(nsub):
                t0 = st * ST + sub * 128
                xk = ("x", xi % 2)
                xt = xs[xi % 2]
                ak = ("acc", sub % 2)
                a = acc[sub % 2]
                P.dma("sp", xt[:], x2_ap[t0:t0 + 128, :], [], [xk], xk)
                for half in range(2):
                    for fc in range(NFF):
                        P.op("pe", lambda e, a=a, half=half, fc=fc, sub=sub: e.matmul(
                            a[:, half * 512:(half + 1) * 512], lhsT=aT[:, fc, sub * 128:(sub + 1) * 128],
                            rhs=wd[:, fc, half * 512:(half + 1) * 512], start=(fc == 0), stop=(fc == NFF - 1)),
                            [("aT", fc), "wd"], [ak])
                r, rk = rstd_of(P, C, a[:], ak, "y", C.tmp["junk"])
                P.op("dve", lambda e, a=a, r=r: e.scalar_tensor_tensor(out=tmp[:], in0=a[:], scalar=r, in1=gqf[:],
                                                                       op0=ALU.mult, op1=ALU.mult), [ak, rk, "gqf"], ["tmp"])
                P.op("pool", lambda e, xt=xt: e.tensor_tensor(out=xt[:], in0=xt[:], in1=tmp[:], op=ALU.add),
                     [xk, "tmp"], [xk])
                P.dma("sp", x3_ap[t0:t0 + 128, :], xt[:], [xk], ["x3_out"], xk)
                if gnx is not None:
                    norm_T(P, C, xt[:], xk, gnx, "gnx", hbf, ident, trp[xi % 2], ("tr", xi % 2), h3T, "h3T", sub * 128)
                xi += 1
            if gnx is not None:
                P.dma("sp", hTv[:, :, st * ST:(st + 1) * ST], h3T[:], ["h3T"], ["hT_out"], "h3T")
        P.barrier()
        return P.emit()


NCH = SEQ // 512


def hT_chunk(hT_ap, ci):
    return hT_ap[:, ci // 8, :, (ci % 8) * 512:(ci % 8 + 1) * 512].rearrange("c p t -> p c t")


def emit_A_lru(nc, P, hT_ap, wa, wg2, pa, mix_ap):
    with ExitStack() as es:
        C = Ctx(nc, es, P)
        stg = {"i": 0, "t": [C.sb("stg%d" % i, [128, 1024]) for i in range(2)]}
        pat = C.sb("pat", [128, 16])
        P.dma("sp", pat[:], pa, [], ["pat"], "pat")
        wl = C.sb("wl", [128, 8, 128], BF16)
        load_weight_bf16(P, C, wa[:, 0:128], D, 128, wl, "wl", stg)
        wgs = C.sb("wgs", [64, 128])
        wgb = C.sb("wgb", [64, 128], BF16)
        P.dma("sp", wgs[:], wg2, [], ["wgs"], "wgs")
        P.op("pool", lambda e: e.tensor_copy(out=wgb[:], in_=wgs[:]), ["wgs"], ["wgb"])
        kap = C.sb("kap", [64, 4])
        P.op("act", lambda e: e.activation(out=kap[:, 0:1], in_=pat[0:64, 7:8], func=AF.Exp, scale=-1.0), ["pat"], ["kap0"])
        P.op("act", lambda e: e.activation(out=kap[:, 1:2], in_=kap[:, 0:1], func=AF.Ln, bias=1.0), ["kap0"], ["kap1"])
        P.op("dve", lambda e: e.tensor_scalar(out=kap[:, 2:3], in0=kap[:, 1:2], scalar1=-8.0, scalar2=None, op0=ALU.mult), ["kap1"], ["kap2"])
        P.op("dve", lambda e: e.tensor_scalar(out=kap[:, 3:4], in0=kap[:, 1:2], scalar1=-16.0, scalar2=None, op0=ALU.mult), ["kap1"], ["kap3"])
        hc = [C.sb("hc%d" % i, [128, 8, 512], BF16) for i in range(2)]
        pm2 = [[C.ps("pm%d%d" % (p_, i), [128, 512]) for i in range(2)] for p_ in range(2)]
        pg2 = [[C.ps("pg%d%d" % (p_, i), [128, 512]) for i in range(2)] for p_ in range(2)]
        lxb2 = [C.sb("lxb%d" % p_, [64, 515]) for p_ in range(2)]
        P.op("pool", lambda e: e.memset(lxb2[0][:, 0:3], 0.0), [], [("lxt", 0)])
        names = ["xc", "sr", "si", "a", "a2", "sq", "ix", "u", "gl"]
        f32t2 = [{n: C.sb(n + str(p_), [64, 512]) for n in names} for p_ in range(2)]
        xcb2 = [C.sb("xcb%d" % p_, [64, 512], BF16) for p_ in range(2)]
        hb = [C.sb("hb%d" % i, [64, 512]) for i in range(2)]
        ob = [C.sb("ob%d" % i, [64, 512], BF16) for i in range(2)]

        def stages(ci):
            par = ci % 2
            T = f32t2[par]
            xcb = xcb2[par]
            pm = pm2[par]
            pg = pg2[par]
            lxb = lxb2[par]
            K_ = lambda n: (n, par)
            hk = ("hc", par)

            def s0():
                P.dma("sp", hc[par][:], hT_chunk(hT_ap, ci), [], [hk], hk)
                for g in range(2):
                    for f in range(8):
                        P.op("pe", lambda e, g=g, f=f: e.matmul(pm[g][0:64, :], lhsT=wl[:, f, g * 64:(g + 1) * 64], rhs=hc[par][:, f, :],
                                                                 start=(f == 0), stop=(f == 7)), ["wl", hk], [("pm", par, g)])
                P.op("act", lambda e: e.copy(out=lxb[:, 3:515], in_=pm[0][0:64, :]), [("pm", par, 0)], [("lxm", par)])
                if ci > 0:
                    P.op("dve", lambda e: e.tensor_copy(out=lxb[:, 0:3], in_=lxb2[1 - par][:, 512:515]), [("lxm", 1 - par)], [("lxt", par)])

            def s1():
                rk = [("lxm", par), ("lxt", par), "pat"]
                P.op("dve", lambda e: e.tensor_scalar(out=T["xc"][:], in0=lxb[:, 3:515], scalar1=pat[0:64, 3:4], scalar2=pat[0:64, 4:5],
                                                      op0=ALU.mult, op1=ALU.add), rk, [K_("xc")])
                for k in (2, 1, 0):
                    P.op("dve", lambda e, k=k: e.scalar_tensor_tensor(out=T["xc"][:], in0=lxb[:, k:k + 512], scalar=pat[0:64, k:k + 1],
                                                                      in1=T["xc"][:], op0=ALU.mult, op1=ALU.add), rk + [K_("xc")], [K_("xc")])
                P.op("pool", lambda e: e.tensor_copy(out=xcb[:], in_=T["xc"][:]), [K_("xc")], [K_("xcb")])
                for g in range(2):
                    P.op("pe", lambda e, g=g: e.matmul(pg[g][0:64, :], lhsT=wgb[:, g * 64:(g + 1) * 64], rhs=xcb[:], start=True, stop=True),
                         ["wgb", K_("xcb")], [("pg", par, g)])

            def s2():
                P.op("act", lambda e: e.activation(out=T["sr"][:], in_=pg[0][0:64, :], func=AF.Sigmoid, bias=pat[0:64, 5:6]), [("pg", par, 0), "pat"], [K_("sr")])
                P.op("act", lambda e: e.activation(out=T["si"][:], in_=pg[1][0:64, :], func=AF.Sigmoid, bias=pat[0:64, 6:7]), [("pg", par, 1), "pat"], [K_("si")])
                P.op("pool", lambda e: e.tensor_tensor(out=T["ix"][:], in0=T["si"][:], in1=T["xc"][:], op=ALU.mult), [K_("si"), K_("xc")], [K_("ix")])

            def s3():
                P.op("act", lambda e: e.activation(out=T["a"][:], in_=T["sr"][:], func=AF.Exp, scale=kap[:, 2:3]), [K_("sr"), "kap2"], [K_("a")])
                P.op("act", lambda e: e.activation(out=T["a2"][:], in_=T["sr"][:], func=AF.Exp, scale=kap[:, 3:4]), [K_("sr"), "kap3"], [K_("a2")])
                P.op("dve", lambda e: e.tensor_scalar(out=T["a2"][:], in0=T["a2"][:], scalar1=-1.0, scalar2=1.0, op0=ALU.mult, op1=ALU.add), [K_("a2")], [K_("a2")])

            def s4():
                P.op("act", lambda e: e.activation(out=T["sq"][:], in_=T["a2"][:], func=AF.Sqrt), [K_("a2")], [K_("sq")])
                P.op("dve", lambda e: e.tensor_tensor(out=T["u"][:], in0=T["sq"][:], in1=T["ix"][:], op=ALU.mult), [K_("sq"), K_("ix")], [K_("u")])
                init = 0.0 if ci == 0 else hb[1 - par][:, 511:512]
                P.op("dve", lambda e: e.tensor_tensor_scan(out=hb[par][:], data0=T["a"][:], data1=T["u"][:], initial=init,
                                                           op0=ALU.mult, op1=ALU.add), [K_("a"), K_("u"), ("hb", 1 - par)], [("hb", par)])

            def s5():
                P.op("act", lambda e: e.activation(out=T["gl"][:], in_=pm[1][0:64, :], func=AF.Gelu_apprx_tanh), [("pm", par, 1)], [K_("gl")])
                P.op("dve", lambda e: e.tensor_tensor(out=ob[par][:], in0=hb[par][:], in1=T["gl"][:], op=ALU.mult), [("hb", par), K_("gl")], [("ob", par)])
                ok = ("ob", par)
                P.dma("sp", mix_ap[ci // 8, 0:64, (ci % 8) * 512:(ci % 8 + 1) * 512], ob[par][:], [ok], ["mix_out"], ok)

            return [s0, s1, s2, s3, s4, s5]

        for p_ in range(NCH // 2):
            sa = stages(2 * p_)
            sb_ = stages(2 * p_ + 1)
            for k in range(len(sa)):
                sa[k]()
                sb_[k]()
        P.barrier()
        return P.emit()


C1_2PI = 6.28125
C2_2PI = float(2.0 * np.pi - 6.28125)
PI_SAFE = 3.1415925


def emit_A_ret(nc, P, hT_ap, wa, pa, cst, pos_ap, mix_ap):
    import os
    STAGE = float(os.environ.get("RET_STAGE", "9"))
    with ExitStack() as es:
        C = Ctx(nc, es, P)
        stg = {"i": 0, "t": [C.sb("stg%d" % i, [128, 1024]) for i in range(2)]}
        pat = C.sb("pat", [128, 16])
        P.dma("sp", pat[:], pa, [], ["pat"], "pat")
        cs = C.sb("cs", [128, 416])
        P.dma("sp", cs[:], cst, [], ["cs"], "cs")
        decT = cs[:, 0:128]
        qwbc = cs[0:64, 128:256]
        invf = cs[:, 256:288]
        wr = C.sb("wr", [128, 8, 256], BF16)
        load_weight_bf16(P, C, wa[:, 514:770], D, 256, wr, "wr", stg)
        identb = C.sb("identb", [128, 128], BF16)
        make_ident(P, identb, "identb")
        identf = C.sb("identf", [128, 128])
        make_ident(P, identf, "identf")
        Tps = C.ps("T", [128, 4, 256])
        trq_ = C.ps("trq", [128, 1024], BF16)
        trq = trq_[0:64, 0:256]
        scp_ = C.ps("scp", [128, 512])
        scp = scp_[:, 0:128]
        Yp_ = C.ps("Yp", [128, 512])
        Yp = Yp_[:, 0:256]
        Up_ = C.ps("Up", [128, 512])
        Up = Up_[0:64, 0:64]
        tro_ = C.ps("tro", [128, 1024], BF16)
        tro = tro_[0:64, 0:512]
        posi = C.sb("posi", [128, 128], I32)
        posf = C.sb("posf", [128, 128])
        post = C.sb("post", [128, 128])
        P.dma("sp", posi[:], pos_ap.rearrange("(n p) -> n p", p=128), [], ["posi"], "posi")
        P.op("dve", lambda e: e.tensor_copy(out=posf[:], in_=posi[:]), ["posi"], ["posf"])
        P.op("pe", lambda e: e.matmul(scp, lhsT=posf[:], rhs=identf[:], start=True, stop=True), ["posf", "identf"], ["scp"])
        P.op("act", lambda e: e.copy(out=post[:], in_=scp), ["scp"], ["post"])
        Sf = C.sb("Sf", [64, 64])
        Sb = C.sb("Sb", [64, 64], BF16)
        P.op("pool", lambda e: e.memset(Sf[:], 0.0), [], ["Sf"])
        P.op("pool", lambda e: e.memset(Sb[:], 0.0), [], ["Sb"])
        hc = [C.sb("hc%d" % i, [128, 8, 512], BF16) for i in range(2)]
        ang = C.sb("ang", [128, 4, 32])
        yy = C.sb("yy", [128, 4, 32])
        yi = C.sb("yi", [128, 4, 32], I32)
        rr = C.sb("rr", [128, 4, 32])
        ar = C.sb("ar", [128, 4, 32])
        sin2 = C.sb("sin2", [128, 4, 2, 32])
        cos2 = C.sb("cos2", [128, 4, 2, 32])
        t1 = C.sb("t1", [128, 2, 32])
        t2 = C.sb("t2", [128, 2, 32])
        t3 = C.sb("t3", [128, 2, 32])
        t4 = C.sb("t4", [128, 2, 32])
        rot = C.sb("rot", [128, 2, 2, 32])
        rot2 = C.sb("rot2", [128, 128])
        qkf = C.sb("qkf", [128, 128])
        qkb = C.sb("qkb", [128, 128], BF16)
        kwb = C.sb("kwb", [128, 64], BF16)
        vb = C.sb("vb", [128, 64], BF16)
        qkT = C.sb("qkT", [64, 256], BF16)
        qwT = C.sb("qwT", [64, 128], BF16)
        sm = C.sb("sm", [128, 128], BF16)
        scf = C.sb("scf", [128, 128])
        qf32 = C.sb("qf32", [64, 128])
        sgl = C.sb("sgl", [128, 4, 64])
        st6 = C.sb("st6", [128, 4, 6])
        mv = C.sb("mv", [128, 4, 2])
        ve = C.sb("ve", [128, 4])
        yn = C.sb("yn", [128, 4, 64])
        obf = C.sb("obf", [128, 4, 64], BF16)
        oT = [C.sb("oT%d" % i, [64, 512], BF16) for i in range(2)]
        for ci in range(NCH):
            par = ci % 2
            hk = ("hc", par)
            P.dma("sp", hc[par][:], hT_chunk(hT_ap, ci), [], [hk], hk)
            for sub in range(4):
                for f in range(8):
                    P.op("pe", lambda e, sub=sub, f=f, par=par: e.matmul(Tps[:, sub, :], lhsT=hc[par][:, f, sub * 128:(sub + 1) * 128],
                                                                          rhs=wr[:, f, :], start=(f == 0), stop=(f == 7)),
                         ["wr", hk], [("T", sub // 2)])
                n = 4 * ci + sub
                P.op("dve", lambda e, sub=sub, n=n: e.tensor_scalar(out=ang[:, sub, :], in0=invf, scalar1=post[:, n:n + 1], scalar2=None,
                                                                    op0=ALU.mult), ["cs", "post"], ["ang"])
            if STAGE < 2:
                continue
            P.op("dve", lambda e: e.tensor_scalar(out=yy[:], in0=ang[:], scalar1=float(1.0 / (2.0 * np.pi)), scalar2=None, op0=ALU.mult), ["ang"], ["yy"])
            P.op("dve", lambda e: e.tensor_copy(out=yi[:], in_=yy[:]), ["yy"], ["yi"])
            P.op("dve", lambda e: e.tensor_copy(out=yy[:], in_=yi[:]), ["yi"], ["yy"])
            P.op("dve", lambda e: e.scalar_tensor_tensor(out=rr[:], in0=yy[:], scalar=-C1_2PI, in1=ang[:], op0=ALU.mult, op1=ALU.add), ["yy", "ang"], ["rr"])
            P.op("dve", lambda e: e.scalar_tensor_tensor(out=rr[:], in0=yy[:], scalar=-C2_2PI, in1=rr[:], op0=ALU.mult, op1=ALU.add), ["yy", "rr"], ["rr"])
            P.op("dve", lambda e: e.tensor_scalar(out=rr[:], in0=rr[:], scalar1=PI_SAFE, scalar2=-PI_SAFE, op0=ALU.min, op1=ALU.max), ["rr"], ["rr"])
            P.op("dve", lambda e: e.scalar_tensor_tensor(out=ar[:], in0=rr[:], scalar=-1.0, in1=rr[:], op0=ALU.mult, op1=ALU.max), ["rr"], ["ar"])
            P.op("dve", lambda e: e.tensor_scalar(out=ar[:], in0=ar[:], scalar1=-1.0, scalar2=float(np.pi / 2), op0=ALU.mult, op1=ALU.add), ["ar"], ["ar"])
            for k in range(2):
                P.op("act", lambda e, k=k: e.activation(out=sin2[:, :, k, :], in_=rr[:], func=AF.Sin), ["rr"], ["sin2"])
                P.op("act", lambda e, k=k: e.activation(out=cos2[:, :, k, :], in_=ar[:], func=AF.Sin), ["ar"], ["cos2"])
            if STAGE < 3:
                continue
            for sub in range(4):
                tk = ("T", sub // 2)
                P.op("act", lambda e, sub=sub: e.copy(out=qkf[:], in_=Tps[:, sub, 0:128]), [tk], ["qkf"])
                cs_ = cos2[:, sub, 0, :]
                sn_ = sin2[:, sub, 0, :]
                if STAGE < 3.05:
                    continue
                for a_ in range(2):
                    x1 = qkf[:, a_ * 64:a_ * 64 + 32]
                    x2 = qkf[:, a_ * 64 + 32:a_ * 64 + 64]
                    o1 = rot2[:, a_ * 64:a_ * 64 + 32]
                    o2 = rot2[:, a_ * 64 + 32:a_ * 64 + 64]
                    P.op("pool", lambda e, x1=x1, cs_=cs_: e.tensor_tensor(out=t1[:, 0, :], in0=x1, in1=cs_, op=ALU.mult), ["qkf", "cos2"], ["t1"])
                    P.op("pool", lambda e, x2=x2, sn_=sn_: e.tensor_tensor(out=t2[:, 0, :], in0=x2, in1=sn_, op=ALU.mult), ["qkf", "sin2"], ["t2"])
                    P.op("pool", lambda e, x1=x1, sn_=sn_: e.tensor_tensor(out=t3[:, 0, :], in0=x1, in1=sn_, op=ALU.mult), ["qkf", "sin2"], ["t3"])
                    P.op("pool", lambda e, x2=x2, cs_=cs_: e.tensor_tensor(out=t4[:, 0, :], in0=x2, in1=cs_, op=ALU.mult), ["qkf", "cos2"], ["t4"])
                    P.op("pool", lambda e, o1=o1: e.tensor_tensor(out=o1, in0=t1[:, 0, :], in1=t2[:, 0, :], op=ALU.subtract), ["t1", "t2"], ["rot0"])
                    P.op("pool", lambda e, o2=o2: e.tensor_tensor(out=o2, in0=t3[:, 0, :], in1=t4[:, 0, :], op=ALU.add), ["t3", "t4"], ["rot1"])
                if STAGE < 3.25:
                    continue
                rotf = rot2[:]
                P.op("pool", lambda e, rotf=rotf: e.tensor_copy(out=qkb[:], in_=rotf), ["rot0", "rot1"], ["qkb"])
                P.op("dve", lambda e, rotf=rotf: e.tensor_scalar(out=kwb[:], in0=rotf[:, 64:128], scalar1=pat[:, 11:12], scalar2=None, op0=ALU.mult),
                     ["rot0", "rot1", "pat"], ["kwb"])
                P.op("act", lambda e, sub=sub: e.copy(out=vb[:], in_=Tps[:, sub, 128:192]), [tk], ["vb"])
                if STAGE < 4:
                    continue
                P.op("pe", lambda e: e.transpose(trq[:, 0:128], qkb[:, 0:64], identb[:]), ["qkb", "identb"], ["trq"])
                P.op("pe", lambda e: e.transpose(trq[:, 128:256], qkb[:, 64:128], identb[:]), ["qkb", "identb"], ["trq"])
                P.op("act", lambda e: e.copy(out=qkT[:], in_=trq), ["trq"], ["qkT"])
                P.op("act", lambda e: e.copy(out=qf32[:], in_=trq[:, 0:128]), ["trq"], ["qf32"])
                P.op("pool", lambda e: e.tensor_tensor(out=qwT[:], in0=qf32[:], in1=qwbc, op=ALU.mult), ["qf32", "cs"], ["qwT"])
                P.op("pe", lambda e: e.matmul(scp, lhsT=qkT[:, 128:256], rhs=qkT[:, 0:128], start=True, stop=True), ["qkT"], ["scp"])
                P.op("act", lambda e: e.copy(out=scf[:], in_=scp), ["scp"], ["scf"])
                P.op("pool", lambda e: e.tensor_tensor(out=sm[:], in0=scf[:], in1=decT, op=ALU.mult), ["scf", "cs"], ["sm"])
                yk = "Y"
                P.op("pe", lambda e, sub=sub: e.matmul(Yp[:, sub * 64:(sub + 1) * 64], lhsT=sm[:], rhs=vb[:], start=True, stop=False), ["sm", "vb"], [yk])
                P.op("pe", lambda e, sub=sub: e.matmul(Yp[:, sub * 64:(sub + 1) * 64], lhsT=qwT[:], rhs=Sb[:], start=False, stop=True), ["qwT", "Sb"], [yk])
                P.op("pe", lambda e: e.matmul(Up, lhsT=kwb[:], rhs=vb[:], start=True, stop=True), ["kwb", "vb"], ["Up"])
                P.op("dve", lambda e: e.scalar_tensor_tensor(out=Sf[:], in0=Sf[:], scalar=pat[0:64, 8:9], in1=Up, op0=ALU.mult, op1=ALU.add),
                     ["Sf", "Up", "pat"], ["Sf"])
                P.op("act", lambda e: e.copy(out=Sb[:], in_=Sf[:]), ["Sf"], ["Sb"])
            if STAGE < 5:
                continue
            for hb_ in range(2):
                P.op("act", lambda e, hb_=hb_: e.activation(out=sgl[:, 2 * hb_:2 * hb_ + 2, :], in_=Tps[:, 2 * hb_:2 * hb_ + 2, 192:256], func=AF.Silu),
                     [("T", hb_)], [("sgl", hb_)])
            for sub in range(4):
                P.op("dve", lambda e, sub=sub: e.bn_stats(out=st6[:, sub, :], in_=Yp[:, sub * 64:(sub + 1) * 64]), ["Y"], [("st6", sub)])
                P.op("dve", lambda e, sub=sub: e.bn_aggr(out=mv[:, sub, :], in_=st6[:, sub, :]), [("st6", sub)], [("mv", sub)])
            mvk = [("mv", s_) for s_ in range(4)]
            P.op("dve", lambda e: e.tensor_scalar(out=ve[:], in0=mv[:, :, 1], scalar1=EPS, scalar2=None, op0=ALU.add), mvk, ["ve"])
            P.op("act", lambda e: e.activation(out=ve[:], in_=ve[:], func=AF.Sqrt), ["ve"], ["ve"])
            P.op("dve", lambda e: e.reciprocal(out=ve[:], in_=ve[:]), ["ve"], ["ve"])
            for sub in range(4):
                P.op("dve", lambda e, sub=sub: e.tensor_scalar(out=yn[:, sub, :], in0=Yp[:, sub * 64:(sub + 1) * 64], scalar1=mv[:, sub, 0:1],
                                                               scalar2=ve[:, sub:sub + 1], op0=ALU.subtract, op1=ALU.mult),
                     ["Y", ("mv", sub), "ve"], [("yn", sub)])
            P.op("pool", lambda e: e.tensor_tensor(out=obf[:], in0=yn[:], in1=sgl[:], op=ALU.mult), [("yn", s_) for s_ in range(4)] + [("sgl", 0), ("sgl", 1)], ["obf"])
            for sub in range(4):
                P.op("pe", lambda e, sub=sub: e.transpose(tro[:, sub * 128:(sub + 1) * 128], obf[:, sub, :], identb[:]), ["obf", "identb"], ["tro"])
            ok = ("oT", par)
            P.op("act", lambda e, par=par: e.copy(out=oT[par][:], in_=tro), ["tro"], [ok])
            P.dma("sp", mix_ap[ci // 8, 192:256, (ci % 8) * 512:(ci % 8 + 1) * 512], oT[par][:], [ok], ["mix_out"], ok)
        P.barrier()
        return P.emit()


def emit_A_fox(nc, P, hT_ap, wa, pa, cst, mix_ap, nch=NCH):
    with ExitStack() as es:
        C = Ctx(nc, es, P)
        stg = {"i": 0, "t": [C.sb("stg%d" % i, [128, 1024]) for i in range(2)]}
        pat = C.sb("pat", [128, 16])
        P.dma("sp", pat[:], pa, [], ["pat"], "pat")
        cs = C.sb("cs", [128, 128])
        P.dma("sp", cs[:], cst[:, 288:416], [], ["cs"], "cs")
        maskb = C.sb("maskb", [128, 128], BF16)
        P.op("pool", lambda e: e.tensor_copy(out=maskb[:], in_=cs[:]), ["cs"], ["maskb"])
        identb = C.sb("identb", [128, 128], BF16)
        make_ident(P, identb, "identb")
        onesf = C.sb("onesf", [128, 512])
        P.op("pool", lambda e: e.memset(onesf[:], 1.0), [], ["onesf"])
        nb = C.sb("nb", [128, 2])
        P.op("dve", lambda e: e.tensor_scalar(out=nb[:], in0=pat[:, 9:11], scalar1=-1.0, scalar2=None, op0=ALU.mult), ["pat"], ["nb"])
        wq = C.sb("wq", [128, 8, 130], BF16)
        wk = C.sb("wk", [128, 8, 128], BF16)
        wv = C.sb("wv", [128, 8, 128], BF16)
        load_weight_bf16(P, C, wa[:, 128:258], D, 130, wq, "wq", stg)
        load_weight_bf16(P, C, wa[:, 258:386], D, 128, wk, "wk", stg)
        load_weight_bf16(P, C, wa[:, 386:514], D, 128, wv, "wv", stg)
        KT = [C.sb("KT%d" % h, [65, SEQ], BF16) for h in range(2)]
        for h in range(2):
            P.op("pool", lambda e, h=h: e.memset(KT[h][64:65, :], 1.0), [], [("KT", h)])
        Vs = C.sb("Vs", [128, 128, 2, 65], BF16)
        P.op("pool", lambda e: e.memset(Vs[:], 1.0), [], ["Vs"])
        negc = C.sb("negc", [128, 128, 2])
        QT = [[C.sb("QT%d%d" % (h, p), [65, 512], BF16) for p in range(2)] for h in range(2)]
        crow = [[C.sb("crow%d%d" % (h, p), [65, 512]) for p in range(2)] for h in range(2)]
        e1 = C.sb("e1", [65, 512])
        Osb = C.sb("Osb", [65, 512])
        rcp = C.sb("rcp", [65, 512])
        ofb = [C.sb("ofb%d" % i, [64, 512], BF16) for i in range(2)]
        PTt = [C.sb("PT%d" % i, [128, 512], BF16) for i in range(3)]
        hc = [C.sb("hc%d" % i, [128, 8, 512], BF16) for i in range(2)]
        sbank = [C.ps("sbk%d" % i, [128, 512]) for i in range(3)]
        Ob = [C.ps("Ob%d" % i, [128, 512]) for i in range(2)]
        pj = [C.ps("pj%d" % i, [128, 512]) for i in range(2)]
        pmz = C.ps("pmz", [128, 512])
        pji = [0]

        def nextpj():
            i = pji[0] % 2
            pji[0] += 1
            return pj[i], ("pj", i)

        oi = 0
        pending = []
        for ci in range(nch):
            par = ci % 2
            hk = ("hc", par)
            P.dma("sp", hc[par][:], hT_chunk(hT_ap, ci), [], [hk], hk)
            for h in range(2):
                qk_ = ("QT", h, par)
                ck_ = ("crow", h, par)
                pq, pqk = nextpj()
                for f in range(8):
                    P.op("pe", lambda e, pq=pq, h=h, f=f, par=par: e.matmul(pq[0:65, :], lhsT=wq[:, f, h * 65:(h + 1) * 65], rhs=hc[par][:, f, :],
                                                                           start=(f == 0), stop=(f == 7)), ["wq", hk], [pqk])
                P.op("act", lambda e, pq=pq, h=h, par=par: e.mul(out=QT[h][par][0:64, :], in_=pq[0:64, :], mul=0.125), [pqk], [qk_])
                P.op("act", lambda e, pq=pq, h=h: e.activation(out=e1[64:65, :], in_=pq[64:65, :], func=AF.Exp, scale=-1.0, bias=nb[64:65, h:h + 1]),
                     [pqk, "nb"], ["e1"])
                P.op("act", lambda e: e.activation(out=e1[64:65, :], in_=e1[64:65, :], func=AF.Ln, bias=1.0), ["e1"], ["e1"])
                init = 0.0 if ci == 0 else crow[h][1 - par][64:65, 511:512]
                P.op("dve", lambda e, h=h, par=par, init=init: e.tensor_tensor_scan(out=crow[h][par][64:65, :], data0=onesf[64:65, :], data1=e1[64:65, :],
                                                                                    initial=init, op0=ALU.mult, op1=ALU.subtract),
                     ["e1", "onesf", ("crow", h, 1 - par)], [ck_])
                P.op("dve", lambda e, h=h, par=par: e.tensor_copy(out=QT[h][par][64:65, :], in_=crow[h][par][64:65, :]), [ck_], [qk_])
                pc, pck = nextpj()
                for sub in range(4):
                    P.op("pe", lambda e, pc=pc, h=h, par=par, sub=sub: e.matmul(pc[:, sub:sub + 1], lhsT=crow[h][par][64:65, sub * 128:(sub + 1) * 128],
                                                                               rhs=onesf[64:65, 0:1], start=True, stop=True), [ck_, "onesf"], [pck])
                P.op("dve", lambda e, pc=pc, h=h, ci=ci: e.tensor_scalar(out=negc[:, 4 * ci:4 * ci + 4, h], in0=pc[:, 0:4], scalar1=-1.0, scalar2=None,
                                                                        op0=ALU.mult), [pck], [("negc", h, ci)])
                pk_, pkk = nextpj()
                for f in range(8):
                    P.op("pe", lambda e, pk_=pk_, h=h, f=f, par=par: e.matmul(pk_[0:64, :], lhsT=wk[:, f, h * 64:(h + 1) * 64], rhs=hc[par][:, f, :],
                                                                             start=(f == 0), stop=(f == 7)), ["wk", hk], [pkk])
                P.op("act", lambda e, pk_=pk_, h=h, ci=ci: e.copy(out=KT[h][0:64, ci * 512:(ci + 1) * 512], in_=pk_[0:64, :]), [pkk], [("KT", h, ci)])
            for sub in range(4):
                pv, pvk = nextpj()
                for f in range(8):
                    P.op("pe", lambda e, pv=pv, sub=sub, f=f, par=par: e.matmul(pv[:, 0:128], lhsT=hc[par][:, f, sub * 128:(sub + 1) * 128], rhs=wv[:, f, :],
                                                                               start=(f == 0), stop=(f == 7)), ["wv", hk], [pvk])
                P.op("dve", lambda e, pv=pv, sub=sub, ci=ci: e.tensor_copy(out=Vs[:, 4 * ci + sub, :, 0:64],
                                                                           in_=pv[:, 0:128].rearrange("p (a d) -> p a d", a=2)), [pvk, "Vs"], [("Vs", ci)])
            for h in range(2):
                qk_ = ("QT", h, par)
                nj = 4 * ci + 4
                Okey = ("Ob", h)

                def S(j, h=h, par=par, ci=ci):
                    r = j - 4 * ci
                    q0 = max(0, r) * 128
                    bk = ("sbk", j % 3)
                    bank = sbank[j % 3]
                    diag = r >= 0
                    P.op("pe", lambda e: e.matmul(bank[:, q0:512], lhsT=KT[h][0:65, j * 128:(j + 1) * 128], rhs=QT[h][par][0:65, q0:512],
                                                  start=True, stop=not diag), [("KT", h), ("KT", h, j // 4), qk_], [bk])
                    if diag:
                        P.op("pe", lambda e: e.matmul(bank[:, q0:q0 + 128], lhsT=identb[:], rhs=maskb[:], start=False, stop=True),
                             ["identb", "maskb"], [bk])
                    P.op("act", lambda e: e.activation(out=PTt[j % 3][:, q0:512], in_=bank[:, q0:512], func=AF.Exp, bias=negc[:, j, h:h + 1]),
                         [bk, ("negc", h, j // 4)], [("PT", j % 3)])

                def PV(j, h=h, ci=ci, nj=nj):
                    r = j - 4 * ci
                    q0 = max(0, r) * 128
                    P.op("pe", lambda e: e.matmul(Ob[h][0:65, q0:512], lhsT=Vs[:, j, h, :], rhs=PTt[j % 3][:, q0:512],
                                                  start=(j == 0), stop=(j == nj - 1)), ["Vs", ("Vs", j // 4), ("PT", j % 3)], [Okey])

                S(0)
                S(1)
                for j in range(nj):
                    if j + 2 < nj:
                        S(j + 2)
                    PV(j)
                    if j == 1 and pending:
                        pending.pop(0)()
                def norm(h=h, ci=ci, Okey=Okey):
                    nonlocal oi
                    P.op("act", lambda e: e.copy(out=Osb[:], in_=Ob[h][0:65, :]), [Okey], ["Osb"])
                    P.op("dve", lambda e: e.reciprocal(out=rcp[64:65, :], in_=Osb[64:65, :]), ["Osb"], ["rcp"])
                    P.op("pe", lambda e: e.matmul(pmz[0:64, :], lhsT=onesf[64:65, 0:64], rhs=rcp[64:65, :], start=True, stop=True), ["onesf", "rcp"], ["pmz"])
                    ok = ("ofb", oi % 2)
                    o_t = ofb[oi % 2]
                    oi += 1
                    P.op("dve", lambda e: e.tensor_tensor(out=o_t[:], in0=Osb[0:64, :], in1=pmz[0:64, :], op=ALU.mult), ["Osb", "pmz"], [ok])
                    P.dma("sp", mix_ap[ci // 8, 64 + 64 * h:128 + 64 * h, (ci % 8) * 512:(ci % 8 + 1) * 512], o_t[:], [ok], ["mix_out"], ok)
                pending.append(norm)
        while pending:
            pending.pop(0)()
        P.barrier()
        return P.emit()


def mix_perm():
    perm = []
    for s in range(4):
        perm += list(range(64 * s, 64 * s + 64)) + list(range(256 + 128 * s, 256 + 128 * s + 128)) \
            + list(range(768 + 64 * s, 768 + 64 * s + 64))
    return np.array(perm)


def a_weights(inp, l, s):
    w = inp["w_in"][l]
    A, B = 2 * s, 2 * s + 1
    cols = []
    cols += list(range(OFF[0] + 64 * s, OFF[0] + 64 * s + 64))
    cols += list(range(OFF[1] + 64 * s, OFF[1] + 64 * s + 64))
    cols += list(range(OFF[2] + 64 * A, OFF[2] + 64 * A + 64)) + [OFF[5] + A]
    cols += list(range(OFF[2] + 64 * B, OFF[2] + 64 * B + 64)) + [OFF[5] + B]
    cols += list(range(OFF[3] + 64 * A, OFF[3] + 64 * A + 64))
    cols += list(range(OFF[3] + 64 * B, OFF[3] + 64 * B + 64))
    cols += list(range(OFF[4] + 64 * A, OFF[4] + 64 * A + 64))
    cols += list(range(OFF[4] + 64 * B, OFF[4] + 64 * B + 64))
    for g in (6, 7, 8, 9):
        cols += list(range(OFF[g] + 64 * s, OFF[g] + 64 * s + 64))
    wa = np.ascontiguousarray(w[:, cols])
    wg2 = np.ascontiguousarray(np.concatenate([inp["w_rg"][l, s], inp["w_ig"][l, s]], axis=1))
    log_gamma = np.log1p(-np.exp2(-5.0 - np.arange(4, dtype=np.float32))).astype(np.float32)
    lg = log_gamma[s]
    idx = np.arange(128, dtype=np.float32)
    pa = np.zeros((128, 16), np.float32)
    sl = slice(64 * s, 64 * s + 64)
    for k in range(4):
        pa[0:64, k] = inp["conv_w"][l, k, sl]
    pa[0:64, 4] = inp["conv_b"][l, sl]
    pa[0:64, 5] = inp["b_rg"][l, sl]
    pa[0:64, 6] = inp["b_ig"][l, sl]
    pa[0:64, 7] = inp["lru_lambda"][l, sl]
    pa[:, 8] = np.exp(lg * np.float32(128.0))
    pa[:, 9] = inp["fox_b_f"][l, A]
    pa[:, 10] = inp["fox_b_f"][l, B]
    pa[:, 11] = np.exp(lg * (np.float32(127.0) - idx)) * np.float32(0.125)
    cst = np.zeros((128, 416), np.float32)
    diff = idx[:, None] - idx[None, :]
    decay = np.where(diff >= 0, np.exp(lg * np.maximum(diff, 0.0)), 0.0).astype(np.float32)
    cst[:, 0:128] = decay.T * np.float32(0.125)
    cst[:, 128:256] = np.exp(lg * (idx + 1.0))[None, :]
    half = 32
    cst[:, 256:288] = (np.float32(10000.0) ** (-np.arange(half, dtype=np.float32) / np.float32(half))).astype(np.float32)[None, :]
    kk = np.arange(128)
    cst[:, 288:416] = np.where(kk[:, None] > kk[None, :], -30000.0, 0.0)
    return wa, wg2, pa, cst


def _dt(nc, n, s, t=F32, k="ExternalInput"):
    return nc.dram_tensor(n, s, t, kind=k).ap()


GROUPS = [[0, 1, 2, 3], [4, 5, 6, 7]]


def build_fused(stop=99):
    from concourse.bass import DynSlice
    nc = bass.Bass("TRN2", target_bir_lowering=False)
    x = _dt(nc, "x", [TOK, D])
    mem = _dt(nc, "mem", [256, D])
    gm = _dt(nc, "gm", [D])
    pos = _dt(nc, "pos", [SEQ], I32)
    cst = _dt(nc, "cst", [128, 416])
    g0 = _dt(nc, "g0", [D])
    L = []
    for l in range(DEPTH):
        d = {"wa": _dt(nc, "wa%d" % l, [D, 770]), "wg2": _dt(nc, "wg2%d" % l, [64, 128]), "pa": _dt(nc, "pa%d" % l, [128, 16])}
        for n in ["w_out", "w_cq", "w_ck", "w_cv", "w_co"]:
            d[n] = _dt(nc, "%s%d" % (n, l), [D, D])
        for n in ["g_post_mix", "g_pre_cross", "g_post_cross", "g_pre_ffn", "g_post_ffn", "g_next"]:
            d[n] = _dt(nc, "%s%d" % (n, l), [D])
        d["w_gate"] = _dt(nc, "w_gate%d" % l, [D, DFF])
        d["w_up"] = _dt(nc, "w_up%d" % l, [D, DFF])
        d["w_down"] = _dt(nc, "w_down%d" % l, [DFF, D])
        L.append(d)
    out = _dt(nc, "out", [TOK, D], F32, "ExternalOutput")
    hT_own = nc.dram_tensor("hT_own", [D, TOK], BF16).ap()
    hT_all = nc.dram_tensor("hT_all", [4 * D, TOK], BF16).ap()
    mix_c = nc.dram_tensor("mix_c", [4 * 256, TOK], BF16).ap()
    mixG = nc.dram_tensor("mixG", [16 * 256, TOK], BF16).ap()
    x2s = nc.dram_tensor("x2s", [TOK, D], F32).ap()
    x3s = nc.dram_tensor("x3s", [TOK, D], F32).ap()
    hT3 = hT_all.rearrange("(c r p) t -> c r p t", c=8, r=4, p=128)
    mix3 = mix_c.rearrange("(j f) t -> j f t", j=4)
    mixGv = mixG.rearrange("(r j a p) t -> p r j a t", r=4, j=4, a=2, p=128)

    mixT_own = nc.dram_tensor("mixT_own", [D, TOK], BF16).ap()
    mixG5 = mixG.rearrange("(j a r p) t -> j a r p t", j=4, a=2, r=4, p=128)

    with ExitStack() as es:
        P = Prog(nc, es)

        def gather8(src, dst):
            for q in range(8):
                P.collective("AllGather", src[q * 128:(q + 1) * 128, :], dst[q * 512:(q + 1) * 512, :], GROUPS, ["a"], [("b", q)])
            P.cc_follow([("b", q) for q in range(8)])

        def gather(src, dst, rk, wk):
            gather8(src, dst)
            P.barrier()
            P.emit()

        emit_P0(nc, P, x, g0, hT_own, TOK)
        if stop <= 0:
            return nc
        gather(hT_own, hT_all, "a", "b")
        if stop <= 1:
            return nc
        for l in range(DEPTH):
            d = L[l]
            last = l == DEPTH - 1
            if stop <= 2 + 10 * l:
                return nc
            emit_A_lru(nc, P, hT3, d["wa"], d["wg2"], d["pa"], mix3)
            emit_A_ret(nc, P, hT3, d["wa"], d["pa"], cst, pos, mix3)
            emit_A_fox(nc, P, hT3, d["wa"], d["pa"], cst, mix3)
            if stop <= 3 + 10 * l:
                return nc
            gather8(mix_c, mixG)
            sv = {}
            for r_ in range(4):
                def sel(e, r_=r_):
                    if "s" not in sv:
                        sv["s"] = e.snap(e.partition_id() % 4)
                    return e.dma_start(out=mixT_own[r_ * 256:(r_ + 1) * 256, :].rearrange("(a p) t -> a p t", a=2),
                                       in_=mixG5[DynSlice(sv["s"], 1), :, r_, :, :].rearrange("j a p t -> (j a) p t"))
                P.op("pool", sel, [("b", q) for q in range(8)], [("mo", r_)], dma=("mo", r_))
            P.barrier()
            P.emit()
            if stop <= 4 + 10 * l:
                return nc
            emit_B1(nc, P, mixT_own, x if l == 0 else x3s, mem, gm, d["w_out"], d["w_cq"], d["w_ck"], d["w_cv"], d["w_co"],
                    d["g_post_mix"], d["g_pre_cross"], d["g_post_cross"], x2s)
            emit_B2(nc, P, x2s, d["w_gate"], d["w_up"], d["w_down"], d["g_pre_ffn"], d["g_post_ffn"],
                    None if last else d["g_next"], out if last else x3s, None if last else hT_own)
            if not last:
                gather(hT_own, hT_all, "a", "b")
    return nc


def kernel(**inp):
    inp = {k: np.asarray(v) for k, v in inp.items()}
    cores = list(range(8))
    perm = mix_perm()
    x = np.ascontiguousarray(inp["x"], dtype=np.float32)
    maps = []
    for c in cores:
        b, s = c // 4, c % 4
        m = {"x": np.ascontiguousarray(x[b, s * TOK:(s + 1) * TOK]), "mem": np.ascontiguousarray(inp["mem"][b]),
             "gm": inp["mem_norm_g"], "pos": np.ascontiguousarray(inp["positions"][b]).astype(np.int32),
             "g0": inp["pre_mix_g"][0]}
        for l in range(DEPTH):
            wa, wg2, pa, cst = a_weights(inp, l, s)
            m["cst"] = cst
            m["wa%d" % l] = wa
            m["wg2%d" % l] = wg2
            m["pa%d" % l] = pa
            m["w_out%d" % l] = np.ascontiguousarray(inp["w_out"][l][perm])
            for n in ["w_cq", "w_ck", "w_cv", "w_co", "w_gate", "w_up", "w_down"]:
                m["%s%d" % (n, l)] = np.ascontiguousarray(inp[n][l])
            m["g_post_mix%d" % l] = inp["post_mix_g"][l]
            m["g_pre_cross%d" % l] = inp["pre_cross_g"][l]
            m["g_post_cross%d" % l] = inp["post_cross_g"][l]
            m["g_pre_ffn%d" % l] = inp["pre_ffn_g"][l]
            m["g_post_ffn%d" % l] = inp["post_ffn_g"][l]
            m["g_next%d" % l] = inp["pre_mix_g"][min(l + 1, DEPTH - 1)]
        maps.append({k: np.ascontiguousarray(v) for k, v in m.items()})
    res = run_bass_kernel_spmd(build_fused(), maps, core_ids=cores)
    out = np.zeros((NB, SEQ, D), np.float32)
    for c in cores:
        out[c // 4, (c % 4) * TOK:(c % 4 + 1) * TOK] = res.results[c]["out"]
    return out
```

```python
import numpy as np
import ml_dtypes
from contextlib import ExitStack
import concourse.bass as bass
import concourse.mybir as mybir
from concourse.bass_utils import run_bass_kernel_spmd

F32 = mybir.dt.float32
BF16 = mybir.dt.bfloat16
I32 = mybir.dt.int32
AF = mybir.ActivationFunctionType
ALU = mybir.AluOpType
AX = mybir.AxisListType

D = 1024
SEQ = 16384
NB = 2
DEPTH = 2
TOK = 4096
DFF = 2816
NFF = DFF // 128
EPS = 1e-6
SPLIT = (256, 256, 512, 512, 512, 8, 256, 256, 256, 256)
OFF = np.concatenate([[0], np.cumsum(SPLIT)]).tolist()
PI = float(np.pi)


class Prog:
    ENGS = ("pe", "act", "dve", "pool", "sp")

    def __init__(self, nc, es, n_dma_sems=48):
        self.nc = nc
        self.eng_sem = {e: es.enter_context(nc.semaphore("s_" + e)) for e in self.ENGS}
        self.eng_cnt = {e: 0 for e in self.ENGS}
        self.dma_sems = [es.enter_context(nc.semaphore("d%d" % i)) for i in range(n_dma_sems)]
        self.dma_cnt = [0] * n_dma_sems
        self.dma_key2idx = {}
        self.waited = {e: {} for e in self.ENGS}
        self.cc_sem = es.enter_context(nc.semaphore("s_cc"))
        self.cc_scratch = es.enter_context(nc.sbuf_tensor("cc_scratch", [128, 8], F32))
        self.cc_cnt = 0
        self.ops = []
        self.state = {}
        self.last = {}
        self.pending_dma = []

    def dma_sem_for(self, key):
        if key not in self.dma_key2idx:
            idx = len(self.dma_key2idx)
            assert idx < len(self.dma_sems), "out of dma sems"
            self.dma_key2idx[key] = idx
        return self.dma_key2idx[key]

    def op(self, eng, fn, reads=(), writes=(), dma=None, extra=()):
        deps = set(extra)
        for k in reads:
            st = self.state.setdefault(k, {"w": None, "r": []})
            if st["w"] is not None:
                deps.add(st["w"])
        for k in writes:
            st = self.state.setdefault(k, {"w": None, "r": []})
            if st["w"] is not None:
                deps.add(st["w"])
            deps.update(st["r"])
        oid = len(self.ops)
        self.ops.append(dict(id=oid, eng=eng, fn=fn, deps=deps,
                             dma=None if dma is None else self.dma_sem_for(dma)))
        for k in reads:
            self.state[k]["r"].append(oid)
        for k in writes:
            self.state[k] = {"w": oid, "r": []}
        if dma is None:
            self.last[eng] = oid
        else:
            self.pending_dma.append(oid)
        return oid

    def dma(self, q, out, in_, reads, writes, semkey, **kw):
        return self.op(q, lambda e: e.dma_start(out=out, in_=in_, **kw), reads, writes, dma=semkey)

    def collective(self, kind, in_ap, out_ap, groups, reads, writes):
        def fn(e):
            return e.collective_compute(kind, ALU.bypass, replica_groups=groups, ins=[in_ap.opt()], outs=[out_ap.opt()])
        oid = self.op("pool", fn, reads, writes)
        self.ops[oid]["cc"] = True
        return oid

    def cc_follow(self, keys):
        scr = self.cc_scratch
        self.op("pool", lambda e: e.memset(scr[:], 0.0), list(keys), list(keys))

    def barrier(self):
        ids = list(self.last.values()) + list(self.pending_dma)
        for e in self.ENGS:
            self.op(e, lambda en: None, extra=ids)
        self.pending_dma = []
        self.state = {}

    def emit(self):
        ops = self.ops

        def pe_pe(a, b):
            return a["eng"] == "pe" and b["eng"] == "pe" and a["dma"] is None and b["dma"] is None

        needed = set()
        for o in ops:
            for d in o["deps"]:
                if not pe_pe(ops[d], o):
                    needed.add(d)
        for o in ops:
            if o.get("cc"):
                self.cc_cnt += 1
                o["sig"] = (self.cc_sem, self.cc_cnt, None)
            elif o["dma"] is not None:
                self.dma_cnt[o["dma"]] += 16
                o["sig"] = (self.dma_sems[o["dma"]], self.dma_cnt[o["dma"]], 16)
            elif o["id"] in needed:
                self.eng_cnt[o["eng"]] += 1
                o["sig"] = (self.eng_sem[o["eng"]], self.eng_cnt[o["eng"]], 1)
            else:
                o["sig"] = None
        per = {e: [] for e in self.ENGS}
        carry = {e: [] for e in self.ENGS}
        for o in ops:
            w = {}
            for d in o["deps"]:
                if pe_pe(ops[d], o):
                    continue
                sg = ops[d]["sig"]
                k = id(sg[0])
                if k not in w or w[k][1] < sg[1]:
                    w[k] = (sg[0], sg[1])
            wl = carry[o["eng"]]
            carry[o["eng"]] = []
            wd = self.waited[o["eng"]]
            for k, (s, v) in w.items():
                if wd.get(k, 0) >= v:
                    continue
                wd[k] = v
                wl.append((s, v))
            o["waits"] = wl
            per[o["eng"]].append(o)

        def replay(lst):
            def f(e):
                pend_sig = None
                for o in lst:
                    for (s, v) in o["waits"]:
                        e.wait_ge(s, v)
                    ins = o["fn"](e)
                    if o["sig"] is not None:
                        assert ins is not None, "signalling op must emit an instruction"
                        if o["sig"][2] is None:
                            ins.then_inc(o["sig"][0])
                        else:
                            ins.then_inc(o["sig"][0], o["sig"][2])
            return f

        with self.nc.Block() as block:
            if per["pe"]:
                block.tensor(replay(per["pe"]))
            if per["act"]:
                block.scalar(replay(per["act"]))
            if per["dve"]:
                block.vector(replay(per["dve"]))
            if per["pool"]:
                block.gpsimd(replay(per["pool"]))
            if per["sp"]:
                block.sync(replay(per["sp"]))
        n = {e: len(per[e]) for e in self.ENGS}
        self.ops = []
        self.state = {}
        self.last = {}
        self.pending_dma = []
        return n


class Ctx:
    def __init__(self, nc, es, P):
        self.nc, self.es, self.P = nc, es, P
        self.n = 0

    UID = [0]

    def sb(self, name, shape, dt=F32):
        Ctx.UID[0] += 1
        return self.es.enter_context(self.nc.sbuf_tensor("sb%d_%s" % (Ctx.UID[0], name), shape, dt))

    def ps(self, name, shape, dt=F32):
        Ctx.UID[0] += 1
        return self.es.enter_context(self.nc.psum_tensor("ps%d_%s" % (Ctx.UID[0], name), shape, dt))


def make_ident(P, ident, key="ident"):
    P.op("pool", lambda e: e.memset(ident[:], 1.0), [], [key])

    def sel(e):
        if getattr(P, "zero_reg", None) is None:
            P.zero_reg = e.to_reg(0.0)
        return e.affine_select(out=ident[:], in_=ident[:], pattern=[[-1, 128]], compare_op=ALU.is_equal,
                               fill=P.zero_reg, base=0, channel_multiplier=1)
    P.op("pool", sel, [key], [key])


def load_bcast_rows(P, C, name, src_row_ap, n):
    t = C.sb(name, [128, n])
    P.dma("sp", t[:], src_row_ap.partition_broadcast(128), [], [name], name)
    return t


def load_weight_bf16(P, C, w_ap, K, N, dst, dkey, stg, eng_cast="pool"):
    wv = w_ap.rearrange("(c p) n -> p c n", p=128)
    nk = K // 128
    i = 0
    for c in range(nk):
        for n0 in range(0, N, 1024):
            n1 = min(N, n0 + 1024)
            sk = ("stg", stg["i"] % 2)
            st = stg["t"][stg["i"] % 2]
            stg["i"] += 1
            P.dma("sp", st[:, 0:n1 - n0], wv[:, c, n0:n1], [], [sk], sk)
            if stg["i"] % 2 == 0:
                P.op("dve", lambda e, st=st, c=c, n0=n0, n1=n1: e.tensor_copy(out=dst[:, c, n0:n1], in_=st[:, 0:n1 - n0]),
                     [sk], [dkey])
            else:
                P.op("act", lambda e, st=st, c=c, n0=n0, n1=n1: e.copy(out=dst[:, c, n0:n1], in_=st[:, 0:n1 - n0]),
                     [sk], [dkey])


def rstd_of(P, C, src, skey, tag, junk, n=D):
    ss = C.tmp["ss"]
    P.op("act", lambda e: e.activation(out=junk[:, 0:n], in_=src, func=AF.Square, accum_out=ss[:, 0:1]),
         [skey], ["junk", "ss"])
    P.op("dve", lambda e: e.tensor_scalar(out=ss[:, 1:2], in0=ss[:, 0:1], scalar1=1.0 / n, scalar2=EPS,
                                          op0=ALU.mult, op1=ALU.add), ["ss"], ["ss1"])
    P.op("act", lambda e: e.activation(out=ss[:, 2:3], in_=ss[:, 1:2], func=AF.Sqrt), ["ss1"], ["ss2"])
    P.op("dve", lambda e: e.reciprocal(out=ss[:, 3:4], in_=ss[:, 2:3]), ["ss2"], ["ss3"])
    return ss[:, 3:4], "ss3"


def norm_T(P, C, src, skey, g_t, gkey, hbf, ident, trp, trkey, dstT, dkey, col0):
    r, rk = rstd_of(P, C, src, skey, "n", C.tmp["junk"])
    P.op("dve", lambda e: e.scalar_tensor_tensor(out=hbf[:], in0=src, scalar=r, in1=g_t[:],
                                                 op0=ALU.mult, op1=ALU.mult), [skey, rk, gkey], ["hbf"])
    for c in range(8):
        P.op("pe", lambda e, c=c: e.transpose(trp[:, c * 128:(c + 1) * 128], hbf[:, c * 128:(c + 1) * 128], ident[:]),
             ["hbf", "ident"], [trkey])
    P.op("act", lambda e: e.copy(out=dstT[:, :, col0:col0 + 128],
                                 in_=trp[:].rearrange("p (c t) -> p c t", c=8)), [trkey], [dkey])


def emit_P0(nc, P, x_ap, g_ap, hT_ap, ntok, gather_cb=None):
    with ExitStack() as es:
        C = Ctx(nc, es, P)
        C.tmp = {"ss": C.sb("ss", [128, 4]), "junk": C.sb("junk", [128, D], BF16)}
        ident = C.sb("ident", [128, 128], BF16)
        make_ident(P, ident)
        g_t = load_bcast_rows(P, C, "g0", g_ap, D)
        hbf = C.sb("hbf", [128, D], BF16)
        xs = [C.sb("x%d" % i, [128, D]) for i in range(2)]
        trp = [C.ps("tr%d" % i, [128, D], BF16) for i in range(2)]
        hT = [C.sb("hT%d" % i, [128, 8, 512], BF16) for i in range(2)]
        for st in range(ntok // 512):
            hk = ("hT", st % 2)
            for sub in range(4):
                i = st * 4 + sub
                xk = ("x", i % 2)
                P.dma("sp", xs[i % 2][:], x_ap[i * 128:(i + 1) * 128, :], [], [xk], xk)
                norm_T(P, C, xs[i % 2][:], xk, g_t, "g0", hbf, ident, trp[i % 2], ("tr", i % 2),
                       hT[st % 2], hk, sub * 128)
            P.dma("sp", hT_ap[st * 1024:(st + 1) * 1024, :].rearrange("(c p) t -> p c t", p=128), hT[st % 2][:], [hk], [("hT_out", st)], hk)
            if gather_cb is not None:
                gather_cb(st)
        if gather_cb is not None:
            gather_cb(None)
        P.barrier()
        return P.emit()


def emit_B1(nc, P, mixT_ap, x_ap, mem_ap, gm_ap, w_out, w_cq, w_ck, w_cv, w_co,
            g_post_mix, g_pre_cross, g_post_cross, x2_ap, mix_fn=None):
    with ExitStack() as es:
        C = Ctx(nc, es, P)
        C.tmp = {"ss": C.sb("ss", [128, 4]), "junk": C.sb("junk", [128, D], BF16)}
        ident = C.sb("ident", [128, 128], BF16)
        make_ident(P, ident)
        ones_bf = C.sb("ones_bf", [128, 128], BF16)
        P.op("pool", lambda e: e.memset(ones_bf[:], 1.0), [], ["ones_bf"])
        stg = {"i": 0, "t": [C.sb("stg%d" % i, [128, 1024]) for i in range(2)]}
        gpm = load_bcast_rows(P, C, "gpm", g_post_mix, D)
        gpc = load_bcast_rows(P, C, "gpc", g_pre_cross, D)
        gqc = load_bcast_rows(P, C, "gqc", g_post_cross, D)
        gmm = load_bcast_rows(P, C, "gmm", gm_ap, D)
        wo = C.sb("wo", [128, 8, D], BF16)
        wq = C.sb("wq", [128, 8, D], BF16)
        wc = C.sb("wc", [128, 8, D], BF16)
        wk = C.sb("wk", [128, 8, D], BF16)
        load_weight_bf16(P, C, w_ck, D, D, wk, "wk", stg)
        hbf = C.sb("hbf", [128, D], BF16)
        acc = [C.ps("acc%d" % i, [128, D]) for i in range(2)]
        trp = [C.ps("tr%d" % i, [128, D], BF16) for i in range(2)]
        mb = [C.ps("mb%d" % i, [128, 512]) for i in range(2)]
        memT = C.sb("memT", [128, 8, 256], BF16)
        xs = [C.sb("x%d" % i, [128, D]) for i in range(4)]
        for i in range(2):
            xk = ("x", i)
            P.dma("sp", xs[i][:], mem_ap[i * 128:(i + 1) * 128, :], [], [xk], xk)
            norm_T(P, C, xs[i][:], xk, gmm, "gmm", hbf, ident, trp[i], ("tr", i), memT, "memT", i * 128)
        kT = C.sb("kT", [128, 8, 256], BF16)
        for cc in range(8):
            for f in range(8):
                P.op("pe", lambda e, cc=cc, f=f: e.matmul(mb[cc % 2][:, 0:256], lhsT=wk[:, f, cc * 128:(cc + 1) * 128],
                                                           rhs=memT[:, f, :], start=(f == 0), stop=(f == 7)),
                     ["wk", "memT"], [("mb", cc % 2)])
            P.op("act", lambda e, cc=cc: e.copy(out=kT[:, cc, :], in_=mb[cc % 2][:, 0:256]), [("mb", cc % 2)], ["kT"])
        load_weight_bf16(P, C, w_cv, D, D, wk, "wk", stg)
        vm = C.sb("vm", [128, 2, D], BF16)
        for m in range(2):
            for half in range(2):
                for f in range(8):
                    P.op("pe", lambda e, m=m, half=half, f=f: e.matmul(
                        acc[m][:, half * 512:(half + 1) * 512], lhsT=memT[:, f, m * 128:(m + 1) * 128],
                        rhs=wk[:, f, half * 512:(half + 1) * 512], start=(f == 0), stop=(f == 7)),
                        ["wk", "memT"], [("acc", m)])
            P.op("act", lambda e, m=m: e.copy(out=vm[:, m, :], in_=acc[m][:]), [("acc", m)], ["vm"])
        load_weight_bf16(P, C, w_out, D, D, wo, "wo", stg)
        load_weight_bf16(P, C, w_cq, D, D, wq, "wq", stg)
        load_weight_bf16(P, C, w_co, D, D, wc, "wc", stg)
        mixT = [C.sb("mixT%d" % i, [128, 8, 512], BF16) for i in range(2)]
        h2T = C.sb("h2T", [128, 8, 512], BF16)
        qT = C.sb("qT", [128, 8, 512], BF16)
        PT = C.sb("PT", [128, 8, 512], BF16)
        oT = C.sb("oT", [128, 8, 512], BF16)
        rec = C.sb("rec", [128, 512])
        tmp = C.sb("tmp", [128, D])
        mix_q = "pool"
        if mix_fn is None:
            mix_q = "sp"
            mixv = mixT_ap.rearrange("(c p) t -> p c t", p=128)
            mix_fn = lambda e, st, r: mixv[:, 2 * r:2 * r + 2, st * 512:(st + 1) * 512]
        nst = TOK // 512
        for st in range(nst):
            mk = ("mixT", st % 2)
            for r_ in range(4):
                mkr = ("mixT", st % 2, r_)
                P.op(mix_q, lambda e, st=st, r_=r_: e.dma_start(out=mixT[st % 2][:, 2 * r_:2 * r_ + 2, :], in_=mix_fn(e, st, r_)), [], [mkr], dma=mkr)
            for sub in range(4):
                t0 = st * 512 + sub * 128
                xk = ("x", sub)
                ak = ("acc", sub % 2)
                a = acc[sub % 2]
                P.dma("sp", xs[sub][:], x_ap[t0:t0 + 128, :], [], [xk], xk)
                for half in range(2):
                    for c in range(8):
                        P.op("pe", lambda e, a=a, half=half, c=c, sub=sub, st=st: e.matmul(
                            a[:, half * 512:(half + 1) * 512], lhsT=mixT[st % 2][:, c, sub * 128:(sub + 1) * 128],
                            rhs=wo[:, c, half * 512:(half + 1) * 512], start=(c == 0), stop=(c == 7)),
                            [("mixT", st % 2, c // 2), "wo"], [ak])
                r, rk = rstd_of(P, C, a[:], ak, "y", C.tmp["junk"])
                P.op("dve", lambda e, a=a, r=r: e.scalar_tensor_tensor(out=tmp[:], in0=a[:], scalar=r, in1=gpm[:],
                                                                       op0=ALU.mult, op1=ALU.mult), [ak, rk, "gpm"], ["tmp"])
                P.op("pool", lambda e, sub=sub: e.tensor_tensor(out=xs[sub][:], in0=xs[sub][:], in1=tmp[:], op=ALU.add),
                     [xk, "tmp"], [xk])
                norm_T(P, C, xs[sub][:], xk, gpc, "gpc", hbf, ident, trp[sub % 2], ("tr", sub % 2), h2T, "h2T", sub * 128)
            for cc in range(8):
                for f in range(8):
                    P.op("pe", lambda e, cc=cc, f=f: e.matmul(mb[cc % 2][:], lhsT=wq[:, f, cc * 128:(cc + 1) * 128],
                                                               rhs=h2T[:, f, :], start=(f == 0), stop=(f == 7)),
                         ["wq", "h2T"], [("mb", cc % 2)])
                if cc % 2 == 0:
                    P.op("act", lambda e, cc=cc: e.copy(out=qT[:, cc, :], in_=mb[cc % 2][:]), [("mb", cc % 2)], [("qT", cc)])
                else:
                    P.op("dve", lambda e, cc=cc: e.tensor_copy(out=qT[:, cc, :], in_=mb[cc % 2][:]), [("mb", cc % 2)], [("qT", cc)])
            for h in range(4):
                for m in range(2):
                    bk = ("mb", m)
                    for dc in range(2):
                        P.op("pe", lambda e, h=h, m=m, dc=dc: e.matmul(
                            mb[m][:], lhsT=kT[:, 2 * h + dc, m * 128:(m + 1) * 128], rhs=qT[:, 2 * h + dc, :],
                            start=(dc == 0), stop=(dc == 1)), ["kT", ("qT", 2 * h + dc)], [bk])
                    P.op("act", lambda e, h=h, m=m: e.activation(out=PT[:, 2 * h + m, :], in_=mb[m][:], func=AF.Exp,
                                                                 scale=1.0 / 16.0), [bk], [("PT", 2 * h + m)])
                for m in range(2):
                    P.op("pe", lambda e, h=h, m=m: e.matmul(acc[0][:, 0:512], lhsT=ones_bf[:], rhs=PT[:, 2 * h + m, :],
                                                            start=(m == 0), stop=(m == 1)),
                         ["ones_bf", ("PT", 2 * h + m)], [("acc", 0)])
                P.op("dve", lambda e: e.reciprocal(out=rec[:], in_=acc[0][:, 0:512]), [("acc", 0)], ["rec"])
                for dc in range(2):
                    for m in range(2):
                        P.op("pe", lambda e, h=h, m=m, dc=dc: e.matmul(
                            acc[1][:, dc * 512:(dc + 1) * 512], lhsT=vm[:, m, (2 * h + dc) * 128:(2 * h + dc + 1) * 128],
                            rhs=PT[:, 2 * h + m, :], start=(m == 0), stop=(m == 1)),
                            ["vm", ("PT", 2 * h + m)], [("acc", 1)])
                for dc in range(2):
                    P.op("dve", lambda e, h=h, dc=dc: e.tensor_tensor(out=oT[:, 2 * h + dc, :],
                                                                      in0=acc[1][:, dc * 512:(dc + 1) * 512], in1=rec[:],
                                                                      op=ALU.mult), [("acc", 1), "rec"], [("oT", 2 * h + dc)])
            for sub in range(4):
                t0 = st * 512 + sub * 128
                xk = ("x", sub)
                ak = ("acc", sub % 2)
                a = acc[sub % 2]
                for half in range(2):
                    for c in range(8):
                        P.op("pe", lambda e, a=a, half=half, c=c, sub=sub: e.matmul(
                            a[:, half * 512:(half + 1) * 512], lhsT=oT[:, c, sub * 128:(sub + 1) * 128],
                            rhs=wc[:, c, half * 512:(half + 1) * 512], start=(c == 0), stop=(c == 7)),
                            [("oT", c), "wc"], [ak])
                r, rk = rstd_of(P, C, a[:], ak, "y", C.tmp["junk"])
                P.op("dve", lambda e, a=a, r=r: e.scalar_tensor_tensor(out=tmp[:], in0=a[:], scalar=r, in1=gqc[:],
                                                                       op0=ALU.mult, op1=ALU.mult), [ak, rk, "gqc"], ["tmp"])
                P.op("pool", lambda e, sub=sub: e.tensor_tensor(out=xs[sub][:], in0=xs[sub][:], in1=tmp[:], op=ALU.add),
                     [xk, "tmp"], [xk])
                P.dma("sp", x2_ap[t0:t0 + 128, :], xs[sub][:], [xk], ["x2_out"], xk)
        P.barrier()
        return P.emit()


def emit_B2(nc, P, x2_ap, w_gate, w_up, w_down, g_pre_ffn, g_post_ffn, g_next, x3_ap, hT_ap, gather_cb=None):
    ST = 512
    with ExitStack() as es:
        C = Ctx(nc, es, P)
        C.tmp = {"ss": C.sb("ss", [128, 4]), "junk": C.sb("junk", [128, D], BF16)}
        ident = C.sb("ident", [128, 128], BF16)
        make_ident(P, ident)
        stg = {"i": 0, "t": [C.sb("stg%d" % i, [128, 1024]) for i in range(2)]}
        gpf = load_bcast_rows(P, C, "gpf", g_pre_ffn, D)
        gqf = load_bcast_rows(P, C, "gqf", g_post_ffn, D)
        gnx = load_bcast_rows(P, C, "gnx", g_next, D) if g_next is not None else None
        wg = C.sb("wg", [128, 8, DFF], BF16)
        wu = C.sb("wu", [128, 8, DFF], BF16)
        wd = C.sb("wd", [128, NFF, D], BF16)
        load_weight_bf16(P, C, w_gate, D, DFF, wg, "wg", stg)
        load_weight_bf16(P, C, w_up, D, DFF, wu, "wu", stg)
        load_weight_bf16(P, C, w_down, DFF, D, wd, "wd", stg)
        hbf = C.sb("hbf", [128, D], BF16)
        acc = [C.ps("acc%d" % i, [128, D]) for i in range(2)]
        trp = [C.ps("tr%d" % i, [128, D], BF16) for i in range(2)]
        mb = [C.ps("mb%d" % i, [128, 512]) for i in range(2)]
        xs = [C.sb("x%d" % i, [128, D]) for i in range(2)]
        h3T = C.sb("h3T", [128, 8, ST], BF16)
        aT = C.sb("aT", [128, NFF, ST], BF16)
        sg = C.sb("sg", [128, ST])
        tmp = C.sb("tmp", [128, D])
        nsub = ST // 128
        xi = 0
        for st in range(TOK // ST):
            for sub in range(nsub):
                t0 = st * ST + sub * 128
                xk = ("x", xi % 2)
                xt = xs[xi % 2]
                P.dma("sp", xt[:], x2_ap[t0:t0 + 128, :], [], [xk], xk)
                norm_T(P, C, xt[:], xk, gpf, "gpf", hbf, ident, trp[xi % 2], ("tr", xi % 2), h3T, "h3T", sub * 128)
                xi += 1
            for fc in range(NFF):
                for f in range(8):
                    P.op("pe", lambda e, fc=fc, f=f: e.matmul(mb[0][:, 0:ST], lhsT=wg[:, f, fc * 128:(fc + 1) * 128],
                                                               rhs=h3T[:, f, :], start=(f == 0), stop=(f == 7)),
                         ["wg", "h3T"], [("mb", 0)])
                for f in range(8):
                    P.op("pe", lambda e, fc=fc, f=f: e.matmul(mb[1][:, 0:ST], lhsT=wu[:, f, fc * 128:(fc + 1) * 128],
                                                               rhs=h3T[:, f, :], start=(f == 0), stop=(f == 7)),
                         ["wu", "h3T"], [("mb", 1)])
                P.op("act", lambda e: e.activation(out=sg[:], in_=mb[0][:, 0:ST], func=AF.Silu), [("mb", 0)], ["sg"])
                P.op("dve", lambda e, fc=fc: e.tensor_tensor(out=aT[:, fc, :], in0=mb[1][:, 0:ST], in1=sg[:], op=ALU.mult),
                     [("mb", 1), "sg"], [("aT", fc)])
            for sub in range(nsub):
                t0 = st * ST + sub * 128
                xk = ("x", xi % 2)
                xt = xs[xi % 2]
                ak = ("acc", sub % 2)
                a = acc[sub % 2]
                P.dma("sp", xt[:], x2_ap[t0:t0 + 128, :], [], [xk], xk)
                for half in range(2):
                    for fc in range(NFF):
                        P.op("pe", lambda e, a=a, half=half, fc=fc, sub=sub: e.matmul(
                            a[:, half * 512:(half + 1) * 512], lhsT=aT[:, fc, sub * 128:(sub + 1) * 128],
                            rhs=wd[:, fc, half * 512:(half + 1) * 512], start=(fc == 0), stop=(fc == NFF - 1)),
                            [("aT", fc), "wd"], [ak])
                r, rk = rstd_of(P, C, a[:], ak, "y", C.tmp["junk"])
                P.op("dve", lambda e, a=a, r=r: e.scalar_tensor_tensor(out=tmp[:], in0=a[:], scalar=r, in1=gqf[:],
                                                                       op0=ALU.mult, op1=ALU.mult), [ak, rk, "gqf"], ["tmp"])
                P.op("pool", lambda e, xt=xt: e.tensor_tensor(out=xt[:], in0=xt[:], in1=tmp[:], op=ALU.add),
                     [xk, "tmp"], [xk])
                P.dma("sp", x3_ap[t0:t0 + 128, :], xt[:], [xk], ["x3_out"], xk)
                if gnx is not None:
                    norm_T(P, C, xt[:], xk, gnx, "gnx", hbf, ident, trp[xi % 2], ("tr", xi % 2), h3T, "h3T", sub * 128)
                xi += 1
            if gnx is not None:
                P.dma("sp", hT_ap[st * 1024:(st + 1) * 1024, :].rearrange("(c p) t -> p c t", p=128), h3T[:], ["h3T"], [("hT_out", st)], "h3T")
                if gather_cb is not None:
                    gather_cb(st)
        if gnx is not None and gather_cb is not None:
            gather_cb(None)
        P.barrier()
        return P.emit()


NCH = SEQ // 512


def hT_chunk(hT_ap, ci):
    base = (ci % 8) * 4096 + (ci // 8) * 1024
    return hT_ap[base:base + 1024, :].rearrange("(c p) t -> p c t", p=128)


def emit_A_lru(nc, P, hT_ap, wa, wg2, pa, mix_ap):
    with ExitStack() as es:
        C = Ctx(nc, es, P)
        stg = {"i": 0, "t": [C.sb("stg%d" % i, [128, 1024]) for i in range(2)]}
        pat = C.sb("pat", [128, 16])
        P.dma("sp", pat[:], pa, [], ["pat"], "pat")
        wl = C.sb("wl", [128, 8, 128], BF16)
        load_weight_bf16(P, C, wa[:, 0:128], D, 128, wl, "wl", stg)
        wgs = C.sb("wgs", [64, 128])
        wgb = C.sb("wgb", [64, 128], BF16)
        P.dma("sp", wgs[:], wg2, [], ["wgs"], "wgs")
        P.op("pool", lambda e: e.tensor_copy(out=wgb[:], in_=wgs[:]), ["wgs"], ["wgb"])
        kap = C.sb("kap", [64, 4])
        P.op("act", lambda e: e.activation(out=kap[:, 0:1], in_=pat[0:64, 7:8], func=AF.Exp, scale=-1.0), ["pat"], ["kap0"])
        P.op("act", lambda e: e.activation(out=kap[:, 1:2], in_=kap[:, 0:1], func=AF.Ln, bias=1.0), ["kap0"], ["kap1"])
        P.op("dve", lambda e: e.tensor_scalar(out=kap[:, 2:3], in0=kap[:, 1:2], scalar1=-8.0, scalar2=None, op0=ALU.mult), ["kap1"], ["kap2"])
        P.op("dve", lambda e: e.tensor_scalar(out=kap[:, 3:4], in0=kap[:, 1:2], scalar1=-16.0, scalar2=None, op0=ALU.mult), ["kap1"], ["kap3"])
        hc = [C.sb("hc%d" % i, [128, 8, 512], BF16) for i in range(2)]
        pm2 = [[C.ps("pm%d%d" % (p_, i), [128, 512]) for i in range(2)] for p_ in range(2)]
        pg2 = [[C.ps("pg%d%d" % (p_, i), [128, 512]) for i in range(2)] for p_ in range(2)]
        lxb2 = [C.sb("lxb%d" % p_, [64, 515]) for p_ in range(2)]
        P.op("pool", lambda e: e.memset(lxb2[0][:, 0:3], 0.0), [], [("lxt", 0)])
        names = ["xc", "sr", "si", "a", "a2", "sq", "ix", "u", "gl"]
        f32t2 = [{n: C.sb(n + str(p_), [64, 512]) for n in names} for p_ in range(2)]
        xcb2 = [C.sb("xcb%d" % p_, [64, 512], BF16) for p_ in range(2)]
        hb = [C.sb("hb%d" % i, [64, 512]) for i in range(2)]
        ob = [C.sb("ob%d" % i, [64, 512], BF16) for i in range(2)]

        def stages(ci):
            par = ci % 2
            T = f32t2[par]
            xcb = xcb2[par]
            pm = pm2[par]
            pg = pg2[par]
            lxb = lxb2[par]
            K_ = lambda n: (n, par)
            hk = ("hc", par)

            def s0():
                P.dma("sp", hc[par][:], hT_chunk(hT_ap, ci), [], [hk], hk)
                for g in range(2):
                    for f in range(8):
                        P.op("pe", lambda e, g=g, f=f: e.matmul(pm[g][0:64, :], lhsT=wl[:, f, g * 64:(g + 1) * 64], rhs=hc[par][:, f, :],
                                                                 start=(f == 0), stop=(f == 7)), ["wl", hk], [("pm", par, g)])
                P.op("act", lambda e: e.copy(out=lxb[:, 3:515], in_=pm[0][0:64, :]), [("pm", par, 0)], [("lxm", par)])
                if ci > 0:
                    P.op("dve", lambda e: e.tensor_copy(out=lxb[:, 0:3], in_=lxb2[1 - par][:, 512:515]), [("lxm", 1 - par)], [("lxt", par)])

            def s1():
                rk = [("lxm", par), ("lxt", par), "pat"]
                P.op("dve", lambda e: e.tensor_scalar(out=T["xc"][:], in0=lxb[:, 3:515], scalar1=pat[0:64, 3:4], scalar2=pat[0:64, 4:5],
                                                      op0=ALU.mult, op1=ALU.add), rk, [K_("xc")])
                for k in (2, 1, 0):
                    P.op("dve", lambda e, k=k: e.scalar_tensor_tensor(out=T["xc"][:], in0=lxb[:, k:k + 512], scalar=pat[0:64, k:k + 1],
                                                                      in1=T["xc"][:], op0=ALU.mult, op1=ALU.add), rk + [K_("xc")], [K_("xc")])
                P.op("pool", lambda e: e.tensor_copy(out=xcb[:], in_=T["xc"][:]), [K_("xc")], [K_("xcb")])
                for g in range(2):
                    P.op("pe", lambda e, g=g: e.matmul(pg[g][0:64, :], lhsT=wgb[:, g * 64:(g + 1) * 64], rhs=xcb[:], start=True, stop=True),
                         ["wgb", K_("xcb")], [("pg", par, g)])

            def s2():
                P.op("act", lambda e: e.activation(out=T["sr"][:], in_=pg[0][0:64, :], func=AF.Sigmoid, bias=pat[0:64, 5:6]), [("pg", par, 0), "pat"], [K_("sr")])
                P.op("act", lambda e: e.activation(out=T["si"][:], in_=pg[1][0:64, :], func=AF.Sigmoid, bias=pat[0:64, 6:7]), [("pg", par, 1), "pat"], [K_("si")])
                P.op("pool", lambda e: e.tensor_tensor(out=T["ix"][:], in0=T["si"][:], in1=T["xc"][:], op=ALU.mult), [K_("si"), K_("xc")], [K_("ix")])

            def s3():
                P.op("act", lambda e: e.activation(out=T["a"][:], in_=T["sr"][:], func=AF.Exp, scale=kap[:, 2:3]), [K_("sr"), "kap2"], [K_("a")])
                P.op("act", lambda e: e.activation(out=T["a2"][:], in_=T["sr"][:], func=AF.Exp, scale=kap[:, 3:4]), [K_("sr"), "kap3"], [K_("a2")])
                P.op("dve", lambda e: e.tensor_scalar(out=T["a2"][:], in0=T["a2"][:], scalar1=-1.0, scalar2=1.0, op0=ALU.mult, op1=ALU.add), [K_("a2")], [K_("a2")])

            def s4():
                P.op("act", lambda e: e.activation(out=T["sq"][:], in_=T["a2"][:], func=AF.Sqrt), [K_("a2")], [K_("sq")])
                P.op("dve", lambda e: e.tensor_tensor(out=T["u"][:], in0=T["sq"][:], in1=T["ix"][:], op=ALU.mult), [K_("sq"), K_("ix")], [K_("u")])
                init = 0.0 if ci == 0 else hb[1 - par][:, 511:512]
                P.op("dve", lambda e: e.tensor_tensor_scan(out=hb[par][:], data0=T["a"][:], data1=T["u"][:], initial=init,
                                                           op0=ALU.mult, op1=ALU.add), [K_("a"), K_("u"), ("hb", 1 - par)], [("hb", par)])

            def s5():
                P.op("act", lambda e: e.activation(out=T["gl"][:], in_=pm[1][0:64, :], func=AF.Gelu_apprx_tanh), [("pm", par, 1)], [K_("gl")])
                P.op("dve", lambda e: e.tensor_tensor(out=ob[par][:], in0=hb[par][:], in1=T["gl"][:], op=ALU.mult), [("hb", par), K_("gl")], [("ob", par)])
                ok = ("ob", par)
                P.dma("sp", mix_ap[ci // 8, 0:64, (ci % 8) * 512:(ci % 8 + 1) * 512], ob[par][:], [ok], ["mix_out"], ok)

            return [s0, s1, s2, s3, s4, s5]

        for p_ in range(NCH // 2):
            sa = stages(2 * p_)
            sb_ = stages(2 * p_ + 1)
            for k in range(len(sa)):
                sa[k]()
                sb_[k]()
        P.barrier()
        return P.emit()


C1_2PI = 6.28125
C2_2PI = float(2.0 * np.pi - 6.28125)
PI_SAFE = 3.1415925


def emit_A_ret(nc, P, hT_ap, wa, pa, cst, pos_ap, mix_ap):
    import os
    STAGE = float(os.environ.get("RET_STAGE", "9"))
    with ExitStack() as es:
        C = Ctx(nc, es, P)
        stg = {"i": 0, "t": [C.sb("stg%d" % i, [128, 1024]) for i in range(2)]}
        pat = C.sb("pat", [128, 16])
        P.dma("sp", pat[:], pa, [], ["pat"], "pat")
        cs = C.sb("cs", [128, 416])
        P.dma("sp", cs[:], cst, [], ["cs"], "cs")
        decT = cs[:, 0:128]
        qwbc = cs[0:64, 128:256]
        invf = cs[:, 256:288]
        wr = C.sb("wr", [128, 8, 256], BF16)
        load_weight_bf16(P, C, wa[:, 514:770], D, 256, wr, "wr", stg)
        identb = C.sb("identb", [128, 128], BF16)
        make_ident(P, identb, "identb")
        identf = C.sb("identf", [128, 128])
        make_ident(P, identf, "identf")
        Tps = C.ps("T", [128, 4, 256])
        trq_ = C.ps("trq", [128, 1024], BF16)
        trq = trq_[0:64, 0:256]
        scp_ = C.ps("scp", [128, 512])
        scp = scp_[:, 0:128]
        Yp_ = C.ps("Yp", [128, 512])
        Yp = Yp_[:, 0:256]
        Up_ = C.ps("Up", [128, 512])
        Up = Up_[0:64, 0:64]
        tro_ = C.ps("tro", [128, 1024], BF16)
        tro = tro_[0:64, 0:512]
        posi = C.sb("posi", [128, 128], I32)
        posf = C.sb("posf", [128, 128])
        post = C.sb("post", [128, 128])
        P.dma("sp", posi[:], pos_ap.rearrange("(n p) -> n p", p=128), [], ["posi"], "posi")
        P.op("dve", lambda e: e.tensor_copy(out=posf[:], in_=posi[:]), ["posi"], ["posf"])
        P.op("pe", lambda e: e.matmul(scp, lhsT=posf[:], rhs=identf[:], start=True, stop=True), ["posf", "identf"], ["scp"])
        P.op("act", lambda e: e.copy(out=post[:], in_=scp), ["scp"], ["post"])
        Sf = C.sb("Sf", [64, 64])
        Sb = C.sb("Sb", [64, 64], BF16)
        P.op("pool", lambda e: e.memset(Sf[:], 0.0), [], ["Sf"])
        P.op("pool", lambda e: e.memset(Sb[:], 0.0), [], ["Sb"])
        hc = [C.sb("hc%d" % i, [128, 8, 512], BF16) for i in range(2)]
        ang = C.sb("ang", [128, 4, 32])
        yy = C.sb("yy", [128, 4, 32])
        yi = C.sb("yi", [128, 4, 32], I32)
        rr = C.sb("rr", [128, 4, 32])
        ar = C.sb("ar", [128, 4, 32])
        sin2 = C.sb("sin2", [128, 4, 2, 32])
        cos2 = C.sb("cos2", [128, 4, 2, 32])
        t1 = C.sb("t1", [128, 2, 32])
        t2 = C.sb("t2", [128, 2, 32])
        t3 = C.sb("t3", [128, 2, 32])
        t4 = C.sb("t4", [128, 2, 32])
        rot = C.sb("rot", [128, 2, 2, 32])
        rot2 = C.sb("rot2", [128, 128])
        qkf = C.sb("qkf", [128, 128])
        qkb = C.sb("qkb", [128, 128], BF16)
        kwb = C.sb("kwb", [128, 64], BF16)
        vb = C.sb("vb", [128, 64], BF16)
        qkT = C.sb("qkT", [64, 256], BF16)
        qwT = C.sb("qwT", [64, 128], BF16)
        sm = C.sb("sm", [128, 128], BF16)
        scf = C.sb("scf", [128, 128])
        qf32 = C.sb("qf32", [64, 128])
        sgl = C.sb("sgl", [128, 4, 64])
        st6 = C.sb("st6", [128, 4, 6])
        mv = C.sb("mv", [128, 4, 2])
        ve = C.sb("ve", [128, 4])
        yn = C.sb("yn", [128, 4, 64])
        obf = C.sb("obf", [128, 4, 64], BF16)
        oT = [C.sb("oT%d" % i, [64, 512], BF16) for i in range(2)]
        for ci in range(NCH):
            par = ci % 2
            hk = ("hc", par)
            P.dma("sp", hc[par][:], hT_chunk(hT_ap, ci), [], [hk], hk)
            for sub in range(4):
                for f in range(8):
                    P.op("pe", lambda e, sub=sub, f=f, par=par: e.matmul(Tps[:, sub, :], lhsT=hc[par][:, f, sub * 128:(sub + 1) * 128],
                                                                          rhs=wr[:, f, :], start=(f == 0), stop=(f == 7)),
                         ["wr", hk], [("T", sub // 2)])
                n = 4 * ci + sub
                P.op("dve", lambda e, sub=sub, n=n: e.tensor_scalar(out=ang[:, sub, :], in0=invf, scalar1=post[:, n:n + 1], scalar2=None,
                                                                    op0=ALU.mult), ["cs", "post"], ["ang"])
            if STAGE < 2:
                continue
            P.op("dve", lambda e: e.tensor_scalar(out=yy[:], in0=ang[:], scalar1=float(1.0 / (2.0 * np.pi)), scalar2=None, op0=ALU.mult), ["ang"], ["yy"])
            P.op("dve", lambda e: e.tensor_copy(out=yi[:], in_=yy[:]), ["yy"], ["yi"])
            P.op("dve", lambda e: e.tensor_copy(out=yy[:], in_=yi[:]), ["yi"], ["yy"])
            P.op("dve", lambda e: e.scalar_tensor_tensor(out=rr[:], in0=yy[:], scalar=-C1_2PI, in1=ang[:], op0=ALU.mult, op1=ALU.add), ["yy", "ang"], ["rr"])
            P.op("dve", lambda e: e.scalar_tensor_tensor(out=rr[:], in0=yy[:], scalar=-C2_2PI, in1=rr[:], op0=ALU.mult, op1=ALU.add), ["yy", "rr"], ["rr"])
            P.op("dve", lambda e: e.tensor_scalar(out=rr[:], in0=rr[:], scalar1=PI_SAFE, scalar2=-PI_SAFE, op0=ALU.min, op1=ALU.max), ["rr"], ["rr"])
            P.op("dve", lambda e: e.scalar_tensor_tensor(out=ar[:], in0=rr[:], scalar=-1.0, in1=rr[:], op0=ALU.mult, op1=ALU.max), ["rr"], ["ar"])
            P.op("dve", lambda e: e.tensor_scalar(out=ar[:], in0=ar[:], scalar1=-1.0, scalar2=float(np.pi / 2), op0=ALU.mult, op1=ALU.add), ["ar"], ["ar"])
            for k in range(2):
                P.op("act", lambda e, k=k: e.activation(out=sin2[:, :, k, :], in_=rr[:], func=AF.Sin), ["rr"], ["sin2"])
                P.op("act", lambda e, k=k: e.activation(out=cos2[:, :, k, :], in_=ar[:], func=AF.Sin), ["ar"], ["cos2"])
            if STAGE < 3:
                continue
            for sub in range(4):
                tk = ("T", sub // 2)
                P.op("act", lambda e, sub=sub: e.copy(out=qkf[:], in_=Tps[:, sub, 0:128]), [tk], ["qkf"])
                cs_ = cos2[:, sub, 0, :]
                sn_ = sin2[:, sub, 0, :]
                if STAGE < 3.05:
                    continue
                for a_ in range(2):
                    x1 = qkf[:, a_ * 64:a_ * 64 + 32]
                    x2 = qkf[:, a_ * 64 + 32:a_ * 64 + 64]
                    o1 = rot2[:, a_ * 64:a_ * 64 + 32]
                    o2 = rot2[:, a_ * 64 + 32:a_ * 64 + 64]
                    P.op("pool", lambda e, x1=x1, cs_=cs_: e.tensor_tensor(out=t1[:, 0, :], in0=x1, in1=cs_, op=ALU.mult), ["qkf", "cos2"], ["t1"])
                    P.op("pool", lambda e, x2=x2, sn_=sn_: e.tensor_tensor(out=t2[:, 0, :], in0=x2, in1=sn_, op=ALU.mult), ["qkf", "sin2"], ["t2"])
                    P.op("pool", lambda e, x1=x1, sn_=sn_: e.tensor_tensor(out=t3[:, 0, :], in0=x1, in1=sn_, op=ALU.mult), ["qkf", "sin2"], ["t3"])
                    P.op("pool", lambda e, x2=x2, cs_=cs_: e.tensor_tensor(out=t4[:, 0, :], in0=x2, in1=cs_, op=ALU.mult), ["qkf", "cos2"], ["t4"])
                    P.op("pool", lambda e, o1=o1: e.tensor_tensor(out=o1, in0=t1[:, 0, :], in1=t2[:, 0, :], op=ALU.subtract), ["t1", "t2"], ["rot0"])
                    P.op("pool", lambda e, o2=o2: e.tensor_tensor(out=o2, in0=t3[:, 0, :], in1=t4[:, 0, :], op=ALU.add), ["t3", "t4"], ["rot1"])
                if STAGE < 3.25:
                    continue
                rotf = rot2[:]
                P.op("pool", lambda e, rotf=rotf: e.tensor_copy(out=qkb[:], in_=rotf), ["rot0", "rot1"], ["qkb"])
                P.op("dve", lambda e, rotf=rotf: e.tensor_scalar(out=kwb[:], in0=rotf[:, 64:128], scalar1=pat[:, 11:12], scalar2=None, op0=ALU.mult),
                     ["rot0", "rot1", "pat"], ["kwb"])
                P.op("act", lambda e, sub=sub: e.copy(out=vb[:], in_=Tps[:, sub, 128:192]), [tk], ["vb"])
                if STAGE < 4:
                    continue
                P.op("pe", lambda e: e.transpose(trq[:, 0:128], qkb[:, 0:64], identb[:]), ["qkb", "identb"], ["trq"])
                P.op("pe", lambda e: e.transpose(trq[:, 128:256], qkb[:, 64:128], identb[:]), ["qkb", "identb"], ["trq"])
                P.op("act", lambda e: e.copy(out=qkT[:], in_=trq), ["trq"], ["qkT"])
                P.op("act", lambda e: e.copy(out=qf32[:], in_=trq[:, 0:128]), ["trq"], ["qf32"])
                P.op("pool", lambda e: e.tensor_tensor(out=qwT[:], in0=qf32[:], in1=qwbc, op=ALU.mult), ["qf32", "cs"], ["qwT"])
                P.op("pe", lambda e: e.matmul(scp, lhsT=qkT[:, 128:256], rhs=qkT[:, 0:128], start=True, stop=True), ["qkT"], ["scp"])
                P.op("act", lambda e: e.copy(out=scf[:], in_=scp), ["scp"], ["scf"])
                P.op("pool", lambda e: e.tensor_tensor(out=sm[:], in0=scf[:], in1=decT, op=ALU.mult), ["scf", "cs"], ["sm"])
                yk = "Y"
                P.op("pe", lambda e, sub=sub: e.matmul(Yp[:, sub * 64:(sub + 1) * 64], lhsT=sm[:], rhs=vb[:], start=True, stop=False), ["sm", "vb"], [yk])
                P.op("pe", lambda e, sub=sub: e.matmul(Yp[:, sub * 64:(sub + 1) * 64], lhsT=qwT[:], rhs=Sb[:], start=False, stop=True), ["qwT", "Sb"], [yk])
                P.op("pe", lambda e: e.matmul(Up, lhsT=kwb[:], rhs=vb[:], start=True, stop=True), ["kwb", "vb"], ["Up"])
                P.op("dve", lambda e: e.scalar_tensor_tensor(out=Sf[:], in0=Sf[:], scalar=pat[0:64, 8:9], in1=Up, op0=ALU.mult, op1=ALU.add),
                     ["Sf", "Up", "pat"], ["Sf"])
                P.op("act", lambda e: e.copy(out=Sb[:], in_=Sf[:]), ["Sf"], ["Sb"])
            if STAGE < 5:
                continue
            for hb_ in range(2):
                P.op("act", lambda e, hb_=hb_: e.activation(out=sgl[:, 2 * hb_:2 * hb_ + 2, :], in_=Tps[:, 2 * hb_:2 * hb_ + 2, 192:256], func=AF.Silu),
                     [("T", hb_)], [("sgl", hb_)])
            for sub in range(4):
                P.op("dve", lambda e, sub=sub: e.bn_stats(out=st6[:, sub, :], in_=Yp[:, sub * 64:(sub + 1) * 64]), ["Y"], [("st6", sub)])
                P.op("dve", lambda e, sub=sub: e.bn_aggr(out=mv[:, sub, :], in_=st6[:, sub, :]), [("st6", sub)], [("mv", sub)])
            mvk = [("mv", s_) for s_ in range(4)]
            P.op("dve", lambda e: e.tensor_scalar(out=ve[:], in0=mv[:, :, 1], scalar1=EPS, scalar2=None, op0=ALU.add), mvk, ["ve"])
            P.op("act", lambda e: e.activation(out=ve[:], in_=ve[:], func=AF.Sqrt), ["ve"], ["ve"])
            P.op("dve", lambda e: e.reciprocal(out=ve[:], in_=ve[:]), ["ve"], ["ve"])
            for sub in range(4):
                P.op("dve", lambda e, sub=sub: e.tensor_scalar(out=yn[:, sub, :], in0=Yp[:, sub * 64:(sub + 1) * 64], scalar1=mv[:, sub, 0:1],
                                                               scalar2=ve[:, sub:sub + 1], op0=ALU.subtract, op1=ALU.mult),
                     ["Y", ("mv", sub), "ve"], [("yn", sub)])
            P.op("pool", lambda e: e.tensor_tensor(out=obf[:], in0=yn[:], in1=sgl[:], op=ALU.mult), [("yn", s_) for s_ in range(4)] + [("sgl", 0), ("sgl", 1)], ["obf"])
            for sub in range(4):
                P.op("pe", lambda e, sub=sub: e.transpose(tro[:, sub * 128:(sub + 1) * 128], obf[:, sub, :], identb[:]), ["obf", "identb"], ["tro"])
            ok = ("oT", par)
            P.op("act", lambda e, par=par: e.copy(out=oT[par][:], in_=tro), ["tro"], [ok])
            P.dma("sp", mix_ap[ci // 8, 192:256, (ci % 8) * 512:(ci % 8 + 1) * 512], oT[par][:], [ok], ["mix_out"], ok)
        P.barrier()
        return P.emit()


def emit_A_fox(nc, P, hT_ap, wa, pa, cst, mix_ap, nch=NCH, gather_cb=None):
    with ExitStack() as es:
        C = Ctx(nc, es, P)
        stg = {"i": 0, "t": [C.sb("stg%d" % i, [128, 1024]) for i in range(2)]}
        pat = C.sb("pat", [128, 16])
        P.dma("sp", pat[:], pa, [], ["pat"], "pat")
        cs = C.sb("cs", [128, 128])
        P.dma("sp", cs[:], cst[:, 288:416], [], ["cs"], "cs")
        maskb = C.sb("maskb", [128, 128], BF16)
        P.op("pool", lambda e: e.tensor_copy(out=maskb[:], in_=cs[:]), ["cs"], ["maskb"])
        identb = C.sb("identb", [128, 128], BF16)
        make_ident(P, identb, "identb")
        onesf = C.sb("onesf", [128, 512])
        P.op("pool", lambda e: e.memset(onesf[:], 1.0), [], ["onesf"])
        nb = C.sb("nb", [128, 2])
        P.op("dve", lambda e: e.tensor_scalar(out=nb[:], in0=pat[:, 9:11], scalar1=-1.0, scalar2=None, op0=ALU.mult), ["pat"], ["nb"])
        wq = C.sb("wq", [128, 8, 130], BF16)
        wk = C.sb("wk", [128, 8, 128], BF16)
        wv = C.sb("wv", [128, 8, 128], BF16)
        load_weight_bf16(P, C, wa[:, 128:258], D, 130, wq, "wq", stg)
        load_weight_bf16(P, C, wa[:, 258:386], D, 128, wk, "wk", stg)
        load_weight_bf16(P, C, wa[:, 386:514], D, 128, wv, "wv", stg)
        KT = [C.sb("KT%d" % h, [65, SEQ], BF16) for h in range(2)]
        for h in range(2):
            P.op("pool", lambda e, h=h: e.memset(KT[h][64:65, :], 1.0), [], [("KT", h)])
        Vs = C.sb("Vs", [128, 128, 2, 65], BF16)
        P.op("pool", lambda e: e.memset(Vs[:], 1.0), [], ["Vs"])
        negc = C.sb("negc", [128, 128, 2])
        QT = [[C.sb("QT%d%d" % (h, p), [65, 512], BF16) for p in range(2)] for h in range(2)]
        crow = [[C.sb("crow%d%d" % (h, p), [65, 512]) for p in range(2)] for h in range(2)]
        e1 = C.sb("e1", [65, 512])
        Osb = C.sb("Osb", [65, 512])
        rcp = C.sb("rcp", [65, 512])
        ofb = [C.sb("ofb%d" % i, [64, 512], BF16) for i in range(2)]
        PTt = [C.sb("PT%d" % i, [128, 512], BF16) for i in range(3)]
        hc = [C.sb("hc%d" % i, [128, 8, 512], BF16) for i in range(2)]
        sbank = [C.ps("sbk%d" % i, [128, 512]) for i in range(3)]
        Ob = [C.ps("Ob%d" % i, [128, 512]) for i in range(2)]
        pj = [C.ps("pj%d" % i, [128, 512]) for i in range(2)]
        pmz = C.ps("pmz", [128, 512])
        pji = [0]

        def nextpj():
            i = pji[0] % 2
            pji[0] += 1
            return pj[i], ("pj", i)

        oi = 0
        pending = []
        for ci in range(nch):
            par = ci % 2
            hk = ("hc", par)
            P.dma("sp", hc[par][:], hT_chunk(hT_ap, ci), [], [hk], hk)
            for h in range(2):
                qk_ = ("QT", h, par)
                ck_ = ("crow", h, par)
                pq, pqk = nextpj()
                for f in range(8):
                    P.op("pe", lambda e, pq=pq, h=h, f=f, par=par: e.matmul(pq[0:65, :], lhsT=wq[:, f, h * 65:(h + 1) * 65], rhs=hc[par][:, f, :],
                                                                           start=(f == 0), stop=(f == 7)), ["wq", hk], [pqk])
                P.op("act", lambda e, pq=pq, h=h, par=par: e.mul(out=QT[h][par][0:64, :], in_=pq[0:64, :], mul=0.125), [pqk], [qk_])
                P.op("act", lambda e, pq=pq, h=h: e.activation(out=e1[64:65, :], in_=pq[64:65, :], func=AF.Exp, scale=-1.0, bias=nb[64:65, h:h + 1]),
                     [pqk, "nb"], ["e1"])
                P.op("act", lambda e: e.activation(out=e1[64:65, :], in_=e1[64:65, :], func=AF.Ln, bias=1.0), ["e1"], ["e1"])
                init = 0.0 if ci == 0 else crow[h][1 - par][64:65, 511:512]
                P.op("dve", lambda e, h=h, par=par, init=init: e.tensor_tensor_scan(out=crow[h][par][64:65, :], data0=onesf[64:65, :], data1=e1[64:65, :],
                                                                                    initial=init, op0=ALU.mult, op1=ALU.subtract),
                     ["e1", "onesf", ("crow", h, 1 - par)], [ck_])
                P.op("dve", lambda e, h=h, par=par: e.tensor_copy(out=QT[h][par][64:65, :], in_=crow[h][par][64:65, :]), [ck_], [qk_])
                pc, pck = nextpj()
                for sub in range(4):
                    P.op("pe", lambda e, pc=pc, h=h, par=par, sub=sub: e.matmul(pc[:, sub:sub + 1], lhsT=crow[h][par][64:65, sub * 128:(sub + 1) * 128],
                                                                               rhs=onesf[64:65, 0:1], start=True, stop=True), [ck_, "onesf"], [pck])
                P.op("dve", lambda e, pc=pc, h=h, ci=ci: e.tensor_scalar(out=negc[:, 4 * ci:4 * ci + 4, h], in0=pc[:, 0:4], scalar1=-1.0, scalar2=None,
                                                                        op0=ALU.mult), [pck], [("negc", h, ci)])
                pk_, pkk = nextpj()
                for f in range(8):
                    P.op("pe", lambda e, pk_=pk_, h=h, f=f, par=par: e.matmul(pk_[0:64, :], lhsT=wk[:, f, h * 64:(h + 1) * 64], rhs=hc[par][:, f, :],
                                                                             start=(f == 0), stop=(f == 7)), ["wk", hk], [pkk])
                P.op("act", lambda e, pk_=pk_, h=h, ci=ci: e.copy(out=KT[h][0:64, ci * 512:(ci + 1) * 512], in_=pk_[0:64, :]), [pkk], [("KT", h, ci)])
            for sub in range(4):
                pv, pvk = nextpj()
                for f in range(8):
                    P.op("pe", lambda e, pv=pv, sub=sub, f=f, par=par: e.matmul(pv[:, 0:128], lhsT=hc[par][:, f, sub * 128:(sub + 1) * 128], rhs=wv[:, f, :],
                                                                               start=(f == 0), stop=(f == 7)), ["wv", hk], [pvk])
                P.op("dve", lambda e, pv=pv, sub=sub, ci=ci: e.tensor_copy(out=Vs[:, 4 * ci + sub, :, 0:64],
                                                                           in_=pv[:, 0:128].rearrange("p (a d) -> p a d", a=2)), [pvk, "Vs"], [("Vs", ci)])
            for h in range(2):
                qk_ = ("QT", h, par)
                nj = 4 * ci + 4
                Okey = ("Ob", h)

                def S(j, h=h, par=par, ci=ci):
                    r = j - 4 * ci
                    q0 = max(0, r) * 128
                    bk = ("sbk", j % 3)
                    bank = sbank[j % 3]
                    diag = r >= 0
                    P.op("pe", lambda e: e.matmul(bank[:, q0:512], lhsT=KT[h][0:65, j * 128:(j + 1) * 128], rhs=QT[h][par][0:65, q0:512],
                                                  start=True, stop=not diag), [("KT", h), ("KT", h, j // 4), qk_], [bk])
                    if diag:
                        P.op("pe", lambda e: e.matmul(bank[:, q0:q0 + 128], lhsT=identb[:], rhs=maskb[:], start=False, stop=True),
                             ["identb", "maskb"], [bk])
                    P.op("act", lambda e: e.activation(out=PTt[j % 3][:, q0:512], in_=bank[:, q0:512], func=AF.Exp, bias=negc[:, j, h:h + 1]),
                         [bk, ("negc", h, j // 4)], [("PT", j % 3)])

                def PV(j, h=h, ci=ci, nj=nj):
                    r = j - 4 * ci
                    q0 = max(0, r) * 128
                    P.op("pe", lambda e: e.matmul(Ob[h][0:65, q0:512], lhsT=Vs[:, j, h, :], rhs=PTt[j % 3][:, q0:512],
                                                  start=(j == 0), stop=(j == nj - 1)), ["Vs", ("Vs", j // 4), ("PT", j % 3)], [Okey])

                S(0)
                S(1)
                for j in range(nj):
                    if j + 2 < nj:
                        S(j + 2)
                    PV(j)
                    if j == 1 and pending:
                        pending.pop(0)()
                def norm(h=h, ci=ci, Okey=Okey):
                    nonlocal oi
                    P.op("act", lambda e: e.copy(out=Osb[:], in_=Ob[h][0:65, :]), [Okey], ["Osb"])
                    P.op("dve", lambda e: e.reciprocal(out=rcp[64:65, :], in_=Osb[64:65, :]), ["Osb"], ["rcp"])
                    P.op("pe", lambda e: e.matmul(pmz[0:64, :], lhsT=onesf[64:65, 0:64], rhs=rcp[64:65, :], start=True, stop=True), ["onesf", "rcp"], ["pmz"])
                    ok = ("ofb", oi % 2)
                    o_t = ofb[oi % 2]
                    oi += 1
                    P.op("dve", lambda e: e.tensor_tensor(out=o_t[:], in0=Osb[0:64, :], in1=pmz[0:64, :], op=ALU.mult), ["Osb", "pmz"], [ok])
                    P.dma("sp", mix_ap[ci // 8, 64 + 64 * h:128 + 64 * h, (ci % 8) * 512:(ci % 8 + 1) * 512], o_t[:], [ok], [("mo", ci, h)], ok)
                pending.append(norm)
                if gather_cb is not None and h == 0 and ci % 8 == 0 and ci > 0:
                    gather_cb(ci // 8 - 1)
        while pending:
            pending.pop(0)()
        if gather_cb is not None:
            gather_cb(nch // 8 - 1)
            gather_cb(None)
        P.barrier()
        return P.emit()


def mix_perm():
    perm = []
    for s in range(4):
        perm += list(range(64 * s, 64 * s + 64)) + list(range(256 + 128 * s, 256 + 128 * s + 128)) \
            + list(range(768 + 64 * s, 768 + 64 * s + 64))
    return np.array(perm)


def a_weights(inp, l, s):
    w = inp["w_in"][l]
    A, B = 2 * s, 2 * s + 1
    cols = []
    cols += list(range(OFF[0] + 64 * s, OFF[0] + 64 * s + 64))
    cols += list(range(OFF[1] + 64 * s, OFF[1] + 64 * s + 64))
    cols += list(range(OFF[2] + 64 * A, OFF[2] + 64 * A + 64)) + [OFF[5] + A]
    cols += list(range(OFF[2] + 64 * B, OFF[2] + 64 * B + 64)) + [OFF[5] + B]
    cols += list(range(OFF[3] + 64 * A, OFF[3] + 64 * A + 64))
    cols += list(range(OFF[3] + 64 * B, OFF[3] + 64 * B + 64))
    cols += list(range(OFF[4] + 64 * A, OFF[4] + 64 * A + 64))
    cols += list(range(OFF[4] + 64 * B, OFF[4] + 64 * B + 64))
    for g in (6, 7, 8, 9):
        cols += list(range(OFF[g] + 64 * s, OFF[g] + 64 * s + 64))
    wa = np.ascontiguousarray(w[:, cols])
    wg2 = np.ascontiguousarray(np.concatenate([inp["w_rg"][l, s], inp["w_ig"][l, s]], axis=1))
    log_gamma = np.log1p(-np.exp2(-5.0 - np.arange(4, dtype=np.float32))).astype(np.float32)
    lg = log_gamma[s]
    idx = np.arange(128, dtype=np.float32)
    pa = np.zeros((128, 16), np.float32)
    sl = slice(64 * s, 64 * s + 64)
    for k in range(4):
        pa[0:64, k] = inp["conv_w"][l, k, sl]
    pa[0:64, 4] = inp["conv_b"][l, sl]
    pa[0:64, 5] = inp["b_rg"][l, sl]
    pa[0:64, 6] = inp["b_ig"][l, sl]
    pa[0:64, 7] = inp["lru_lambda"][l, sl]
    pa[:, 8] = np.exp(lg * np.float32(128.0))
    pa[:, 9] = inp["fox_b_f"][l, A]
    pa[:, 10] = inp["fox_b_f"][l, B]
    pa[:, 11] = np.exp(lg * (np.float32(127.0) - idx)) * np.float32(0.125)
    cst = np.zeros((128, 416), np.float32)
    diff = idx[:, None] - idx[None, :]
    decay = np.where(diff >= 0, np.exp(lg * np.maximum(diff, 0.0)), 0.0).astype(np.float32)
    cst[:, 0:128] = decay.T * np.float32(0.125)
    cst[:, 128:256] = np.exp(lg * (idx + 1.0))[None, :]
    half = 32
    cst[:, 256:288] = (np.float32(10000.0) ** (-np.arange(half, dtype=np.float32) / np.float32(half))).astype(np.float32)[None, :]
    kk = np.arange(128)
    cst[:, 288:416] = np.where(kk[:, None] > kk[None, :], -30000.0, 0.0)
    return wa, wg2, pa, cst


def _dt(nc, n, s, t=F32, k="ExternalInput"):
    return nc.dram_tensor(n, s, t, kind=k).ap()


GROUPS = [[0, 1, 2, 3], [4, 5, 6, 7]]


def build_fused(stop=99):
    from concourse.bass import DynSlice
    nc = bass.Bass("TRN2", target_bir_lowering=False)
    x = _dt(nc, "x", [TOK, D])
    mem = _dt(nc, "mem", [256, D])
    gm = _dt(nc, "gm", [D])
    pos = _dt(nc, "pos", [SEQ], I32)
    cst = _dt(nc, "cst", [128, 416])
    g0 = _dt(nc, "g0", [D])
    L = []
    for l in range(DEPTH):
        d = {"wa": _dt(nc, "wa%d" % l, [D, 770]), "wg2": _dt(nc, "wg2%d" % l, [64, 128]), "pa": _dt(nc, "pa%d" % l, [128, 16])}
        for n in ["w_out", "w_cq", "w_ck", "w_cv", "w_co"]:
            d[n] = _dt(nc, "%s%d" % (n, l), [D, D])
        for n in ["g_post_mix", "g_pre_cross", "g_post_cross", "g_pre_ffn", "g_post_ffn", "g_next"]:
            d[n] = _dt(nc, "%s%d" % (n, l), [D])
        d["w_gate"] = _dt(nc, "w_gate%d" % l, [D, DFF])
        d["w_up"] = _dt(nc, "w_up%d" % l, [D, DFF])
        d["w_down"] = _dt(nc, "w_down%d" % l, [DFF, D])
        L.append(d)
    out = _dt(nc, "out", [TOK, D], F32, "ExternalOutput")
    hT_own = nc.dram_tensor("hT_own", [8 * D, 512], BF16).ap()
    hT_all = nc.dram_tensor("hT_all", [32 * D, 512], BF16).ap()
    mix_c = nc.dram_tensor("mix_c", [4 * 256, TOK], BF16).ap()
    mixG = nc.dram_tensor("mixG", [16 * 256, TOK], BF16).ap()
    x2s = nc.dram_tensor("x2s", [TOK, D], F32).ap()
    x3s = nc.dram_tensor("x3s", [TOK, D], F32).ap()
    hT3 = hT_all
    mix3 = mix_c.rearrange("(j f) t -> j f t", j=4)
    mixGv = mixG.rearrange("(r j a p) t -> p r j a t", r=4, j=4, a=2, p=128)

    mixT_own = nc.dram_tensor("mixT_own", [D, TOK], BF16).ap()
    mixG5 = mixG.rearrange("(j a r p) t -> j a r p t", j=4, a=2, r=4, p=128)

    with ExitStack() as es:
        P = Prog(nc, es)

        def gather8(src, dst):
            for q in range(8):
                P.collective("AllGather", src[q * 128:(q + 1) * 128, :], dst[q * 512:(q + 1) * 512, :], GROUPS, ["a"], [("b", q)])
            P.cc_follow([("b", q) for q in range(8)])

        def gather(src, dst, rk, wk):
            gather8(src, dst)
            P.barrier()
            P.emit()

        def hT_gather(st):
            if st is None:
                P.cc_follow([("hTg", q) for q in range(8)])
            else:
                P.collective("AllGather", hT_own[st * 1024:(st + 1) * 1024, :], hT_all[st * 4096:(st + 1) * 4096, :], GROUPS,
                             [("hT_out", st)], [("hTg", st)])

        def mix_gather(j):
            if j is None:
                P.cc_follow([("b", q) for q in range(8)])
            else:
                rk = [("mo", c_, h_) for c_ in range(8 * j, 8 * j + 8) for h_ in range(2)]
                for q in (2 * j, 2 * j + 1):
                    P.collective("AllGather", mix_c[q * 128:(q + 1) * 128, :], mixG[q * 512:(q + 1) * 512, :], GROUPS, rk, [("b", q)])

        emit_P0(nc, P, x, g0, hT_own, TOK, gather_cb=hT_gather)
        for l in range(DEPTH):
            d = L[l]
            last = l == DEPTH - 1
            if stop <= 2 + 10 * l:
                return nc
            emit_A_lru(nc, P, hT3, d["wa"], d["wg2"], d["pa"], mix3)
            emit_A_ret(nc, P, hT3, d["wa"], d["pa"], cst, pos, mix3)
            emit_A_fox(nc, P, hT3, d["wa"], d["pa"], cst, mix3, gather_cb=mix_gather)
            sv = {}
            for r_ in range(4):
                def sel(e, r_=r_):
                    if "s" not in sv:
                        sv["s"] = e.snap(e.partition_id() % 4)
                    return e.dma_start(out=mixT_own[r_ * 256:(r_ + 1) * 256, :].rearrange("(a p) t -> a p t", a=2),
                                       in_=mixG5[DynSlice(sv["s"], 1), :, r_, :, :].rearrange("j a p t -> (j a) p t"))
                P.op("pool", sel, [("b", q) for q in range(8)], [("mo", r_)], dma=("mo", r_))
            P.barrier()
            P.emit()
            if stop <= 4 + 10 * l:
                return nc
            emit_B1(nc, P, mixT_own, x if l == 0 else x3s, mem, gm, d["w_out"], d["w_cq"], d["w_ck"], d["w_cv"], d["w_co"],
                    d["g_post_mix"], d["g_pre_cross"], d["g_post_cross"], x2s)
            emit_B2(nc, P, x2s, d["w_gate"], d["w_up"], d["w_down"], d["g_pre_ffn"], d["g_post_ffn"],
                    None if last else d["g_next"], out if last else x3s, None if last else hT_own, gather_cb=hT_gather)
    return nc


def kernel(**inp):
    inp = {k: np.asarray(v) for k, v in inp.items()}
    cores = list(range(8))
    perm = mix_perm()
    x = np.ascontiguousarray(inp["x"], dtype=np.float32)
    maps = []
    for c in cores:
        b, s = c // 4, c % 4
        m = {"x": np.ascontiguousarray(x[b, s * TOK:(s + 1) * TOK]), "mem": np.ascontiguousarray(inp["mem"][b]),
             "gm": inp["mem_norm_g"], "pos": np.ascontiguousarray(inp["positions"][b]).astype(np.int32),
             "g0": inp["pre_mix_g"][0]}
        for l in range(DEPTH):
            wa, wg2, pa, cst = a_weights(inp, l, s)
            m["cst"] = cst
            m["wa%d" % l] = wa
            m["wg2%d" % l] = wg2
            m["pa%d" % l] = pa
            m["w_out%d" % l] = np.ascontiguousarray(inp["w_out"][l][perm])
            for n in ["w_cq", "w_ck", "w_cv", "w_co", "w_gate", "w_up", "w_down"]:
                m["%s%d" % (n, l)] = np.ascontiguousarray(inp[n][l])
            m["g_post_mix%d" % l] = inp["post_mix_g"][l]
            m["g_pre_cross%d" % l] = inp["pre_cross_g"][l]
            m["g_post_cross%d" % l] = inp["post_cross_g"][l]
            m["g_pre_ffn%d" % l] = inp["pre_ffn_g"][l]
            m["g_post_ffn%d" % l] = inp["post_ffn_g"][l]
            m["g_next%d" % l] = inp["pre_mix_g"][min(l + 1, DEPTH - 1)]
        maps.append({k: np.ascontiguousarray(v) for k, v in m.items()})
    res = run_bass_kernel_spmd(build_fused(), maps, core_ids=cores)
    out = np.zeros((NB, SEQ, D), np.float32)
    for c in cores:
        out[c // 4, (c % 4) * TOK:(c % 4 + 1) * TOK] = res.results[c]["out"]
    return out
```

```python
import numpy as np
import ml_dtypes
from contextlib import ExitStack
import concourse.bass as bass
import concourse.mybir as mybir
from concourse.bass_utils import run_bass_kernel_spmd

F32 = mybir.dt.float32
BF16 = mybir.dt.bfloat16
I32 = mybir.dt.int32
AF = mybir.ActivationFunctionType
ALU = mybir.AluOpType
AX = mybir.AxisListType

D = 1024
SEQ = 16384
NB = 2
DEPTH = 2
TOK = 4096
DFF = 2816
NFF = DFF // 128
EPS = 1e-6
SPLIT = (256, 256, 512, 512, 512, 8, 256, 256, 256, 256)
OFF = np.concatenate([[0], np.cumsum(SPLIT)]).tolist()
PI = float(np.pi)


class Prog:
    ENGS = ("pe", "act", "dve", "pool", "sp")

    def __init__(self, nc, es, n_dma_sems=48):
        self.nc = nc
        self.eng_sem = {e: es.enter_context(nc.semaphore("s_" + e)) for e in self.ENGS}
        self.eng_cnt = {e: 0 for e in self.ENGS}
        self.dma_sems = [es.enter_context(nc.semaphore("d%d" % i)) for i in range(n_dma_sems)]
        self.dma_cnt = [0] * n_dma_sems
        self.dma_key2idx = {}
        self.waited = {e: {} for e in self.ENGS}
        self.cc_sem = es.enter_context(nc.semaphore("s_cc"))
        self.cc_scratch = es.enter_context(nc.sbuf_tensor("cc_scratch", [128, 8], F32))
        self.cc_cnt = 0
        self.ops = []
        self.state = {}
        self.last = {}
        self.pending_dma = []

    def dma_sem_for(self, key):
        if key not in self.dma_key2idx:
            idx = len(self.dma_key2idx)
            assert idx < len(self.dma_sems), "out of dma sems"
            self.dma_key2idx[key] = idx
        return self.dma_key2idx[key]

    def op(self, eng, fn, reads=(), writes=(), dma=None, extra=()):
        deps = set(extra)
        for k in reads:
            st = self.state.setdefault(k, {"w": None, "r": []})
            if st["w"] is not None:
                deps.add(st["w"])
        for k in writes:
            st = self.state.setdefault(k, {"w": None, "r": []})
            if st["w"] is not None:
                deps.add(st["w"])
            deps.update(st["r"])
        oid = len(self.ops)
        self.ops.append(dict(id=oid, eng=eng, fn=fn, deps=deps,
                             dma=None if dma is None else self.dma_sem_for(dma)))
        for k in reads:
            self.state[k]["r"].append(oid)
        for k in writes:
            self.state[k] = {"w": oid, "r": []}
        if dma is None:
            self.last[eng] = oid
        else:
            self.pending_dma.append(oid)
        return oid

    def dma(self, q, out, in_, reads, writes, semkey, **kw):
        return self.op(q, lambda e: e.dma_start(out=out, in_=in_, **kw), reads, writes, dma=semkey)

    def collective(self, kind, in_ap, out_ap, groups, reads, writes):
        def fn(e):
            return e.collective_compute(kind, ALU.bypass, replica_groups=groups, ins=[in_ap.opt()], outs=[out_ap.opt()])
        oid = self.op("pool", fn, reads, writes)
        self.ops[oid]["cc"] = True
        return oid

    def cc_follow(self, keys):
        scr = self.cc_scratch
        self.op("pool", lambda e: e.memset(scr[:], 0.0), list(keys), list(keys))

    def barrier(self):
        ids = list(self.last.values()) + list(self.pending_dma)
        for e in self.ENGS:
            self.op(e, lambda en: None, extra=ids)
        self.pending_dma = []
        self.state = {}

    def emit(self):
        ops = self.ops

        def pe_pe(a, b):
            return a["eng"] == "pe" and b["eng"] == "pe" and a["dma"] is None and b["dma"] is None

        needed = set()
        for o in ops:
            for d in o["deps"]:
                if not pe_pe(ops[d], o):
                    needed.add(d)
        for o in ops:
            if o.get("cc"):
                self.cc_cnt += 1
                o["sig"] = (self.cc_sem, self.cc_cnt, None)
            elif o["dma"] is not None:
                self.dma_cnt[o["dma"]] += 16
                o["sig"] = (self.dma_sems[o["dma"]], self.dma_cnt[o["dma"]], 16)
            elif o["id"] in needed:
                self.eng_cnt[o["eng"]] += 1
                o["sig"] = (self.eng_sem[o["eng"]], self.eng_cnt[o["eng"]], 1)
            else:
                o["sig"] = None
        per = {e: [] for e in self.ENGS}
        carry = {e: [] for e in self.ENGS}
        for o in ops:
            w = {}
            for d in o["deps"]:
                if pe_pe(ops[d], o):
                    continue
                sg = ops[d]["sig"]
                k = id(sg[0])
                if k not in w or w[k][1] < sg[1]:
                    w[k] = (sg[0], sg[1])
            wl = carry[o["eng"]]
            carry[o["eng"]] = []
            wd = self.waited[o["eng"]]
            for k, (s, v) in w.items():
                if wd.get(k, 0) >= v:
                    continue
                wd[k] = v
                wl.append((s, v))
            o["waits"] = wl
            per[o["eng"]].append(o)

        def replay(lst):
            def f(e):
                pend_sig = None
                for o in lst:
                    for (s, v) in o["waits"]:
                        e.wait_ge(s, v)
                    ins = o["fn"](e)
                    if o["sig"] is not None:
                        assert ins is not None, "signalling op must emit an instruction"
                        if o["sig"][2] is None:
                            ins.then_inc(o["sig"][0])
                        else:
                            ins.then_inc(o["sig"][0], o["sig"][2])
            return f

        with self.nc.Block() as block:
            if per["pe"]:
                block.tensor(replay(per["pe"]))
            if per["act"]:
                block.scalar(replay(per["act"]))
            if per["dve"]:
                block.vector(replay(per["dve"]))
            if per["pool"]:
                block.gpsimd(replay(per["pool"]))
            if per["sp"]:
                block.sync(replay(per["sp"]))
        n = {e: len(per[e]) for e in self.ENGS}
        self.ops = []
        self.state = {}
        self.last = {}
        self.pending_dma = []
        return n


class Ctx:
    def __init__(self, nc, es, P):
        self.nc, self.es, self.P = nc, es, P
        self.n = 0

    UID = [0]

    def sb(self, name, shape, dt=F32):
        Ctx.UID[0] += 1
        return self.es.enter_context(self.nc.sbuf_tensor("sb%d_%s" % (Ctx.UID[0], name), shape, dt))

    def ps(self, name, shape, dt=F32):
        Ctx.UID[0] += 1
        return self.es.enter_context(self.nc.psum_tensor("ps%d_%s" % (Ctx.UID[0], name), shape, dt))


def make_ident(P, ident, key="ident"):
    P.op("pool", lambda e: e.memset(ident[:], 1.0), [], [key])

    def sel(e):
        if getattr(P, "zero_reg", None) is None:
            P.zero_reg = e.to_reg(0.0)
        return e.affine_select(out=ident[:], in_=ident[:], pattern=[[-1, 128]], compare_op=ALU.is_equal,
                               fill=P.zero_reg, base=0, channel_multiplier=1)
    P.op("pool", sel, [key], [key])


def load_bcast_rows(P, C, name, src_row_ap, n):
    t = C.sb(name, [128, n])
    P.dma("sp", t[:], src_row_ap.partition_broadcast(128), [], [name], name)
    return t


def load_weight_bf16(P, C, w_ap, K, N, dst, dkey, stg, eng_cast="pool"):
    wv = w_ap.rearrange("(c p) n -> p c n", p=128)
    nk = K // 128
    i = 0
    for c in range(nk):
        for n0 in range(0, N, 1024):
            n1 = min(N, n0 + 1024)
            sk = ("stg", stg["i"] % 2)
            st = stg["t"][stg["i"] % 2]
            stg["i"] += 1
            P.dma("sp", st[:, 0:n1 - n0], wv[:, c, n0:n1], [], [sk], sk)
            if stg["i"] % 2 == 0:
                P.op("dve", lambda e, st=st, c=c, n0=n0, n1=n1: e.tensor_copy(out=dst[:, c, n0:n1], in_=st[:, 0:n1 - n0]),
                     [sk], [dkey])
            else:
                P.op("act", lambda e, st=st, c=c, n0=n0, n1=n1: e.copy(out=dst[:, c, n0:n1], in_=st[:, 0:n1 - n0]),
                     [sk], [dkey])


def rstd_of(P, C, src, skey, tag, junk, n=D):
    ss = C.tmp["ss"]
    P.op("act", lambda e: e.activation(out=junk[:, 0:n], in_=src, func=AF.Square, accum_out=ss[:, 0:1]),
         [skey], ["junk", "ss"])
    P.op("dve", lambda e: e.tensor_scalar(out=ss[:, 1:2], in0=ss[:, 0:1], scalar1=1.0 / n, scalar2=EPS,
                                          op0=ALU.mult, op1=ALU.add), ["ss"], ["ss1"])
    P.op("act", lambda e: e.activation(out=ss[:, 2:3], in_=ss[:, 1:2], func=AF.Sqrt), ["ss1"], ["ss2"])
    P.op("dve", lambda e: e.reciprocal(out=ss[:, 3:4], in_=ss[:, 2:3]), ["ss2"], ["ss3"])
    return ss[:, 3:4], "ss3"


def norm_T(P, C, src, skey, g_t, gkey, hbf, ident, trp, trkey, dstT, dkey, col0):
    r, rk = rstd_of(P, C, src, skey, "n", C.tmp["junk"])
    P.op("dve", lambda e: e.scalar_tensor_tensor(out=hbf[:], in0=src, scalar=r, in1=g_t[:],
                                                 op0=ALU.mult, op1=ALU.mult), [skey, rk, gkey], ["hbf"])
    for c in range(8):
        P.op("pe", lambda e, c=c: e.transpose(trp[:, c * 128:(c + 1) * 128], hbf[:, c * 128:(c + 1) * 128], ident[:]),
             ["hbf", "ident"], [trkey])
    P.op("act", lambda e: e.copy(out=dstT[:, :, col0:col0 + 128],
                                 in_=trp[:].rearrange("p (c t) -> p c t", c=8)), [trkey], [dkey])


def emit_P0(nc, P, x_ap, g_ap, hT_ap, ntok, gather_cb=None):
    with ExitStack() as es:
        C = Ctx(nc, es, P)
        C.tmp = {"ss": C.sb("ss", [128, 4]), "junk": C.sb("junk", [128, D], BF16)}
        ident = C.sb("ident", [128, 128], BF16)
        make_ident(P, ident)
        g_t = load_bcast_rows(P, C, "g0", g_ap, D)
        hbf = C.sb("hbf", [128, D], BF16)
        xs = [C.sb("x%d" % i, [128, D]) for i in range(2)]
        trp = [C.ps("tr%d" % i, [128, D], BF16) for i in range(2)]
        hT = [C.sb("hT%d" % i, [128, 8, 512], BF16) for i in range(2)]
        for st in range(ntok // 512):
            hk = ("hT", st % 2)
            for sub in range(4):
                i = st * 4 + sub
                xk = ("x", i % 2)
                P.dma("sp", xs[i % 2][:], x_ap[i * 128:(i + 1) * 128, :], [], [xk], xk)
                norm_T(P, C, xs[i % 2][:], xk, g_t, "g0", hbf, ident, trp[i % 2], ("tr", i % 2),
                       hT[st % 2], hk, sub * 128)
            P.dma("sp", hT_ap[st * 1024:(st + 1) * 1024, :].rearrange("(c p) t -> p c t", p=128), hT[st % 2][:], [hk], [("hT_out", st)], hk)
            if gather_cb is not None:
                gather_cb(st)
        if gather_cb is not None:
            gather_cb(None)
        P.barrier()
        return P.emit()


def emit_B1(nc, P, mixT_ap, x_ap, mem_ap, gm_ap, w_out, w_cq, w_ck, w_cv, w_co,
            g_post_mix, g_pre_cross, g_post_cross, x2_ap, mix_fn=None):
    with ExitStack() as es:
        C = Ctx(nc, es, P)
        C.tmp = {"ss": C.sb("ss", [128, 4]), "junk": C.sb("junk", [128, D], BF16)}
        ident = C.sb("ident", [128, 128], BF16)
        make_ident(P, ident)
        ones_bf = C.sb("ones_bf", [128, 128], BF16)
        P.op("pool", lambda e: e.memset(ones_bf[:], 1.0), [], ["ones_bf"])
        stg = {"i": 0, "t": [C.sb("stg%d" % i, [128, 1024]) for i in range(2)]}
        gpm = load_bcast_rows(P, C, "gpm", g_post_mix, D)
        gpc = load_bcast_rows(P, C, "gpc", g_pre_cross, D)
        gqc = load_bcast_rows(P, C, "gqc", g_post_cross, D)
        gmm = load_bcast_rows(P, C, "gmm", gm_ap, D)
        wo = C.sb("wo", [128, 8, D], BF16)
        wq = C.sb("wq", [128, 8, D], BF16)
        wc = C.sb("wc", [128, 8, D], BF16)
        wk = C.sb("wk", [128, 8, D], BF16)
        load_weight_bf16(P, C, w_ck, D, D, wk, "wk", stg)
        hbf = C.sb("hbf", [128, D], BF16)
        acc = [C.ps("acc%d" % i, [128, D]) for i in range(2)]
        trp = [C.ps("tr%d" % i, [128, D], BF16) for i in range(2)]
        mb = [C.ps("mb%d" % i, [128, 512]) for i in range(2)]
        memT = C.sb("memT", [128, 8, 256], BF16)
        xs = [C.sb("x%d" % i, [128, D]) for i in range(4)]
        for i in range(2):
            xk = ("x", i)
            P.dma("sp", xs[i][:], mem_ap[i * 128:(i + 1) * 128, :], [], [xk], xk)
            norm_T(P, C, xs[i][:], xk, gmm, "gmm", hbf, ident, trp[i], ("tr", i), memT, "memT", i * 128)
        kT = C.sb("kT", [128, 8, 256], BF16)
        for cc in range(8):
            for f in range(8):
                P.op("pe", lambda e, cc=cc, f=f: e.matmul(mb[cc % 2][:, 0:256], lhsT=wk[:, f, cc * 128:(cc + 1) * 128],
                                                           rhs=memT[:, f, :], start=(f == 0), stop=(f == 7)),
                     ["wk", "memT"], [("mb", cc % 2)])
            P.op("act", lambda e, cc=cc: e.copy(out=kT[:, cc, :], in_=mb[cc % 2][:, 0:256]), [("mb", cc % 2)], ["kT"])
        load_weight_bf16(P, C, w_cv, D, D, wk, "wk", stg)
        vm = C.sb("vm", [128, 2, D], BF16)
        for m in range(2):
            for half in range(2):
                for f in range(8):
                    P.op("pe", lambda e, m=m, half=half, f=f: e.matmul(
                        acc[m][:, half * 512:(half + 1) * 512], lhsT=memT[:, f, m * 128:(m + 1) * 128],
                        rhs=wk[:, f, half * 512:(half + 1) * 512], start=(f == 0), stop=(f == 7)),
                        ["wk", "memT"], [("acc", m)])
            P.op("act", lambda e, m=m: e.copy(out=vm[:, m, :], in_=acc[m][:]), [("acc", m)], ["vm"])
        load_weight_bf16(P, C, w_out, D, D, wo, "wo", stg)
        load_weight_bf16(P, C, w_cq, D, D, wq, "wq", stg)
        load_weight_bf16(P, C, w_co, D, D, wc, "wc", stg)
        mixT = [C.sb("mixT%d" % i, [128, 8, 512], BF16) for i in range(2)]
        h2T = C.sb("h2T", [128, 8, 512], BF16)
        qT = C.sb("qT", [128, 8, 512], BF16)
        PT = C.sb("PT", [128, 8, 512], BF16)
        oT = C.sb("oT", [128, 8, 512], BF16)
        rec = C.sb("rec", [128, 512])
        tmp = C.sb("tmp", [128, D])
        mix_q = "pool"
        if mix_fn is None:
            mix_q = "sp"
            mixv = mixT_ap.rearrange("(c p) t -> p c t", p=128)
            mix_fn = lambda e, st, r: mixv[:, 2 * r:2 * r + 2, st * 512:(st + 1) * 512]
        nst = TOK // 512
        for st in range(nst):
            mk = ("mixT", st % 2)
            for r_ in range(4):
                mkr = ("mixT", st % 2, r_)
                P.op(mix_q, lambda e, st=st, r_=r_: e.dma_start(out=mixT[st % 2][:, 2 * r_:2 * r_ + 2, :], in_=mix_fn(e, st, r_)), [], [mkr], dma=mkr)
            for sub in range(4):
                t0 = st * 512 + sub * 128
                xk = ("x", sub)
                ak = ("acc", sub % 2)
                a = acc[sub % 2]
                P.dma("sp", xs[sub][:], x_ap[t0:t0 + 128, :], [], [xk], xk)
                for half in range(2):
                    for c in range(8):
                        P.op("pe", lambda e, a=a, half=half, c=c, sub=sub, st=st: e.matmul(
                            a[:, half * 512:(half + 1) * 512], lhsT=mixT[st % 2][:, c, sub * 128:(sub + 1) * 128],
                            rhs=wo[:, c, half * 512:(half + 1) * 512], start=(c == 0), stop=(c == 7)),
                            [("mixT", st % 2, c // 2), "wo"], [ak])
                r, rk = rstd_of(P, C, a[:], ak, "y", C.tmp["junk"])
                P.op("dve", lambda e, a=a, r=r: e.scalar_tensor_tensor(out=tmp[:], in0=a[:], scalar=r, in1=gpm[:],
                                                                       op0=ALU.mult, op1=ALU.mult), [ak, rk, "gpm"], ["tmp"])
                P.op("pool", lambda e, sub=sub: e.tensor_tensor(out=xs[sub][:], in0=xs[sub][:], in1=tmp[:], op=ALU.add),
                     [xk, "tmp"], [xk])
                norm_T(P, C, xs[sub][:], xk, gpc, "gpc", hbf, ident, trp[sub % 2], ("tr", sub % 2), h2T, "h2T", sub * 128)
            for cc in range(8):
                for f in range(8):
                    P.op("pe", lambda e, cc=cc, f=f: e.matmul(mb[cc % 2][:], lhsT=wq[:, f, cc * 128:(cc + 1) * 128],
                                                               rhs=h2T[:, f, :], start=(f == 0), stop=(f == 7)),
                         ["wq", "h2T"], [("mb", cc % 2)])
                if cc % 2 == 0:
                    P.op("act", lambda e, cc=cc: e.copy(out=qT[:, cc, :], in_=mb[cc % 2][:]), [("mb", cc % 2)], [("qT", cc)])
                else:
                    P.op("dve", lambda e, cc=cc: e.tensor_copy(out=qT[:, cc, :], in_=mb[cc % 2][:]), [("mb", cc % 2)], [("qT", cc)])
            for h in range(4):
                for m in range(2):
                    bk = ("mb", m)
                    for dc in range(2):
                        P.op("pe", lambda e, h=h, m=m, dc=dc: e.matmul(
                            mb[m][:], lhsT=kT[:, 2 * h + dc, m * 128:(m + 1) * 128], rhs=qT[:, 2 * h + dc, :],
                            start=(dc == 0), stop=(dc == 1)), ["kT", ("qT", 2 * h + dc)], [bk])
                    P.op("act", lambda e, h=h, m=m: e.activation(out=PT[:, 2 * h + m, :], in_=mb[m][:], func=AF.Exp,
                                                                 scale=1.0 / 16.0), [bk], [("PT", 2 * h + m)])
                for m in range(2):
                    P.op("pe", lambda e, h=h, m=m: e.matmul(acc[0][:, 0:512], lhsT=ones_bf[:], rhs=PT[:, 2 * h + m, :],
                                                            start=(m == 0), stop=(m == 1)),
                         ["ones_bf", ("PT", 2 * h + m)], [("acc", 0)])
                P.op("dve", lambda e: e.reciprocal(out=rec[:], in_=acc[0][:, 0:512]), [("acc", 0)], ["rec"])
                for dc in range(2):
                    for m in range(2):
                        P.op("pe", lambda e, h=h, m=m, dc=dc: e.matmul(
                            acc[1][:, dc * 512:(dc + 1) * 512], lhsT=vm[:, m, (2 * h + dc) * 128:(2 * h + dc + 1) * 128],
                            rhs=PT[:, 2 * h + m, :], start=(m == 0), stop=(m == 1)),
                            ["vm", ("PT", 2 * h + m)], [("acc", 1)])
                for dc in range(2):
                    P.op("dve", lambda e, h=h, dc=dc: e.tensor_tensor(out=oT[:, 2 * h + dc, :],
                                                                      in0=acc[1][:, dc * 512:(dc + 1) * 512], in1=rec[:],
                                                                      op=ALU.mult), [("acc", 1), "rec"], [("oT", 2 * h + dc)])
            for sub in range(4):
                t0 = st * 512 + sub * 128
                xk = ("x", sub)
                ak = ("acc", sub % 2)
                a = acc[sub % 2]
                for half in range(2):
                    for c in range(8):
                        P.op("pe", lambda e, a=a, half=half, c=c, sub=sub: e.matmul(
                            a[:, half * 512:(half + 1) * 512], lhsT=oT[:, c, sub * 128:(sub + 1) * 128],
                            rhs=wc[:, c, half * 512:(half + 1) * 512], start=(c == 0), stop=(c == 7)),
                            [("oT", c), "wc"], [ak])
                r, rk = rstd_of(P, C, a[:], ak, "y", C.tmp["junk"])
                P.op("dve", lambda e, a=a, r=r: e.scalar_tensor_tensor(out=tmp[:], in0=a[:], scalar=r, in1=gqc[:],
                                                                       op0=ALU.mult, op1=ALU.mult), [ak, rk, "gqc"], ["tmp"])
                P.op("pool", lambda e, sub=sub: e.tensor_tensor(out=xs[sub][:], in0=xs[sub][:], in1=tmp[:], op=ALU.add),
                     [xk, "tmp"], [xk])
                P.dma("sp", x2_ap[t0:t0 + 128, :], xs[sub][:], [xk], ["x2_out"], xk)
        P.barrier()
        return P.emit()


def emit_B2(nc, P, x2_ap, w_gate, w_up, w_down, g_pre_ffn, g_post_ffn, g_next, x3_ap, hT_ap, gather_cb=None):
    ST = 512
    with ExitStack() as es:
        C = Ctx(nc, es, P)
        C.tmp = {"ss": C.sb("ss", [128, 4]), "junk": C.sb("junk", [128, D], BF16)}
        ident = C.sb("ident", [128, 128], BF16)
        make_ident(P, ident)
        stg = {"i": 0, "t": [C.sb("stg%d" % i, [128, 1024]) for i in range(2)]}
        gpf = load_bcast_rows(P, C, "gpf", g_pre_ffn, D)
        gqf = load_bcast_rows(P, C, "gqf", g_post_ffn, D)
        gnx = load_bcast_rows(P, C, "gnx", g_next, D) if g_next is not None else None
        wg = C.sb("wg", [128, 8, DFF], BF16)
        wu = C.sb("wu", [128, 8, DFF], BF16)
        wd = C.sb("wd", [128, NFF, D], BF16)
        load_weight_bf16(P, C, w_gate, D, DFF, wg, "wg", stg)
        load_weight_bf16(P, C, w_up, D, DFF, wu, "wu", stg)
        load_weight_bf16(P, C, w_down, DFF, D, wd, "wd", stg)
        hbf = C.sb("hbf", [128, D], BF16)
        acc = [C.ps("acc%d" % i, [128, D]) for i in range(2)]
        trp = [C.ps("tr%d" % i, [128, D], BF16) for i in range(2)]
        mb = [C.ps("mb%d" % i, [128, 512]) for i in range(2)]
        xs = [C.sb("x%d" % i, [128, D]) for i in range(2)]
        h3T = C.sb("h3T", [128, 8, ST], BF16)
        aT = C.sb("aT", [128, NFF, ST], BF16)
        sg = C.sb("sg", [128, ST])
        tmp = C.sb("tmp", [128, D])
        nsub = ST // 128
        xi = 0
        for st in range(TOK // ST):
            for sub in range(nsub):
                t0 = st * ST + sub * 128
                xk = ("x", xi % 2)
                xt = xs[xi % 2]
                P.dma("sp", xt[:], x2_ap[t0:t0 + 128, :], [], [xk], xk)
                norm_T(P, C, xt[:], xk, gpf, "gpf", hbf, ident, trp[xi % 2], ("tr", xi % 2), h3T, "h3T", sub * 128)
                xi += 1
            for fc in range(NFF):
                for f in range(8):
                    P.op("pe", lambda e, fc=fc, f=f: e.matmul(mb[0][:, 0:ST], lhsT=wg[:, f, fc * 128:(fc + 1) * 128],
                                                               rhs=h3T[:, f, :], start=(f == 0), stop=(f == 7)),
                         ["wg", "h3T"], [("mb", 0)])
                for f in range(8):
                    P.op("pe", lambda e, fc=fc, f=f: e.matmul(mb[1][:, 0:ST], lhsT=wu[:, f, fc * 128:(fc + 1) * 128],
                                                               rhs=h3T[:, f, :], start=(f == 0), stop=(f == 7)),
                         ["wu", "h3T"], [("mb", 1)])
                P.op("act", lambda e: e.activation(out=sg[:], in_=mb[0][:, 0:ST], func=AF.Silu), [("mb", 0)], ["sg"])
                P.op("dve", lambda e, fc=fc: e.tensor_tensor(out=aT[:, fc, :], in0=mb[1][:, 0:ST], in1=sg[:], op=ALU.mult),
                     [("mb", 1), "sg"], [("aT", fc)])
            for sub in range(nsub):
                t0 = st * ST + sub * 128
                xk = ("x", xi % 2)
                xt = xs[xi % 2]
                ak = ("acc", sub % 2)
                a = acc[sub % 2]
                P.dma("sp", xt[:], x2_ap[t0:t0 + 128, :], [], [xk], xk)
                for half in range(2):
                    for fc in range(NFF):
                        P.op("pe", lambda e, a=a, half=half, fc=fc, sub=sub: e.matmul(
                            a[:, half * 512:(half + 1) * 512], lhsT=aT[:, fc, sub * 128:(sub + 1) * 128],
                            rhs=wd[:, fc, half * 512:(half + 1) * 512], start=(fc == 0), stop=(fc == NFF - 1)),
                            [("aT", fc), "wd"], [ak])
                r, rk = rstd_of(P, C, a[:], ak, "y", C.tmp["junk"])
                P.op("dve", lambda e, a=a, r=r: e.scalar_tensor_tensor(out=tmp[:], in0=a[:], scalar=r, in1=gqf[:],
                                                                       op0=ALU.mult, op1=ALU.mult), [ak, rk, "gqf"], ["tmp"])
                P.op("pool", lambda e, xt=xt: e.tensor_tensor(out=xt[:], in0=xt[:], in1=tmp[:], op=ALU.add),
                     [xk, "tmp"], [xk])
                P.dma("sp", x3_ap[t0:t0 + 128, :], xt[:], [xk], ["x3_out"], xk)
                if gnx is not None:
                    norm_T(P, C, xt[:], xk, gnx, "gnx", hbf, ident, trp[xi % 2], ("tr", xi % 2), h3T, "h3T", sub * 128)
                xi += 1
            if gnx is not None:
                P.dma("sp", hT_ap[st * 1024:(st + 1) * 1024, :].rearrange("(c p) t -> p c t", p=128), h3T[:], ["h3T"], [("hT_out", st)], "h3T")
                if gather_cb is not None:
                    gather_cb(st)
        if gnx is not None and gather_cb is not None:
            gather_cb(None)
        P.barrier()
        return P.emit()


NCH = SEQ // 512


def hT_chunk(hT_ap, ci):
    base = (ci % 8) * 4096 + (ci // 8) * 1024
    return hT_ap[base:base + 1024, :].rearrange("(c p) t -> p c t", p=128)


def emit_A_lru(nc, P, hT_ap, wa, wg2, pa, mix_ap):
    with ExitStack() as es:
        C = Ctx(nc, es, P)
        stg = {"i": 0, "t": [C.sb("stg%d" % i, [128, 1024]) for i in range(2)]}
        pat = C.sb("pat", [128, 16])
        P.dma("sp", pat[:], pa, [], ["pat"], "pat")
        wl = C.sb("wl", [128, 8, 128], BF16)
        load_weight_bf16(P, C, wa[:, 0:128], D, 128, wl, "wl", stg)
        wgs = C.sb("wgs", [64, 128])
        wgb = C.sb("wgb", [64, 128], BF16)
        P.dma("sp", wgs[:], wg2, [], ["wgs"], "wgs")
        P.op("pool", lambda e: e.tensor_copy(out=wgb[:], in_=wgs[:]), ["wgs"], ["wgb"])
        kap = C.sb("kap", [64, 4])
        P.op("act", lambda e: e.activation(out=kap[:, 0:1], in_=pat[0:64, 7:8], func=AF.Exp, scale=-1.0), ["pat"], ["kap0"])
        P.op("act", lambda e: e.activation(out=kap[:, 1:2], in_=kap[:, 0:1], func=AF.Ln, bias=1.0), ["kap0"], ["kap1"])
        P.op("dve", lambda e: e.tensor_scalar(out=kap[:, 2:3], in0=kap[:, 1:2], scalar1=-8.0, scalar2=None, op0=ALU.mult), ["kap1"], ["kap2"])
        P.op("dve", lambda e: e.tensor_scalar(out=kap[:, 3:4], in0=kap[:, 1:2], scalar1=-16.0, scalar2=None, op0=ALU.mult), ["kap1"], ["kap3"])
        hc = [C.sb("hc%d" % i, [128, 8, 512], BF16) for i in range(2)]
        pm2 = [[C.ps("pm%d%d" % (p_, i), [128, 512]) for i in range(2)] for p_ in range(2)]
        pg2 = [[C.ps("pg%d%d" % (p_, i), [128, 512]) for i in range(2)] for p_ in range(2)]
        lxb2 = [C.sb("lxb%d" % p_, [64, 515]) for p_ in range(2)]
        P.op("pool", lambda e: e.memset(lxb2[0][:, 0:3], 0.0), [], [("lxt", 0)])
        names = ["xc", "sr", "si", "a", "a2", "sq", "ix", "u", "gl"]
        f32t2 = [{n: C.sb(n + str(p_), [64, 512]) for n in names} for p_ in range(2)]
        xcb2 = [C.sb("xcb%d" % p_, [64, 512], BF16) for p_ in range(2)]
        hb = [C.sb("hb%d" % i, [64, 512]) for i in range(2)]
        ob = [C.sb("ob%d" % i, [64, 512], BF16) for i in range(2)]

        def stages(ci):
            par = ci % 2
            T = f32t2[par]
            xcb = xcb2[par]
            pm = pm2[par]
            pg = pg2[par]
            lxb = lxb2[par]
            K_ = lambda n: (n, par)
            hk = ("hc", par)

            def s0():
                P.dma("sp", hc[par][:], hT_chunk(hT_ap, ci), [], [hk], hk)
                for g in range(2):
                    for f in range(8):
                        P.op("pe", lambda e, g=g, f=f: e.matmul(pm[g][0:64, :], lhsT=wl[:, f, g * 64:(g + 1) * 64], rhs=hc[par][:, f, :],
                                                                 start=(f == 0), stop=(f == 7)), ["wl", hk], [("pm", par, g)])
                P.op("act", lambda e: e.copy(out=lxb[:, 3:515], in_=pm[0][0:64, :]), [("pm", par, 0)], [("lxm", par)])
                if ci > 0:
                    P.op("dve", lambda e: e.tensor_copy(out=lxb[:, 0:3], in_=lxb2[1 - par][:, 512:515]), [("lxm", 1 - par)], [("lxt", par)])

            def s1():
                rk = [("lxm", par), ("lxt", par), "pat"]
                P.op("dve", lambda e: e.tensor_scalar(out=T["xc"][:], in0=lxb[:, 3:515], scalar1=pat[0:64, 3:4], scalar2=pat[0:64, 4:5],
                                                      op0=ALU.mult, op1=ALU.add), rk, [K_("xc")])
                for k in (2, 1, 0):
                    P.op("dve", lambda e, k=k: e.scalar_tensor_tensor(out=T["xc"][:], in0=lxb[:, k:k + 512], scalar=pat[0:64, k:k + 1],
                                                                      in1=T["xc"][:], op0=ALU.mult, op1=ALU.add), rk + [K_("xc")], [K_("xc")])
                P.op("pool", lambda e: e.tensor_copy(out=xcb[:], in_=T["xc"][:]), [K_("xc")], [K_("xcb")])
                for g in range(2):
                    P.op("pe", lambda e, g=g: e.matmul(pg[g][0:64, :], lhsT=wgb[:, g * 64:(g + 1) * 64], rhs=xcb[:], start=True, stop=True),
                         ["wgb", K_("xcb")], [("pg", par, g)])

            def s2():
                P.op("act", lambda e: e.activation(out=T["sr"][:], in_=pg[0][0:64, :], func=AF.Sigmoid, bias=pat[0:64, 5:6]), [("pg", par, 0), "pat"], [K_("sr")])
                P.op("act", lambda e: e.activation(out=T["si"][:], in_=pg[1][0:64, :], func=AF.Sigmoid, bias=pat[0:64, 6:7]), [("pg", par, 1), "pat"], [K_("si")])
                P.op("pool", lambda e: e.tensor_tensor(out=T["ix"][:], in0=T["si"][:], in1=T["xc"][:], op=ALU.mult), [K_("si"), K_("xc")], [K_("ix")])

            def s3():
                P.op("act", lambda e: e.activation(out=T["a"][:], in_=T["sr"][:], func=AF.Exp, scale=kap[:, 2:3]), [K_("sr"), "kap2"], [K_("a")])
                P.op("act", lambda e: e.activation(out=T["a2"][:], in_=T["sr"][:], func=AF.Exp, scale=kap[:, 3:4]), [K_("sr"), "kap3"], [K_("a2")])
                P.op("dve", lambda e: e.tensor_scalar(out=T["a2"][:], in0=T["a2"][:], scalar1=-1.0, scalar2=1.0, op0=ALU.mult, op1=ALU.add), [K_("a2")], [K_("a2")])

            def s4():
                P.op("act", lambda e: e.activation(out=T["sq"][:], in_=T["a2"][:], func=AF.Sqrt), [K_("a2")], [K_("sq")])
                P.op("dve", lambda e: e.tensor_tensor(out=T["u"][:], in0=T["sq"][:], in1=T["ix"][:], op=ALU.mult), [K_("sq"), K_("ix")], [K_("u")])
                init = 0.0 if ci == 0 else hb[1 - par][:, 511:512]
                P.op("dve", lambda e: e.tensor_tensor_scan(out=hb[par][:], data0=T["a"][:], data1=T["u"][:], initial=init,
                                                           op0=ALU.mult, op1=ALU.add), [K_("a"), K_("u"), ("hb", 1 - par)], [("hb", par)])

            def s5():
                P.op("act", lambda e: e.activation(out=T["gl"][:], in_=pm[1][0:64, :], func=AF.Gelu_apprx_tanh), [("pm", par, 1)], [K_("gl")])
                P.op("dve", lambda e: e.tensor_tensor(out=ob[par][:], in0=hb[par][:], in1=T["gl"][:], op=ALU.mult), [("hb", par), K_("gl")], [("ob", par)])
                ok = ("ob", par)
                P.dma("sp", mix_ap[ci // 8, 0:64, (ci % 8) * 512:(ci % 8 + 1) * 512], ob[par][:], [ok], ["mix_out"], ok)

            return [s0, s1, s2, s3, s4, s5]

        for p_ in range(NCH // 2):
            sa = stages(2 * p_)
            sb_ = stages(2 * p_ + 1)
            for k in range(len(sa)):
                sa[k]()
                sb_[k]()
        P.barrier()
        return P.emit()


C1_2PI = 6.28125
C2_2PI = float(2.0 * np.pi - 6.28125)
PI_SAFE = 3.1415925


def emit_A_ret(nc, P, hT_ap, wa, pa, cst, pos_ap, mix_ap):
    import os
    STAGE = float(os.environ.get("RET_STAGE", "9"))
    with ExitStack() as es:
        C = Ctx(nc, es, P)
        stg = {"i": 0, "t": [C.sb("stg%d" % i, [128, 1024]) for i in range(2)]}
        pat = C.sb("pat", [128, 16])
        P.dma("sp", pat[:], pa, [], ["pat"], "pat")
        cs = C.sb("cs", [128, 416])
        P.dma("sp", cs[:], cst, [], ["cs"], "cs")
        decT = cs[:, 0:128]
        qwbc = cs[0:64, 128:256]
        invf = cs[:, 256:288]
        wr = C.sb("wr", [128, 8, 256], BF16)
        load_weight_bf16(P, C, wa[:, 514:770], D, 256, wr, "wr", stg)
        identb = C.sb("identb", [128, 128], BF16)
        make_ident(P, identb, "identb")
        identf = C.sb("identf", [128, 128])
        make_ident(P, identf, "identf")
        Tps = C.ps("T", [128, 4, 256])
        trq2 = [C.ps("trq%d" % i, [128, 1024], BF16)[0:64, 0:256] for i in range(2)]
        scp2 = [C.ps("scp%d" % i, [128, 512])[:, 0:128] for i in range(2)]
        scp = scp2[0]
        Yp_ = C.ps("Yp", [128, 512])
        Yp = Yp_[:, 0:256]
        Up = Yp_[0:64, 256:320]
        tro_ = C.ps("tro", [128, 1024], BF16)
        tro = tro_[0:64, 0:512]
        posi = C.sb("posi", [128, 128], I32)
        posf = C.sb("posf", [128, 128])
        post = C.sb("post", [128, 128])
        P.dma("sp", posi[:], pos_ap.rearrange("(n p) -> n p", p=128), [], ["posi"], "posi")
        P.op("dve", lambda e: e.tensor_copy(out=posf[:], in_=posi[:]), ["posi"], ["posf"])
        P.op("pe", lambda e: e.matmul(scp, lhsT=posf[:], rhs=identf[:], start=True, stop=True), ["posf", "identf"], [("scp", 0)])
        P.op("act", lambda e: e.copy(out=post[:], in_=scp), [("scp", 0)], ["post"])
        Sf = C.sb("Sf", [64, 64])
        Sb = C.sb("Sb", [64, 64], BF16)
        P.op("pool", lambda e: e.memset(Sf[:], 0.0), [], ["Sf"])
        P.op("pool", lambda e: e.memset(Sb[:], 0.0), [], ["Sb"])
        hc = [C.sb("hc%d" % i, [128, 8, 512], BF16) for i in range(2)]
        ang = C.sb("ang", [128, 4, 32])
        yy = C.sb("yy", [128, 4, 32])
        yi = C.sb("yi", [128, 4, 32], I32)
        rr = C.sb("rr", [128, 4, 32])
        ar = C.sb("ar", [128, 4, 32])
        sin2 = C.sb("sin2", [128, 4, 2, 32])
        cos2 = C.sb("cos2", [128, 4, 2, 32])
        t1 = C.sb("t1", [128, 2, 32])
        t2 = C.sb("t2", [128, 2, 32])
        t3 = C.sb("t3", [128, 2, 32])
        t4 = C.sb("t4", [128, 2, 32])
        rot = C.sb("rot", [128, 2, 2, 32])
        rot2 = C.sb("rot2", [128, 128])
        qkf = C.sb("qkf", [128, 128])
        qkb = C.sb("qkb", [128, 128], BF16)
        kwb = C.sb("kwb", [128, 64], BF16)
        vb = C.sb("vb", [128, 64], BF16)
        qkT = C.sb("qkT", [64, 256], BF16)
        qwT = C.sb("qwT", [64, 128], BF16)
        sm = C.sb("sm", [128, 128], BF16)
        scf = C.sb("scf", [128, 128])
        qf32 = C.sb("qf32", [64, 128])
        sgl = C.sb("sgl", [128, 4, 64])
        st6 = C.sb("st6", [128, 4, 6])
        mv = C.sb("mv", [128, 4, 2])
        ve = C.sb("ve", [128, 4])
        yn = C.sb("yn", [128, 4, 64])
        obf = C.sb("obf", [128, 4, 64], BF16)
        oT = [C.sb("oT%d" % i, [64, 512], BF16) for i in range(2)]
        R2 = []
        for q_ in range(2):
            R2.append({"qkf": C.sb("qkf_%d" % q_, [128, 128]), "rot2": C.sb("rot2_%d" % q_, [128, 128]),
                       "t1": C.sb("t1_%d" % q_, [128, 32]), "t2": C.sb("t2_%d" % q_, [128, 32]),
                       "t3": C.sb("t3_%d" % q_, [128, 32]), "t4": C.sb("t4_%d" % q_, [128, 32]),
                       "qkb": C.sb("qkb_%d" % q_, [128, 128], BF16), "kwb": C.sb("kwb_%d" % q_, [128, 64], BF16),
                       "vb": C.sb("vb_%d" % q_, [128, 64], BF16), "qkT": C.sb("qkT_%d" % q_, [64, 256], BF16),
                       "qwT": C.sb("qwT_%d" % q_, [64, 128], BF16), "sm": C.sb("sm_%d" % q_, [128, 128], BF16),
                       "scf": C.sb("scf_%d" % q_, [128, 128]), "qf32": C.sb("qf32_%d" % q_, [64, 128])})
        for ci in range(NCH):
            par = ci % 2
            hk = ("hc", par)
            P.dma("sp", hc[par][:], hT_chunk(hT_ap, ci), [], [hk], hk)
            for sub in range(4):
                for f in range(8):
                    P.op("pe", lambda e, sub=sub, f=f, par=par: e.matmul(Tps[:, sub, :], lhsT=hc[par][:, f, sub * 128:(sub + 1) * 128],
                                                                          rhs=wr[:, f, :], start=(f == 0), stop=(f == 7)),
                         ["wr", hk], [("T", sub // 2)])
                n = 4 * ci + sub
                P.op("dve", lambda e, sub=sub, n=n: e.tensor_scalar(out=ang[:, sub, :], in0=invf, scalar1=post[:, n:n + 1], scalar2=None,
                                                                    op0=ALU.mult), ["cs", "post"], ["ang"])
            if STAGE < 2:
                continue
            P.op("dve", lambda e: e.tensor_scalar(out=yy[:], in0=ang[:], scalar1=float(1.0 / (2.0 * np.pi)), scalar2=None, op0=ALU.mult), ["ang"], ["yy"])
            P.op("dve", lambda e: e.tensor_copy(out=yi[:], in_=yy[:]), ["yy"], ["yi"])
            P.op("dve", lambda e: e.tensor_copy(out=yy[:], in_=yi[:]), ["yi"], ["yy"])
            P.op("dve", lambda e: e.scalar_tensor_tensor(out=rr[:], in0=yy[:], scalar=-C1_2PI, in1=ang[:], op0=ALU.mult, op1=ALU.add), ["yy", "ang"], ["rr"])
            P.op("dve", lambda e: e.scalar_tensor_tensor(out=rr[:], in0=yy[:], scalar=-C2_2PI, in1=rr[:], op0=ALU.mult, op1=ALU.add), ["yy", "rr"], ["rr"])
            P.op("dve", lambda e: e.tensor_scalar(out=rr[:], in0=rr[:], scalar1=PI_SAFE, scalar2=-PI_SAFE, op0=ALU.min, op1=ALU.max), ["rr"], ["rr"])
            P.op("dve", lambda e: e.scalar_tensor_tensor(out=ar[:], in0=rr[:], scalar=-1.0, in1=rr[:], op0=ALU.mult, op1=ALU.max), ["rr"], ["ar"])
            P.op("dve", lambda e: e.tensor_scalar(out=ar[:], in0=ar[:], scalar1=-1.0, scalar2=float(np.pi / 2), op0=ALU.mult, op1=ALU.add), ["ar"], ["ar"])
            for k in range(2):
                P.op("act", lambda e, k=k: e.activation(out=sin2[:, :, k, :], in_=rr[:], func=AF.Sin), ["rr"], ["sin2"])
                P.op("act", lambda e, k=k: e.activation(out=cos2[:, :, k, :], in_=ar[:], func=AF.Sin), ["ar"], ["cos2"])
            if STAGE < 3:
                continue
            def sub_stages(sub):
                q_ = sub % 2
                R = R2[q_]
                K = lambda n: (n, q_)
                trq = trq2[q_]
                scp = scp2[q_]
                tk = ("T", sub // 2)
                cs_ = cos2[:, sub, 0, :]
                sn_ = sin2[:, sub, 0, :]

                def sa():
                    P.op("act", lambda e: e.copy(out=R["qkf"][:], in_=Tps[:, sub, 0:128]), [tk], [K("qkf")])
                    for a_ in range(2):
                        x1 = R["qkf"][:, a_ * 64:a_ * 64 + 32]
                        x2 = R["qkf"][:, a_ * 64 + 32:a_ * 64 + 64]
                        o1 = R["rot2"][:, a_ * 64:a_ * 64 + 32]
                        o2 = R["rot2"][:, a_ * 64 + 32:a_ * 64 + 64]
                        P.op("pool", lambda e, x1=x1: e.tensor_tensor(out=R["t1"][:], in0=x1, in1=cs_, op=ALU.mult), [K("qkf"), "cos2"], [K("t1")])
                        P.op("pool", lambda e, x2=x2: e.tensor_tensor(out=R["t2"][:], in0=x2, in1=sn_, op=ALU.mult), [K("qkf"), "sin2"], [K("t2")])
                        P.op("pool", lambda e, x1=x1: e.tensor_tensor(out=R["t3"][:], in0=x1, in1=sn_, op=ALU.mult), [K("qkf"), "sin2"], [K("t3")])
                        P.op("pool", lambda e, x2=x2: e.tensor_tensor(out=R["t4"][:], in0=x2, in1=cs_, op=ALU.mult), [K("qkf"), "cos2"], [K("t4")])
                        P.op("pool", lambda e, o1=o1: e.tensor_tensor(out=o1, in0=R["t1"][:], in1=R["t2"][:], op=ALU.subtract), [K("t1"), K("t2")], [K("rot0")])
                        P.op("pool", lambda e, o2=o2: e.tensor_tensor(out=o2, in0=R["t3"][:], in1=R["t4"][:], op=ALU.add), [K("t3"), K("t4")], [K("rot1")])

                def sb_():
                    P.op("pool", lambda e: e.tensor_copy(out=R["qkb"][:], in_=R["rot2"][:]), [K("rot0"), K("rot1")], [K("qkb")])
                    P.op("dve", lambda e: e.tensor_scalar(out=R["kwb"][:], in0=R["rot2"][:, 64:128], scalar1=pat[:, 11:12], scalar2=None, op0=ALU.mult),
                         [K("rot0"), K("rot1"), "pat"], [K("kwb")])
                    P.op("act", lambda e: e.copy(out=R["vb"][:], in_=Tps[:, sub, 128:192]), [tk], [K("vb")])

                def sc():
                    P.op("pe", lambda e: e.transpose(trq[:, 0:128], R["qkb"][:, 0:64], identb[:]), [K("qkb"), "identb"], [K("trq")])
                    P.op("pe", lambda e: e.transpose(trq[:, 128:256], R["qkb"][:, 64:128], identb[:]), [K("qkb"), "identb"], [K("trq")])
                    P.op("act", lambda e: e.copy(out=R["qkT"][:], in_=trq), [K("trq")], [K("qkT")])
                    P.op("act", lambda e: e.copy(out=R["qf32"][:], in_=trq[:, 0:128]), [K("trq")], [K("qf32")])
                    P.op("pool", lambda e: e.tensor_tensor(out=R["qwT"][:], in0=R["qf32"][:], in1=qwbc, op=ALU.mult), [K("qf32"), "cs"], [K("qwT")])

                def sd():
                    P.op("pe", lambda e: e.matmul(scp, lhsT=R["qkT"][:, 128:256], rhs=R["qkT"][:, 0:128], start=True, stop=True), [K("qkT")], [K("scp")])
                    P.op("act", lambda e: e.copy(out=R["scf"][:], in_=scp), [K("scp")], [K("scf")])
                    P.op("pool", lambda e: e.tensor_tensor(out=R["sm"][:], in0=R["scf"][:], in1=decT, op=ALU.mult), [K("scf"), "cs"], [K("sm")])

                def sf():
                    yk = "Y"
                    P.op("pe", lambda e: e.matmul(Yp[:, sub * 64:(sub + 1) * 64], lhsT=R["sm"][:], rhs=R["vb"][:], start=True, stop=False), [K("sm"), K("vb")], [yk])
                    P.op("pe", lambda e: e.matmul(Yp[:, sub * 64:(sub + 1) * 64], lhsT=R["qwT"][:], rhs=Sb[:], start=False, stop=True), [K("qwT"), "Sb"], [yk])
                    P.op("pe", lambda e: e.matmul(Up, lhsT=R["kwb"][:], rhs=R["vb"][:], start=True, stop=True), [K("kwb"), K("vb")], ["Up"])
                    P.op("dve", lambda e: e.scalar_tensor_tensor(out=Sf[:], in0=Sf[:], scalar=pat[0:64, 8:9], in1=Up, op0=ALU.mult, op1=ALU.add),
                         ["Sf", "Up", "pat"], ["Sf"])
                    P.op("act", lambda e: e.copy(out=Sb[:], in_=Sf[:]), ["Sf"], ["Sb"])

                return [sa, sb_, sc, sd, sf]

            for pr in ((0, 1), (2, 3)):
                sA = sub_stages(pr[0])
                sB = sub_stages(pr[1])
                for k_ in range(5):
                    sA[k_]()
                    sB[k_]()
            if STAGE < 5:
                continue
            for hb_ in range(2):
                P.op("act", lambda e, hb_=hb_: e.activation(out=sgl[:, 2 * hb_:2 * hb_ + 2, :], in_=Tps[:, 2 * hb_:2 * hb_ + 2, 192:256], func=AF.Silu),
                     [("T", hb_)], [("sgl", hb_)])
            for sub in range(4):
                P.op("dve", lambda e, sub=sub: e.bn_stats(out=st6[:, sub, :], in_=Yp[:, sub * 64:(sub + 1) * 64]), ["Y"], [("st6", sub)])
                P.op("dve", lambda e, sub=sub: e.bn_aggr(out=mv[:, sub, :], in_=st6[:, sub, :]), [("st6", sub)], [("mv", sub)])
            mvk = [("mv", s_) for s_ in range(4)]
            P.op("dve", lambda e: e.tensor_scalar(out=ve[:], in0=mv[:, :, 1], scalar1=EPS, scalar2=None, op0=ALU.add), mvk, ["ve"])
            P.op("act", lambda e: e.activation(out=ve[:], in_=ve[:], func=AF.Sqrt), ["ve"], ["ve"])
            P.op("dve", lambda e: e.reciprocal(out=ve[:], in_=ve[:]), ["ve"], ["ve"])
            for sub in range(4):
                P.op("dve", lambda e, sub=sub: e.tensor_scalar(out=yn[:, sub, :], in0=Yp[:, sub * 64:(sub + 1) * 64], scalar1=mv[:, sub, 0:1],
                                                               scalar2=ve[:, sub:sub + 1], op0=ALU.subtract, op1=ALU.mult),
                     ["Y", ("mv", sub), "ve"], [("yn", sub)])
            P.op("pool", lambda e: e.tensor_tensor(out=obf[:], in0=yn[:], in1=sgl[:], op=ALU.mult), [("yn", s_) for s_ in range(4)] + [("sgl", 0), ("sgl", 1)], ["obf"])
            for sub in range(4):
                P.op("pe", lambda e, sub=sub: e.transpose(tro[:, sub * 128:(sub + 1) * 128], obf[:, sub, :], identb[:]), ["obf", "identb"], ["tro"])
            ok = ("oT", par)
            P.op("act", lambda e, par=par: e.copy(out=oT[par][:], in_=tro), ["tro"], [ok])
            P.dma("sp", mix_ap[ci // 8, 192:256, (ci % 8) * 512:(ci % 8 + 1) * 512], oT[par][:], [ok], ["mix_out"], ok)
        P.barrier()
        return P.emit()


def emit_A_fox(nc, P, hT_ap, wa, pa, cst, mix_ap, nch=NCH, gather_cb=None):
    with ExitStack() as es:
        C = Ctx(nc, es, P)
        stg = {"i": 0, "t": [C.sb("stg%d" % i, [128, 1024]) for i in range(2)]}
        pat = C.sb("pat", [128, 16])
        P.dma("sp", pat[:], pa, [], ["pat"], "pat")
        cs = C.sb("cs", [128, 128])
        P.dma("sp", cs[:], cst[:, 288:416], [], ["cs"], "cs")
        maskb = C.sb("maskb", [128, 128], BF16)
        P.op("pool", lambda e: e.tensor_copy(out=maskb[:], in_=cs[:]), ["cs"], ["maskb"])
        identb = C.sb("identb", [128, 128], BF16)
        make_ident(P, identb, "identb")
        onesf = C.sb("onesf", [128, 512])
        P.op("pool", lambda e: e.memset(onesf[:], 1.0), [], ["onesf"])
        nb = C.sb("nb", [128, 2])
        P.op("dve", lambda e: e.tensor_scalar(out=nb[:], in0=pat[:, 9:11], scalar1=-1.0, scalar2=None, op0=ALU.mult), ["pat"], ["nb"])
        wq = C.sb("wq", [128, 8, 130], BF16)
        wk = C.sb("wk", [128, 8, 128], BF16)
        wv = C.sb("wv", [128, 8, 128], BF16)
        load_weight_bf16(P, C, wa[:, 128:258], D, 130, wq, "wq", stg)
        load_weight_bf16(P, C, wa[:, 258:386], D, 128, wk, "wk", stg)
        load_weight_bf16(P, C, wa[:, 386:514], D, 128, wv, "wv", stg)
        KT = [C.sb("KT%d" % h, [65, SEQ], BF16) for h in range(2)]
        for h in range(2):
            P.op("pool", lambda e, h=h: e.memset(KT[h][64:65, :], 1.0), [], [("KT", h)])
        Vs = C.sb("Vs", [128, 128, 2, 65], BF16)
        P.op("pool", lambda e: e.memset(Vs[:], 1.0), [], ["Vs"])
        negc = C.sb("negc", [128, 128, 2])
        QT = [[C.sb("QT%d%d" % (h, p), [65, 512], BF16) for p in range(2)] for h in range(2)]
        crow = [[C.sb("crow%d%d" % (h, p), [65, 512]) for p in range(2)] for h in range(2)]
        e1 = C.sb("e1", [65, 512])
        Osb = C.sb("Osb", [65, 512])
        rcp = C.sb("rcp", [65, 512])
        ofb = [C.sb("ofb%d" % i, [64, 512], BF16) for i in range(2)]
        PTt = [C.sb("PT%d" % i, [128, 512], BF16) for i in range(3)]
        hc = [C.sb("hc%d" % i, [128, 8, 512], BF16) for i in range(2)]
        sbank = [C.ps("sbk%d" % i, [128, 512]) for i in range(3)]
        Ob = [C.ps("Ob%d" % i, [128, 512]) for i in range(2)]
        pj = [C.ps("pj%d" % i, [128, 512]) for i in range(2)]
        pmz = C.ps("pmz", [128, 512])
        pji = [0]

        def nextpj():
            i = pji[0] % 2
            pji[0] += 1
            return pj[i], ("pj", i)

        oi = 0
        pending = []
        for ci in range(nch):
            par = ci % 2
            hk = ("hc", par)
            P.dma("sp", hc[par][:], hT_chunk(hT_ap, ci), [], [hk], hk)
            for h in range(2):
                qk_ = ("QT", h, par)
                ck_ = ("crow", h, par)
                pq, pqk = nextpj()
                for f in range(8):
                    P.op("pe", lambda e, pq=pq, h=h, f=f, par=par: e.matmul(pq[0:65, :], lhsT=wq[:, f, h * 65:(h + 1) * 65], rhs=hc[par][:, f, :],
                                                                           start=(f == 0), stop=(f == 7)), ["wq", hk], [pqk])
                P.op("act", lambda e, pq=pq, h=h, par=par: e.mul(out=QT[h][par][0:64, :], in_=pq[0:64, :], mul=0.125), [pqk], [qk_])
                P.op("act", lambda e, pq=pq, h=h: e.activation(out=e1[64:65, :], in_=pq[64:65, :], func=AF.Exp, scale=-1.0, bias=nb[64:65, h:h + 1]),
                     [pqk, "nb"], ["e1"])
                P.op("act", lambda e: e.activation(out=e1[64:65, :], in_=e1[64:65, :], func=AF.Ln, bias=1.0), ["e1"], ["e1"])
                init = 0.0 if ci == 0 else crow[h][1 - par][64:65, 511:512]
                P.op("dve", lambda e, h=h, par=par, init=init: e.tensor_tensor_scan(out=crow[h][par][64:65, :], data0=onesf[64:65, :], data1=e1[64:65, :],
                                                                                    initial=init, op0=ALU.mult, op1=ALU.subtract),
                     ["e1", "onesf", ("crow", h, 1 - par)], [ck_])
                P.op("dve", lambda e, h=h, par=par: e.tensor_copy(out=QT[h][par][64:65, :], in_=crow[h][par][64:65, :]), [ck_], [qk_])
                pc, pck = nextpj()
                for sub in range(4):
                    P.op("pe", lambda e, pc=pc, h=h, par=par, sub=sub: e.matmul(pc[:, sub:sub + 1], lhsT=crow[h][par][64:65, sub * 128:(sub + 1) * 128],
                                                                               rhs=onesf[64:65, 0:1], start=True, stop=True), [ck_, "onesf"], [pck])
                P.op("dve", lambda e, pc=pc, h=h, ci=ci: e.tensor_scalar(out=negc[:, 4 * ci:4 * ci + 4, h], in0=pc[:, 0:4], scalar1=-1.0, scalar2=None,
                                                                        op0=ALU.mult), [pck], [("negc", h, ci)])
                pk_, pkk = nextpj()
                for f in range(8):
                    P.op("pe", lambda e, pk_=pk_, h=h, f=f, par=par: e.matmul(pk_[0:64, :], lhsT=wk[:, f, h * 64:(h + 1) * 64], rhs=hc[par][:, f, :],
                                                                             start=(f == 0), stop=(f == 7)), ["wk", hk], [pkk])
                P.op("act", lambda e, pk_=pk_, h=h, ci=ci: e.copy(out=KT[h][0:64, ci * 512:(ci + 1) * 512], in_=pk_[0:64, :]), [pkk], [("KT", h, ci)])
            for sub in range(4):
                pv, pvk = nextpj()
                for f in range(8):
                    P.op("pe", lambda e, pv=pv, sub=sub, f=f, par=par: e.matmul(pv[:, 0:128], lhsT=hc[par][:, f, sub * 128:(sub + 1) * 128], rhs=wv[:, f, :],
                                                                               start=(f == 0), stop=(f == 7)), ["wv", hk], [pvk])
                P.op("dve", lambda e, pv=pv, sub=sub, ci=ci: e.tensor_copy(out=Vs[:, 4 * ci + sub, :, 0:64],
                                                                           in_=pv[:, 0:128].rearrange("p (a d) -> p a d", a=2)), [pvk, "Vs"], [("Vs", ci)])
            for h in range(2):
                qk_ = ("QT", h, par)
                nj = 4 * ci + 4
                Okey = ("Ob", h)

                def S(j, h=h, par=par, ci=ci):
                    r = j - 4 * ci
                    q0 = max(0, r) * 128
                    bk = ("sbk", j % 3)
                    bank = sbank[j % 3]
                    diag = r >= 0
                    P.op("pe", lambda e: e.matmul(bank[:, q0:512], lhsT=KT[h][0:65, j * 128:(j + 1) * 128], rhs=QT[h][par][0:65, q0:512],
                                                  start=True, stop=not diag), [("KT", h), ("KT", h, j // 4), qk_], [bk])
                    if diag:
                        P.op("pe", lambda e: e.matmul(bank[:, q0:q0 + 128], lhsT=identb[:], rhs=maskb[:], start=False, stop=True),
                             ["identb", "maskb"], [bk])
                    P.op("act", lambda e: e.activation(out=PTt[j % 3][:, q0:512], in_=bank[:, q0:512], func=AF.Exp, bias=negc[:, j, h:h + 1]),
                         [bk, ("negc", h, j // 4)], [("PT", j % 3)])

                def PV(j, h=h, ci=ci, nj=nj):
                    r = j - 4 * ci
                    q0 = max(0, r) * 128
                    P.op("pe", lambda e: e.matmul(Ob[h][0:65, q0:512], lhsT=Vs[:, j, h, :], rhs=PTt[j % 3][:, q0:512],
                                                  start=(j == 0), stop=(j == nj - 1)), ["Vs", ("Vs", j // 4), ("PT", j % 3)], [Okey])

                S(0)
                S(1)
                for j in range(nj):
                    if j + 2 < nj:
                        S(j + 2)
                    PV(j)
                    if j == 1 and pending:
                        pending.pop(0)()
                def norm(h=h, ci=ci, Okey=Okey):
                    nonlocal oi
                    P.op("act", lambda e: e.copy(out=Osb[:], in_=Ob[h][0:65, :]), [Okey], ["Osb"])
                    P.op("dve", lambda e: e.reciprocal(out=rcp[64:65, :], in_=Osb[64:65, :]), ["Osb"], ["rcp"])
                    P.op("pe", lambda e: e.matmul(pmz[0:64, :], lhsT=onesf[64:65, 0:64], rhs=rcp[64:65, :], start=True, stop=True), ["onesf", "rcp"], ["pmz"])
                    ok = ("ofb", oi % 2)
                    o_t = ofb[oi % 2]
                    oi += 1
                    P.op("dve", lambda e: e.tensor_tensor(out=o_t[:], in0=Osb[0:64, :], in1=pmz[0:64, :], op=ALU.mult), ["Osb", "pmz"], [ok])
                    P.dma("sp", mix_ap[ci // 8, 64 + 64 * h:128 + 64 * h, (ci % 8) * 512:(ci % 8 + 1) * 512], o_t[:], [ok], [("mo", ci, h)], ok)
                pending.append(norm)
                if gather_cb is not None and h == 0 and ci % 8 == 0 and ci > 0:
                    gather_cb(ci // 8 - 1)
        while pending:
            pending.pop(0)()
        if gather_cb is not None:
            gather_cb(nch // 8 - 1)
            gather_cb(None)
        P.barrier()
        return P.emit()


def mix_perm():
    perm = []
    for s in range(4):
        perm += list(range(64 * s, 64 * s + 64)) + list(range(256 + 128 * s, 256 + 128 * s + 128)) \
            + list(range(768 + 64 * s, 768 + 64 * s + 64))
    return np.array(perm)


def a_weights(inp, l, s):
    w = inp["w_in"][l]
    A, B = 2 * s, 2 * s + 1
    cols = []
    cols += list(range(OFF[0] + 64 * s, OFF[0] + 64 * s + 64))
    cols += list(range(OFF[1] + 64 * s, OFF[1] + 64 * s + 64))
    cols += list(range(OFF[2] + 64 * A, OFF[2] + 64 * A + 64)) + [OFF[5] + A]
    cols += list(range(OFF[2] + 64 * B, OFF[2] + 64 * B + 64)) + [OFF[5] + B]
    cols += list(range(OFF[3] + 64 * A, OFF[3] + 64 * A + 64))
    cols += list(range(OFF[3] + 64 * B, OFF[3] + 64 * B + 64))
    cols += list(range(OFF[4] + 64 * A, OFF[4] + 64 * A + 64))
    cols += list(range(OFF[4] + 64 * B, OFF[4] + 64 * B + 64))
    for g in (6, 7, 8, 9):
        cols += list(range(OFF[g] + 64 * s, OFF[g] + 64 * s + 64))
    wa = np.ascontiguousarray(w[:, cols])
    wg2 = np.ascontiguousarray(np.concatenate([inp["w_rg"][l, s], inp["w_ig"][l, s]], axis=1))
    log_gamma = np.log1p(-np.exp2(-5.0 - np.arange(4, dtype=np.float32))).astype(np.float32)
    lg = log_gamma[s]
    idx = np.arange(128, dtype=np.float32)
    pa = np.zeros((128, 16), np.float32)
    sl = slice(64 * s, 64 * s + 64)
    for k in range(4):
        pa[0:64, k] = inp["conv_w"][l, k, sl]
    pa[0:64, 4] = inp["conv_b"][l, sl]
    pa[0:64, 5] = inp["b_rg"][l, sl]
    pa[0:64, 6] = inp["b_ig"][l, sl]
    pa[0:64, 7] = inp["lru_lambda"][l, sl]
    pa[:, 8] = np.exp(lg * np.float32(128.0))
    pa[:, 9] = inp["fox_b_f"][l, A]
    pa[:, 10] = inp["fox_b_f"][l, B]
    pa[:, 11] = np.exp(lg * (np.float32(127.0) - idx)) * np.float32(0.125)
    cst = np.zeros((128, 416), np.float32)
    diff = idx[:, None] - idx[None, :]
    decay = np.where(diff >= 0, np.exp(lg * np.maximum(diff, 0.0)), 0.0).astype(np.float32)
    cst[:, 0:128] = decay.T * np.float32(0.125)
    cst[:, 128:256] = np.exp(lg * (idx + 1.0))[None, :]
    half = 32
    cst[:, 256:288] = (np.float32(10000.0) ** (-np.arange(half, dtype=np.float32) / np.float32(half))).astype(np.float32)[None, :]
    kk = np.arange(128)
    cst[:, 288:416] = np.where(kk[:, None] > kk[None, :], -30000.0, 0.0)
    return wa, wg2, pa, cst


def _dt(nc, n, s, t=F32, k="ExternalInput"):
    return nc.dram_tensor(n, s, t, kind=k).ap()


GROUPS = [[0, 1, 2, 3], [4, 5, 6, 7]]


def build_fused(stop=99):
    from concourse.bass import DynSlice
    nc = bass.Bass("TRN2", target_bir_lowering=False)
    x = _dt(nc, "x", [TOK, D])
    mem = _dt(nc, "mem", [256, D])
    gm = _dt(nc, "gm", [D])
    pos = _dt(nc, "pos", [SEQ], I32)
    cst = _dt(nc, "cst", [128, 416])
    g0 = _dt(nc, "g0", [D])
    L = []
    for l in range(DEPTH):
        d = {"wa": _dt(nc, "wa%d" % l, [D, 770]), "wg2": _dt(nc, "wg2%d" % l, [64, 128]), "pa": _dt(nc, "pa%d" % l, [128, 16])}
        for n in ["w_out", "w_cq", "w_ck", "w_cv", "w_co"]:
            d[n] = _dt(nc, "%s%d" % (n, l), [D, D])
        for n in ["g_post_mix", "g_pre_cross", "g_post_cross", "g_pre_ffn", "g_post_ffn", "g_next"]:
            d[n] = _dt(nc, "%s%d" % (n, l), [D])
        d["w_gate"] = _dt(nc, "w_gate%d" % l, [D, DFF])
        d["w_up"] = _dt(nc, "w_up%d" % l, [D, DFF])
        d["w_down"] = _dt(nc, "w_down%d" % l, [DFF, D])
        L.append(d)
    out = _dt(nc, "out", [TOK, D], F32, "ExternalOutput")
    hT_own = nc.dram_tensor("hT_own", [8 * D, 512], BF16).ap()
    hT_all = nc.dram_tensor("hT_all", [32 * D, 512], BF16).ap()
    mix_c = nc.dram_tensor("mix_c", [4 * 256, TOK], BF16).ap()
    mixG = nc.dram_tensor("mixG", [16 * 256, TOK], BF16).ap()
    x2s = nc.dram_tensor("x2s", [TOK, D], F32).ap()
    x3s = nc.dram_tensor("x3s", [TOK, D], F32).ap()
    hT3 = hT_all
    mix3 = mix_c.rearrange("(j f) t -> j f t", j=4)
    mixGv = mixG.rearrange("(r j a p) t -> p r j a t", r=4, j=4, a=2, p=128)

    mixT_own = nc.dram_tensor("mixT_own", [D, TOK], BF16).ap()
    mixG5 = mixG.rearrange("(j a r p) t -> j a r p t", j=4, a=2, r=4, p=128)

    with ExitStack() as es:
        P = Prog(nc, es)

        def gather8(src, dst):
            for q in range(8):
                P.collective("AllGather", src[q * 128:(q + 1) * 128, :], dst[q * 512:(q + 1) * 512, :], GROUPS, ["a"], [("b", q)])
            P.cc_follow([("b", q) for q in range(8)])

        def gather(src, dst, rk, wk):
            gather8(src, dst)
            P.barrier()
            P.emit()

        def hT_gather(st):
            if st is None:
                P.cc_follow([("hTg", q) for q in range(8)])
            else:
                P.collective("AllGather", hT_own[st * 1024:(st + 1) * 1024, :], hT_all[st * 4096:(st + 1) * 4096, :], GROUPS,
                             [("hT_out", st)], [("hTg", st)])

        def mix_gather(j):
            if j is None:
                P.cc_follow([("b", q) for q in range(8)])
            else:
                rk = [("mo", c_, h_) for c_ in range(8 * j, 8 * j + 8) for h_ in range(2)]
                for q in (2 * j, 2 * j + 1):
                    P.collective("AllGather", mix_c[q * 128:(q + 1) * 128, :], mixG[q * 512:(q + 1) * 512, :], GROUPS, rk, [("b", q)])

        emit_P0(nc, P, x, g0, hT_own, TOK, gather_cb=hT_gather)
        for l in range(DEPTH):
            d = L[l]
            last = l == DEPTH - 1
            if stop <= 2 + 10 * l:
                return nc
            emit_A_lru(nc, P, hT3, d["wa"], d["wg2"], d["pa"], mix3)
            emit_A_ret(nc, P, hT3, d["wa"], d["pa"], cst, pos, mix3)
            emit_A_fox(nc, P, hT3, d["wa"], d["pa"], cst, mix3, gather_cb=mix_gather)
            sv = {}
            for r_ in range(4):
                def sel(e, r_=r_):
                    if "s" not in sv:
                        sv["s"] = e.snap(e.partition_id() % 4)
                    return e.dma_start(out=mixT_own[r_ * 256:(r_ + 1) * 256, :].rearrange("(a p) t -> a p t", a=2),
                                       in_=mixG5[DynSlice(sv["s"], 1), :, r_, :, :].rearrange("j a p t -> (j a) p t"))
                P.op("pool", sel, [("b", q) for q in range(8)], [("mo", r_)], dma=("mo", r_))
            P.barrier()
            P.emit()
            if stop <= 4 + 10 * l:
                return nc
            emit_B1(nc, P, mixT_own, x if l == 0 else x3s, mem, gm, d["w_out"], d["w_cq"], d["w_ck"], d["w_cv"], d["w_co"],
                    d["g_post_mix"], d["g_pre_cross"], d["g_post_cross"], x2s)
            emit_B2(nc, P, x2s, d["w_gate"], d["w_up"], d["w_down"], d["g_pre_ffn"], d["g_post_ffn"],
                    None if last else d["g_next"], out if last else x3s, None if last else hT_own, gather_cb=hT_gather)
    return nc


def kernel(**inp):
    inp = {k: np.asarray(v) for k, v in inp.items()}
    cores = list(range(8))
    perm = mix_perm()
    x = np.ascontiguousarray(inp["x"], dtype=np.float32)
    maps = []
    for c in cores:
        b, s = c // 4, c % 4
        m = {"x": np.ascontiguousarray(x[b, s * TOK:(s + 1) * TOK]), "mem": np.ascontiguousarray(inp["mem"][b]),
             "gm": inp["mem_norm_g"], "pos": np.ascontiguousarray(inp["positions"][b]).astype(np.int32),
             "g0": inp["pre_mix_g"][0]}
        for l in range(DEPTH):
            wa, wg2, pa, cst = a_weights(inp, l, s)
            m["cst"] = cst
            m["wa%d" % l] = wa
            m["wg2%d" % l] = wg2
            m["pa%d" % l] = pa
            m["w_out%d" % l] = np.ascontiguousarray(inp["w_out"][l][perm])
            for n in ["w_cq", "w_ck", "w_cv", "w_co", "w_gate", "w_up", "w_down"]:
                m["%s%d" % (n, l)] = np.ascontiguousarray(inp[n][l])
            m["g_post_mix%d" % l] = inp["post_mix_g"][l]
            m["g_pre_cross%d" % l] = inp["pre_cross_g"][l]
            m["g_post_cross%d" % l] = inp["post_cross_g"][l]
            m["g_pre_ffn%d" % l] = inp["pre_ffn_g"][l]
            m["g_post_ffn%d" % l] = inp["post_ffn_g"][l]
            m["g_next%d" % l] = inp["pre_mix_g"][min(l + 1, DEPTH - 1)]
        maps.append({k: np.ascontiguousarray(v) for k, v in m.items()})
    res = run_bass_kernel_spmd(build_fused(), maps, core_ids=cores)
    out = np.zeros((NB, SEQ, D), np.float32)
    for c in cores:
        out[c // 4, (c % 4) * TOK:(c % 4 + 1) * TOK] = res.results[c]["out"]
    return out
```
